# Optimizing a Trainium2 kernel written in Bass

```python
import math
import jax
import jax.numpy as jnp
from jax import lax
import numpy as np

D_MODEL = 1024
BATCH = 2
SEQ = 16384
DEPTH = 2

N_MIXERS = 2
HEAD_DIM = 64
N_HEADS = 12
N_KV_HEADS_B = 2
N_MEM_HEADS = 4
MEM_LEN = 256
ATTN_WIDTH = N_HEADS * HEAD_DIM
MEM_WIDTH = N_MEM_HEADS * HEAD_DIM
BRANCH_WIDTH = ATTN_WIDTH + MEM_WIDTH
KV_WIDTH_B = N_KV_HEADS_B * HEAD_DIM
IN_WIDTH_A = 3 * ATTN_WIDTH + MEM_WIDTH + BRANCH_WIDTH
IN_WIDTH_B = ATTN_WIDTH + 2 * KV_WIDTH_B + MEM_WIDTH + BRANCH_WIDTH
MOBA_BLOCK = 256
MOBA_TOPK = 3
MOBA_QCHUNK = 32
WINDOW = 128
N_LAYERS_A = (DEPTH + 1) // 2
N_LAYERS_B = DEPTH // 2
RMS_EPS = 1e-6

kernel_name = "moba_swa_sink_hybrid_trunk"


def rms_norm(x, g):
    xf = x.astype(jnp.float32)
    y = xf * lax.rsqrt(jnp.mean(xf * xf, axis=-1, keepdims=True) + RMS_EPS)
    return (y * g.astype(jnp.float32)).astype(x.dtype)


def alibi_slopes(n_heads):
    def pow2_slopes(n):
        start = 2.0 ** (-8.0 / n)
        return [start ** (i + 1) for i in range(n)]
    if math.log2(n_heads).is_integer():
        vals = pow2_slopes(n_heads)
    else:
        c = 2 ** math.floor(math.log2(n_heads))
        vals = pow2_slopes(c) + pow2_slopes(2 * c)[0::2][: n_heads - c]
    return jnp.asarray(np.array(vals, dtype=np.float32))


def moba_attention(q, k, v, slopes):
    b, s, h, dh = q.shape
    nblk = -(-s // MOBA_BLOCK)
    sp = nblk * MOBA_BLOCK
    topk = min(MOBA_TOPK, nblk)
    pad = ((0, 0), (0, sp - s), (0, 0), (0, 0))
    q = jnp.pad(q, pad).transpose(0, 2, 1, 3)
    kb = jnp.pad(k, pad).transpose(0, 2, 1, 3).reshape(b, h, nblk, MOBA_BLOCK, dh)
    vb = jnp.pad(v, pad).transpose(0, 2, 1, 3).reshape(b, h, nblk, MOBA_BLOCK, dh)
    kmean = jnp.mean(kb.astype(jnp.float32), axis=3).astype(kb.dtype)
    scale = dh ** -0.5
    bi = jnp.arange(b)[:, None, None, None]
    hi = jnp.arange(h)[None, :, None, None]
    blk_ids = jnp.arange(nblk, dtype=jnp.int32)
    offs = jnp.arange(MOBA_BLOCK, dtype=jnp.int32)
    qoffs = jnp.arange(MOBA_QCHUNK, dtype=jnp.int32)
    sl5 = slopes[None, :, None, None, None]
    sl4 = slopes[None, :, None, None]

    def chunk(c):
        start = c * MOBA_QCHUNK
        j = start // MOBA_BLOCK
        qc = lax.dynamic_slice_in_dim(q, start, MOBA_QCHUNK, axis=2)
        tpos = start + qoffs
        gate = jnp.einsum('bhqd,bhnd->bhqn', qc, kmean).astype(jnp.float32)
        gate = jnp.where(blk_ids < j, gate, -jnp.inf)
        _, idx = lax.top_k(gate, topk)
        valid = idx < j
        ksel = kb[bi, hi, idx]
        vsel = vb[bi, hi, idx]
        s_sel = jnp.einsum('bhqd,bhqkld->bhqkl', qc, ksel).astype(jnp.float32) * scale
        spos = idx[..., None] * MOBA_BLOCK + offs
        dist_sel = (tpos[None, None, :, None, None] - spos).astype(jnp.float32)
        s_sel = s_sel - sl5 * dist_sel
        s_sel = jnp.where(valid[..., None], s_sel, -jnp.inf).reshape(b, h, MOBA_QCHUNK, topk * MOBA_BLOCK)
        kown = lax.dynamic_index_in_dim(kb, j, axis=2, keepdims=False)
        vown = lax.dynamic_index_in_dim(vb, j, axis=2, keepdims=False)
        dist_own = tpos[:, None] - (j * MOBA_BLOCK + offs)[None, :]
        s_own = jnp.einsum('bhqd,bhld->bhql', qc, kown).astype(jnp.float32) * scale
        s_own = s_own - sl4 * dist_own.astype(jnp.float32)
        s_own = jnp.where(dist_own >= 0, s_own, -jnp.inf)
        p = jax.nn.softmax(jnp.concatenate([s_sel, s_own], axis=-1), axis=-1).astype(vb.dtype)
        p_sel = p[..., : topk * MOBA_BLOCK].reshape(b, h, MOBA_QCHUNK, topk, MOBA_BLOCK)
        p_own = p[..., topk * MOBA_BLOCK:]
        return (jnp.einsum('bhqkl,bhqkld->bhqd', p_sel, vsel)
                + jnp.einsum('bhql,bhld->bhqd', p_own, vown))

    n_chunks = sp // MOBA_QCHUNK
    out = lax.map(chunk, jnp.arange(n_chunks, dtype=jnp.int32))
    out = out.transpose(1, 0, 3, 2, 4).reshape(b, sp, h * dh)
    return out[:, :s]


def swa_sink_attention(q, k, v, sinks, slopes):
    b, s, hq, dh = q.shape
    hkv = k.shape[2]
    g = hq // hkv
    nb = s // WINDOW
    qb = q.reshape(b, nb, WINDOW, hkv, g, dh)
    kb = k.reshape(b, nb, WINDOW, hkv, dh)
    vb = v.reshape(b, nb, WINDOW, hkv, dh)
    padb = ((0, 0), (1, 0), (0, 0), (0, 0), (0, 0))
    kband = jnp.concatenate([jnp.pad(kb, padb)[:, :-1], kb], axis=2)
    vband = jnp.concatenate([jnp.pad(vb, padb)[:, :-1], vb], axis=2)
    logits = jnp.einsum('bnqkgd,bnlkd->bkgnql', qb, kband).astype(jnp.float32) * (dh ** -0.5)
    qi = jnp.arange(WINDOW, dtype=jnp.int32)
    li = jnp.arange(2 * WINDOW, dtype=jnp.int32)
    dist = qi[:, None] + WINDOW - li[None, :]
    kpos = jnp.arange(nb, dtype=jnp.int32)[:, None] * WINDOW - WINDOW + li[None, :]
    mask = ((dist >= 0) & (dist < WINDOW))[None] & (kpos >= 0)[:, None, :]
    sl = slopes.reshape(hkv, g)[None, :, :, None, None, None]
    logits = logits - sl * dist.astype(jnp.float32)
    logits = jnp.where(mask, logits, -jnp.inf)
    sink = sinks.astype(jnp.float32).reshape(hkv, g)[None, :, :, None, None, None]
    m = jnp.maximum(jnp.max(logits, axis=-1, keepdims=True), sink)
    e = jnp.exp(logits - m)
    p = e / (jnp.sum(e, axis=-1, keepdims=True) + jnp.exp(sink - m))
    out = jnp.einsum('bkgnql,bnlkd->bnqkgd', p.astype(vband.dtype), vband)
    return out.reshape(b, s, hq * dh)


def memory_attention(qm, mem_k, mem_v):
    b, s, hm, dh = qm.shape
    logits = jnp.einsum('bshd,bmhd->bhsm', qm, mem_k).astype(jnp.float32) * (dh ** -0.5)
    p = jax.nn.softmax(logits, axis=-1).astype(mem_v.dtype)
    return jnp.einsum('bhsm,bmhd->bshd', p, mem_v).reshape(b, s, hm * dh)


def setup_inputs(seed: int = 0) -> dict:
    key = jax.random.key(seed)
    ks = jax.random.split(key, 10)
    f32 = jnp.float32
    x = jax.random.normal(ks[0], (BATCH, SEQ, D_MODEL), f32)
    mem = jax.random.normal(ks[1], (BATCH, MEM_LEN, D_MODEL), f32)
    norm_g = 1.0 + 0.05 * jax.random.normal(ks[2], (DEPTH, D_MODEL), f32)
    w_in_a = jax.random.normal(ks[3], (N_LAYERS_A, D_MODEL, IN_WIDTH_A), f32) * (D_MODEL ** -0.5)
    w_in_b = jax.random.normal(ks[4], (N_LAYERS_B, D_MODEL, IN_WIDTH_B), f32) * (D_MODEL ** -0.5)
    sinks_b = 0.5 * jax.random.normal(ks[5], (N_LAYERS_B, N_HEADS), f32)
    w_mem_kv = jax.random.normal(ks[6], (DEPTH, D_MODEL, 2 * MEM_WIDTH), f32) * (D_MODEL ** -0.5)
    w_out = jax.random.normal(ks[7], (DEPTH, BRANCH_WIDTH, D_MODEL), f32) * (BRANCH_WIDTH ** -0.5)
    mem_norm_g = 1.0 + 0.05 * jax.random.normal(ks[8], (D_MODEL,), f32)
    final_norm_g = 1.0 + 0.05 * jax.random.normal(ks[9], (D_MODEL,), f32)
    return {"x": x, "mem": mem, "norm_g": norm_g, "w_in_a": w_in_a, "w_in_b": w_in_b,
            "sinks_b": sinks_b, "w_mem_kv": w_mem_kv, "w_out": w_out,
            "mem_norm_g": mem_norm_g, "final_norm_g": final_norm_g}


def reference(x, mem, norm_g, w_in_a, w_in_b, sinks_b, w_mem_kv, w_out, mem_norm_g, final_norm_g):
    b, s, _ = x.shape
    slopes = alibi_slopes(N_HEADS)
    mem_n = rms_norm(mem, mem_norm_g)
    m_len = mem.shape[1]
    for i in range(DEPTH):
        h = rms_norm(x, norm_g[i])
        kvm = mem_n @ w_mem_kv[i]
        mem_k = kvm[..., :MEM_WIDTH].reshape(b, m_len, N_MEM_HEADS, HEAD_DIM)
        mem_v = kvm[..., MEM_WIDTH:].reshape(b, m_len, N_MEM_HEADS, HEAD_DIM)
        if i % N_MIXERS == 0:
            proj = h @ w_in_a[i // N_MIXERS]
            q, k, v, qm, z = jnp.split(
                proj, [ATTN_WIDTH, 2 * ATTN_WIDTH, 3 * ATTN_WIDTH, 3 * ATTN_WIDTH + MEM_WIDTH], axis=-1)
            y_self = moba_attention(q.reshape(b, s, N_HEADS, HEAD_DIM),
                                    k.reshape(b, s, N_HEADS, HEAD_DIM),
                                    v.reshape(b, s, N_HEADS, HEAD_DIM), slopes)
        else:
            proj = h @ w_in_b[i // N_MIXERS]
            q, k, v, qm, z = jnp.split(
                proj, [ATTN_WIDTH, ATTN_WIDTH + KV_WIDTH_B, ATTN_WIDTH + 2 * KV_WIDTH_B,
                       ATTN_WIDTH + 2 * KV_WIDTH_B + MEM_WIDTH], axis=-1)
            y_self = swa_sink_attention(q.reshape(b, s, N_HEADS, HEAD_DIM),
                                        k.reshape(b, s, N_KV_HEADS_B, HEAD_DIM),
                                        v.reshape(b, s, N_KV_HEADS_B, HEAD_DIM),
                                        sinks_b[i // N_MIXERS], slopes)
        y_mem = memory_attention(qm.reshape(b, s, N_MEM_HEADS, HEAD_DIM), mem_k, mem_v)
        y = jnp.concatenate([y_self, y_mem], axis=-1) * jax.nn.silu(z)
        x = x + y @ w_out[i]
    return rms_norm(x, final_norm_g)
```

```python
import contextlib
import math
import numpy as np
import ml_dtypes
import concourse.bass as bass
import concourse.mybir as mybir
from concourse.bass_utils import run_bass_kernel_spmd

F32 = mybir.dt.float32
BF16 = mybir.dt.bfloat16
AF = mybir.ActivationFunctionType
ALU = mybir.AluOpType
AX = mybir.AxisListType
NPBF = ml_dtypes.bfloat16

D = 1024
S = 16384
H = 12
DH = 64
NBLK = 64
NTG = 128
UT = 10
NU = 4
NLT = NU * UT
NLOC = NLT * 128
NQT = 36
WA = 3584
EPS = 1e-6
BIG = 32768.0
NEG = -30000.0
_DBG = {}


def _slopes():
    def p2(n):
        st = 2.0 ** (-8.0 / n)
        return [st ** (i + 1) for i in range(n)]
    c = 2 ** math.floor(math.log2(H))
    vals = p2(c) + p2(2 * c)[0::2][: H - c]
    return np.array(vals, dtype=np.float32)


def _split3(x):
    x = np.asarray(x, dtype=np.float32)
    hi = x.astype(NPBF)
    r1 = (x - hi.astype(np.float32)).astype(np.float32)
    mid = r1.astype(NPBF)
    r2 = (r1 - mid.astype(np.float32)).astype(np.float32)
    lo = r2.astype(NPBF)
    return hi, mid, lo


class Res:
    __slots__ = ("w", "r", "name")

    def __init__(self, name=""):
        self.w = None
        self.r = {}
        self.name = name


class Trk:
    def __init__(self, nc, es):
        self.nc = nc
        self.eng = {"pe": nc.tensor, "act": nc.scalar, "dve": nc.vector,
                    "pool": nc.gpsimd, "sp": nc.sync}
        self.sem = {}
        self.cnt = {}
        for e in ("pe", "act", "dve", "pool"):
            self.sem[e] = es.enter_context(nc.semaphore("c_" + e))
            self.cnt[e] = 0
        self.seen = {e: {} for e in self.eng}
        self.dsem = {}
        self.dcnt = {}
        self.dnext = {}
        for q, n in (("sp", 16), ("act", 16), ("pool", 2)):
            self.dsem[q] = [es.enter_context(nc.semaphore("d_%s%d" % (q, i))) for i in range(n)]
            self.dcnt[q] = [0] * n
            self.dnext[q] = 0
        self.ninstr = 0

    def _wait(self, eng, ev):
        sem, val, key = ev
        if self.seen[eng].get(key, 0) >= val:
            return
        self.eng[eng].wait_ge(sem, val)
        self.seen[eng][key] = val

    def _deps(self, eng, r, w):
        evs = {}

        def add(ev):
            if ev is None:
                return
            k = ev[2]
            if k not in evs or evs[k][1] < ev[1]:
                evs[k] = ev
        for x in r:
            add(x.w)
        for x in w:
            add(x.w)
            for ev in x.r.values():
                add(ev)
        for ev in evs.values():
            if eng == "pe" and ev[2] == "c_pe":
                continue
            self._wait(eng, ev)

    def _upd(self, ev, r, w):
        k = ev[2]
        for x in r:
            x.r[k] = ev
        for x in w:
            x.w = ev
            x.r = {}

    def op(self, eng, fn, r=(), w=()):
        self._deps(eng, r, w)
        ins = fn(self.eng[eng])
        self.cnt[eng] += 1
        ins.then_inc(self.sem[eng], 1)
        ev = (self.sem[eng], self.cnt[eng], "c_" + eng)
        self._upd(ev, r, w)
        self.ninstr += 1
        return ev

    def dma(self, q, out, in_, r=(), w=()):
        self._deps(q, r, w)
        i = self.dnext[q]
        self.dnext[q] = (i + 1) % len(self.dsem[q])
        sem = self.dsem[q][i]
        key = "d_%s%d" % (q, i)
        if self.dcnt[q][i] > 0:
            self._wait(q, (sem, self.dcnt[q][i], key))
        self.eng[q].dma_start(out=out, in_=in_).then_inc(sem, 16)
        self.dcnt[q][i] += 16
        ev = (sem, self.dcnt[q][i], key)
        self._upd(ev, r, w)
        self.ninstr += 1
        return ev

    def barrier(self):
        evs = []
        for e in self.sem:
            if self.cnt[e] > 0:
                evs.append((self.sem[e], self.cnt[e], "c_" + e))
        for q in self.dsem:
            for i, sem in enumerate(self.dsem[q]):
                if self.dcnt[q][i] > 0:
                    evs.append((sem, self.dcnt[q][i], "d_%s%d" % (q, i)))
        for e in self.eng:
            for ev in evs:
                self._wait(e, ev)


def build(stop_after="all", debug=False):
    nc = bass.Bass("TRN2", target_bir_lowering=False)
    es = contextlib.ExitStack()

    def din(name, shape, dt=F32):
        return nc.dram_tensor(name, list(shape), dt, kind="ExternalInput").ap()

    dbg = set(debug) if debug else set()

    def dscr(name, shape, dt):
        kind = "ExternalOutput" if name in dbg else "Internal"
        return nc.dram_tensor(name, list(shape), dt, kind=kind).ap()

    xg = din("xg", [S, D])
    xm = din("xm", [NLOC, D])
    memb = din("memb", [256, D])
    w_in_a = din("w_in_a", [D, WA])
    w_in_b = din("w_in_b", [D, 2304])
    w_mem = din("w_mem", [2, D, 512])
    w_out = din("w_out", [2, D, D])
    gcols = din("gcols", [128, 24])
    fgr = din("fgr", [128, D])
    sinkr = din("sinkr", [128, H])
    kx = din("kx", [64, S], BF16)
    kxo = din("kxo", [64, NLOC], BF16)
    qac = din("qac", [H, 6, NLOC], BF16)
    abias_d = din("abias", [128, H])
    bmask_d = din("bmask", [128, NLT * NBLK])
    cm_d = din("cm", [128, 512], BF16)
    ident_d = din("ident", [128, 128], BF16)
    swac_d = din("swac", [128, 2 * H * 128])
    halob_d = din("halob", [128, NU])
    out_d = nc.dram_tensor("out", [NU * 1024, D], F32, kind="ExternalOutput").ap()

    KT = dscr("KT", [H, 128, S], BF16)
    VP = dscr("VP", [H, 128, NTG, 65], BF16)
    KTo = dscr("KTo", [H, 128, NLOC], BF16)
    VPo = dscr("VPo", [H, 128, NLT, 65], BF16)
    QA = dscr("QA", [H, 2, 128, NLOC], BF16)
    QM = dscr("QM", [2, 128, NLOC], BF16)
    GT = dscr("GT", [NLOC, D], F32)
    YS = dscr("YS", [128, NQT * D], BF16)
    KMD = dscr("KMD", [128, 6 * NBLK], BF16) if "KMD" in dbg else None
    YD = dscr("YD", [128, NQT * D], BF16) if "YD" in dbg else None

    with es:
        T = Trk(nc, es)

        def sb(name, shape, dt, st=None):
            return (st or es).enter_context(nc.sbuf_tensor("s_" + name, list(shape), dt))

        pbig = es.enter_context(nc.psum_tensor("pbig", [128, 4096], F32))
        bank = [Res("bank%d" % i) for i in range(8)]

        def pf(b, n=512, off=0):
            return pbig[:, b * 512 + off: b * 512 + off + n]

        def pbf(b):
            return pbig[:, b * 512:(b + 1) * 512].bitcast(BF16)

        ident = sb("ident", [128, 128], BF16)
        abias = sb("abias", [128, H], F32)
        gc = sb("gc", [128, 24], F32)
        epst = sb("epst", [128, 1], F32)
        junk = sb("junk", [128, 1024], BF16)
        r_const = Res("const")
        T.dma("sp", ident[:], ident_d, w=[r_const])
        T.dma("sp", abias[:], abias_d, w=[r_const])
        T.dma("sp", gc[:], gcols, w=[r_const])
        T.op("dve", lambda e: e.memset(epst[:], EPS), w=[r_const])

        r_KT = [Res("KT%d" % h) for h in range(H)]
        r_KTo = [Res("KTo%d" % h) for h in range(H)]
        r_QA = [Res("QA%d" % h) for h in range(H)]
        for h in range(H):
            eb = 64 if h % 2 == 0 else 0
            T.dma("sp", KT[h, eb:eb + 64, :], kx, w=[r_KT[h]])
            T.dma("sp", KTo[h, eb:eb + 64, :], kxo, w=[r_KTo[h]])
            for v in range(2):
                T.dma("sp", QA[h, v, eb:eb + 6, :], qac[h], w=[r_QA[h]])

        kmem = sb("kmem", [128, 2, 2, 256], BF16)
        vmem = sb("vmem", [128, 2, 2, 4, 65], BF16)
        kmT = sb("kmT", [128, 6, NBLK], BF16)

        stA = contextlib.ExitStack()
        es.enter_context(stA)
        NXS = 3
        xt = [sb("xt%d" % i, [128, D], F32, stA) for i in range(NXS)]
        r_xt = [Res() for _ in range(NXS)]
        xs = [sb("xs%d" % i, [128, D], BF16, stA) for i in range(2)]
        r_xs = [Res() for _ in range(2)]
        ssq = [sb("ssq%d" % i, [128, 4], F32, stA) for i in range(NXS)]
        r_ss = [Res() for _ in range(NXS)]
        hT = [sb("hT%d" % i, [128, 8, 512], BF16, stA) for i in range(2)]
        r_hT = [Res() for _ in range(2)]
        st = {"x": 0, "xs": 0, "tb": 0, "ev": 0}
        TB = (0, 1)

        def evac(out, in_, r, w, scale=None):
            st["ev"] += 1
            if st["ev"] % 2 == 0:
                if scale is None:
                    T.op("act", lambda e: e.copy(out=out, in_=in_), r=r, w=w)
                else:
                    T.op("act", lambda e: e.mul(out=out, in_=in_, mul=scale), r=r, w=w)
            else:
                if scale is None:
                    T.op("dve", lambda e: e.tensor_copy(out=out, in_=in_), r=r, w=w)
                else:
                    T.op("dve", lambda e: e.tensor_scalar(out=out, in0=in_, scalar1=scale, scalar2=None,
                                                          op0=ALU.mult), r=r, w=w)

        def norm_T(src, row0, ntiles, hslot):
            for i in range(ntiles):
                s = st["x"] % NXS
                st["x"] += 1
                T.dma("sp", xt[s][:], src[row0 + i * 128: row0 + (i + 1) * 128, :], w=[r_xt[s]])
                T.op("act", lambda e: e.activation(out=junk[:], in_=xt[s][:], func=AF.Square,
                                                   accum_out=ssq[s][:, 0:1]), r=[r_xt[s]], w=[r_ss[s]])
                T.op("act", lambda e: e.activation(out=ssq[s][:, 1:2], in_=ssq[s][:, 0:1], func=AF.Ln,
                                                   bias=epst[:, 0:1], scale=1.0 / D),
                     r=[r_const], w=[r_ss[s]])
                T.op("act", lambda e: e.activation(out=ssq[s][:, 2:3], in_=ssq[s][:, 1:2], func=AF.Exp, scale=-0.5),
                     r=[], w=[r_ss[s]])
                b = st["xs"] % 2
                st["xs"] += 1
                T.op("dve", lambda e: e.tensor_scalar(out=xs[b][:], in0=xt[s][:], scalar1=ssq[s][:, 2:3],
                                                      scalar2=None, op0=ALU.mult),
                     r=[r_xt[s], r_ss[s]], w=[r_xs[b]])
                tb = TB[st["tb"] % 2]
                st["tb"] += 1
                pv = pbf(tb)
                for k in range(8):
                    T.op("pe", lambda e: e.transpose(pv[:, k * 128:(k + 1) * 128], xs[b][:, k * 128:(k + 1) * 128],
                                                     ident[:]),
                         r=[r_xs[b], r_const], w=[bank[tb]])
                evac(hT[hslot][:, :, i * 128:(i + 1) * 128],
                     pv.rearrange("p (k t) -> p k t", k=8), r=[], w=[bank[tb], r_hT[hslot]])

        def run_interleaved(main, side):
            n, m_ = len(main), len(side)
            j = 0
            for i_, f in enumerate(main):
                f()
                while j < m_ and (j + 1) * n <= (i_ + 1) * m_:
                    side[j]()
                    j += 1
            while j < m_:
                side[j]()
                j += 1

        def norm_items(src, row0, ntiles, hslot, tbs):
            slots = []
            for i in range(ntiles):
                slots.append((st["x"] % NXS, st["xs"] % 2, tbs[st["tb"] % len(tbs)]))
                st["x"] += 1
                st["xs"] += 1
                st["tb"] += 1

            def p0(i):
                s = slots[i][0]
                T.dma("sp", xt[s][:], src[row0 + i * 128: row0 + (i + 1) * 128, :], w=[r_xt[s]])

            def p1(i):
                s, b, tb = slots[i]
                T.op("act", lambda e: e.activation(out=junk[:], in_=xt[s][:], func=AF.Square,
                                                   accum_out=ssq[s][:, 0:1]), r=[r_xt[s]], w=[r_ss[s]])
                T.op("act", lambda e: e.activation(out=ssq[s][:, 1:2], in_=ssq[s][:, 0:1], func=AF.Ln,
                                                   bias=epst[:, 0:1], scale=1.0 / D), r=[r_const], w=[r_ss[s]])
                T.op("act", lambda e: e.activation(out=ssq[s][:, 2:3], in_=ssq[s][:, 1:2], func=AF.Exp, scale=-0.5),
                     r=[], w=[r_ss[s]])
                T.op("dve", lambda e: e.tensor_scalar(out=xs[b][:], in0=xt[s][:], scalar1=ssq[s][:, 2:3],
                                                      scalar2=None, op0=ALU.mult),
                     r=[r_xt[s], r_ss[s]], w=[r_xs[b]])

            def p2(i):
                s, b, tb = slots[i]
                pv = pbf(tb)
                for k in range(8):
                    T.op("pe", lambda e: e.transpose(pv[:, k * 128:(k + 1) * 128], xs[b][:, k * 128:(k + 1) * 128],
                                                     ident[:]), r=[r_xs[b], r_const], w=[bank[tb]])
                evac(hT[hslot][:, :, i * 128:(i + 1) * 128],
                     pv.rearrange("p (k t) -> p k t", k=8), r=[], w=[bank[tb], r_hT[hslot]])

            items = []
            for k_ in range(-1, ntiles + 1):
                def f(k_=k_):
                    if 0 <= k_ + 1 < ntiles:
                        p0(k_ + 1)
                    if 0 <= k_ - 1 < ntiles:
                        p2(k_ - 1)
                    if 0 <= k_ < ntiles:
                        p1(k_)
                items.append(f)
            return items

        wA = sb("wA", [128, 8, WA], BF16, stA)
        r_wA = Res()
        r_wM = Res()
        r_kmT = Res()
        kms = sb("kms", [128, 2], F32, stA)
        r_kms = Res()
        kst = [sb("kst%d" % i, [128, 512], BF16, stA) for i in range(3)]
        r_kst = [Res() for _ in range(3)]
        vst = [sb("vst%d" % i, [128, H, 4, 65], BF16, stA) for i in range(2)]
        r_vst = [Res() for _ in range(2)]
        bmask = sb("bmask", [128, NLT, NBLK], F32, stA)
        Et = [sb("Et%d" % i, [128, H, 2, 128], BF16, stA) for i in range(2)]
        r_Et = [Res(), Res()]
        gm = sb("gm", [128, 6, NBLK], F32, stA)
        r_gm = Res()
        m8 = sb("m8", [128, 6, 8], F32, stA)
        r_m8 = [Res() for _ in range(6)]
        thr = sb("thr", [128, 6], F32, stA)
        selb = sb("selb", [128, 6, NBLK], F32, stA)
        qmst = sb("qmst", [128, 2, 512], BF16, stA)
        r_qmst = Res()
        esb = [sb("esb%d" % i, [128, D], F32, stA) for i in range(2)]
        r_esb = [Res(), Res()]
        gts = [sb("gts%d" % i, [128, D], F32, stA) for i in range(2)]
        r_gts = [Res(), Res()]
        qa_sts = [sb("qa_st0", [128, H, 2, 512], BF16, stA), None]
        r_qasts = [Res(), Res()]
        stW0 = contextlib.ExitStack()
        stA.enter_context(stW0)
        wM = sb("wM", [128, 2, 8, 512], BF16, stW0)
        wst = [sb("wst%d" % i, [128, 1792], F32, stW0) for i in range(2)]
        r_wst = [Res(), Res()]
        stw = {"i": 0}

        def wload(dst, src, ncols, gcol, rdst):
            for c0 in range(0, ncols, 1792):
                n = min(1792, ncols - c0)
                s = stw["i"] % 2
                stw["i"] += 1
                T.dma("sp", wst[s][:, 0:n], src[:, c0:c0 + n], w=[r_wst[s]])
                if s == 0:
                    if gcol is None:
                        T.op("dve", lambda e: e.tensor_copy(out=dst[:, c0:c0 + n], in_=wst[s][:, 0:n]),
                             r=[r_wst[s]], w=[rdst])
                    else:
                        T.op("dve", lambda e: e.tensor_scalar(out=dst[:, c0:c0 + n], in0=wst[s][:, 0:n],
                                                              scalar1=gcol, scalar2=None, op0=ALU.mult),
                             r=[r_wst[s], r_const], w=[rdst])
                else:
                    if gcol is None:
                        T.op("act", lambda e: e.copy(out=dst[:, c0:c0 + n], in_=wst[s][:, 0:n]),
                             r=[r_wst[s]], w=[rdst])
                    else:
                        T.op("act", lambda e: e.activation(out=dst[:, c0:c0 + n], in_=wst[s][:, 0:n], func=AF.Copy,
                                                           scale=gcol),
                             r=[r_wst[s], r_const], w=[rdst])

        for L in range(2):
            for k in range(8):
                wload(wM[:, L, k, :], w_mem[L, k * 128:(k + 1) * 128, :], 512, gc[:, 16 + k:17 + k], r_wM)
        for k in range(8):
            wload(wA[:, k, :], w_in_a[k * 128:(k + 1) * 128, :], WA, gc[:, k:k + 1], r_wA)
        T.dma("sp", bmask[:].rearrange("p a b -> p (a b)"), bmask_d, w=[r_const])
        for i in range(2):
            T.op("dve", lambda e: e.memset(vst[i][:].rearrange("p a b c -> p (a b c)"), 1.0), w=[r_vst[i]])
            T.op("dve", lambda e: e.memset(Et[i][:].rearrange("p a b c -> p (a b c)"), 0.0), w=[r_Et[i]])

        r_kmem = Res()
        r_vmem = Res()
        T.op("dve", lambda e: e.memset(vmem[:].rearrange("p a b c d -> p (a b c d)"), 1.0), w=[r_vmem])
        norm_T(memb, 0, 2, 0)
        for L in range(2):
            for pr in range(2):
                bk = 2 + (L * 2 + pr) % 2
                for k in range(8):
                    T.op("pe", lambda e: e.matmul(pf(bk, 256), lhsT=wM[:, L, k, pr * 128:(pr + 1) * 128],
                                                  rhs=hT[0][:, k, 0:256], start=(k == 0), stop=(k == 7)),
                         r=[r_wM, r_hT[0]], w=[bank[bk]])
                evac(kmem[:, L, pr, :], pf(bk, 256), r=[], w=[bank[bk], r_kmem])
            for mt in range(2):
                bk = 4 + mt
                for k in range(8):
                    T.op("pe", lambda e: e.matmul(pf(bk, 256), lhsT=hT[0][:, k, mt * 128:(mt + 1) * 128],
                                                  rhs=wM[:, L, k, 256:512], start=(k == 0), stop=(k == 7)),
                         r=[r_wM, r_hT[0]], w=[bank[bk]])
                evac(vmem[:, L, mt, :, 0:64], pf(bk, 256).rearrange("p (h c) -> p h c", h=4),
                     r=[], w=[bank[bk], r_vmem])
        T.barrier()
        stW0.close()
        qa_sts[1] = sb("qa_st1", [128, H, 2, 512], BF16, stA)

        sk = {"k": 0, "pb": 0, "PB": (2, 3, 4, 5, 6, 7)}

        def nextbank():
            PB = sk["PB"]
            b = PB[sk["pb"] % len(PB)]
            sk["pb"] += 1
            return b

        def proj_items(hslot, ktdst, r_ktdst, vpdst, r_vpdst, col0, gi, do_kmean):
            items = []

            def kpair(p):
                bk = nextbank()
                for k in range(8):
                    T.op("pe", lambda e: e.matmul(pf(bk), lhsT=wA[:, k, 768 + p * 128: 768 + (p + 1) * 128],
                                                  rhs=hT[hslot][:, k, :], start=(k == 0), stop=(k == 7)),
                         r=[r_wA, r_hT[hslot]], w=[bank[bk]])
                s = sk["k"] % 3
                sk["k"] += 1
                if do_kmean:
                    T.op("dve", lambda e: e.tensor_reduce(out=kms[:], in_=pf(bk).rearrange("p (b t) -> p b t", b=2),
                                                          axis=AX.X, op=ALU.add),
                         r=[], w=[bank[bk], r_kms])
                    T.op("dve", lambda e: e.tensor_scalar(out=kmT[:, p, 2 * (gi // 4): 2 * (gi // 4) + 2], in0=kms[:],
                                                          scalar1=1.0 / 256.0, scalar2=None, op0=ALU.mult),
                         r=[r_kms], w=[r_kmT])
                evac(kst[s][:], pf(bk), r=[], w=[bank[bk], r_kst[s]])
                if not _DBG.get("nokst"):
                    T.dma("act", ktdst[2 * p, 0:64, col0:col0 + 512], kst[s][0:64, :], r=[r_kst[s]], w=[r_ktdst[2 * p]])
                    T.dma("act", ktdst[2 * p + 1, 64:128, col0:col0 + 512], kst[s][64:128, :], r=[r_kst[s]],
                          w=[r_ktdst[2 * p + 1]])

            vs = (gi // 4) % 2

            def vgrp(i, c0, n, h0, last):
                bk = nextbank()
                for k in range(8):
                    T.op("pe", lambda e: e.matmul(pf(bk, n), lhsT=hT[hslot][:, k, i * 128:(i + 1) * 128],
                                                  rhs=wA[:, k, 1536 + c0: 1536 + c0 + n],
                                                  start=(k == 0), stop=(k == 7)),
                         r=[r_wA, r_hT[hslot]], w=[bank[bk]])
                evac(vst[vs][:, h0:h0 + n // 64, i, 0:64], pf(bk, n).rearrange("p (h c) -> p h c", c=64),
                     r=[], w=[bank[bk], r_vst[vs]])
                if last and not _DBG.get("novst"):
                    T.dma("act", vpdst[:, :, gi:gi + 4, :].rearrange("h p t c -> p h (t c)"),
                          vst[vs][:].rearrange("p h t c -> p h (t c)"), r=[r_vst[vs]], w=r_vpdst)

            for p in range(6):
                items.append(lambda p=p: kpair(p))
            for i in range(4):
                items.append(lambda i=i: vgrp(i, 0, 512, 0, False))
                items.append(lambda i=i: vgrp(i, 512, 256, 8, i == 3))
            return items

        r_VP = [Res("VP")]
        r_VPo = [Res("VPo")]
        NG1 = NTG // 4
        if stop_after == "A1s":
            NG1 = 2
        for f in norm_items(xg, 0, 4, 0, TB):
            f()
        for G in range(NG1):
            main = proj_items(G % 2, KT, r_KT, VP, r_VP, G * 512, G * 4, True)
            side = norm_items(xg, (G + 1) * 512, 4, (G + 1) % 2, TB) if G + 1 < NG1 else []
            run_interleaved(main, side)
        if KMD is not None:
            T.dma("sp", KMD, kmT[:].rearrange("p a b -> p (a b)"), r=[r_kmT])
        if stop_after in ("A1", "A1s"):
            T.barrier()
            return nc

        r_QM = Res()
        r_GT = Res()
        NG2 = NLT // 4
        if stop_after == "A2s":
            NG2 = 1
        cnt2 = {"e": 0, "z": 0}
        sk["PB"] = (5, 6, 7)
        TB2 = (0,)

        def a2_main_items(G):
            hs = G % 2
            qa_st = qa_sts[G % 2]
            r_qast = r_qasts[G % 2]
            items = proj_items(hs, KTo, r_KTo, VPo, r_VPo, G * 512, G * 4, False)

            def qpair(p):
                bk = nextbank()
                for k in range(8):
                    T.op("pe", lambda e: e.matmul(pf(bk), lhsT=wA[:, k, p * 128:(p + 1) * 128],
                                                  rhs=hT[hs][:, k, :], start=(k == 0), stop=(k == 7)),
                         r=[r_wA, r_hT[hs]], w=[bank[bk]])
                for e_ in range(2):
                    rows = slice(e_ * 64, (e_ + 1) * 64)
                    for v in range(2):
                        evac(qa_st[rows, 2 * p + e_, v, :], pbig[rows, bk * 512:(bk + 1) * 512], r=[],
                             w=[bank[bk], r_qast], scale=0.125)

            def qmpair(p):
                bk = nextbank()
                for k in range(8):
                    T.op("pe", lambda e: e.matmul(pf(bk), lhsT=wA[:, k, 2304 + p * 128: 2304 + (p + 1) * 128],
                                                  rhs=hT[hs][:, k, :], start=(k == 0), stop=(k == 7)),
                         r=[r_wA, r_hT[hs]], w=[bank[bk]])
                evac(qmst[:, p, :], pf(bk), r=[], w=[bank[bk], r_qmst], scale=0.125)
                if p == 1:
                    T.dma("act", QM[:, :, G * 512:(G + 1) * 512].rearrange("a p c -> p a c"), qmst[:],
                          r=[r_qmst], w=[r_QM])

            def ztile(i):
                zb = 6
                for half in range(2):
                    for k in range(8):
                        T.op("pe", lambda e: e.matmul(pf(zb + half), lhsT=hT[hs][:, k, i * 128:(i + 1) * 128],
                                                      rhs=wA[:, k, 2560 + half * 512: 2560 + (half + 1) * 512],
                                                      start=(k == 0), stop=(k == 7)),
                             r=[r_wA, r_hT[hs]], w=[bank[zb + half]])
                zs = cnt2["z"] % 2
                cnt2["z"] += 1
                zps = pbig[:, zb * 512:(zb + 2) * 512]
                T.op("act", lambda e: e.activation(out=esb[zs][:], in_=zps, func=AF.Exp, scale=-1.0),
                     r=[], w=[bank[zb], bank[zb + 1], r_esb[zs]])
                T.op("act", lambda e: e.copy(out=gts[zs][:], in_=zps), r=[], w=[bank[zb], bank[zb + 1], r_gts[zs]])
                T.op("act", lambda e: e.activation(out=esb[zs][:], in_=esb[zs][:], func=AF.Ln, bias=1.0),
                     r=[], w=[r_esb[zs]])
                T.op("act", lambda e: e.activation(out=esb[zs][:], in_=esb[zs][:], func=AF.Exp, scale=-1.0),
                     r=[], w=[r_esb[zs]])
                T.op("dve", lambda e: e.tensor_tensor(out=gts[zs][:], in0=gts[zs][:], in1=esb[zs][:], op=ALU.mult),
                     r=[r_esb[zs]], w=[r_gts[zs]])
                lt = G * 4 + i
                T.dma("act", GT[lt * 128:(lt + 1) * 128, :], gts[zs][:], r=[r_gts[zs]], w=[r_GT])

            for p in range(6):
                items.append(lambda p=p: qpair(p))
            for p in range(2):
                items.append(lambda p=p: qmpair(p))
            for i in range(4):
                items.append(lambda i=i: ztile(i))
            return items

        def a2_gate_items(G):
            qa_st = qa_sts[G % 2]
            r_qast = r_qasts[G % 2]
            items = []

            def gate(i, e_):
                lt = G * 4 + i
                E = Et[i % 2]
                r_E = r_Et[i % 2]
                gb = 1 + e_
                rows = slice(e_ * 64, (e_ + 1) * 64)
                for p in range(6):
                    T.op("pe", lambda e: e.matmul(pf(gb, 64, p * 64), lhsT=qa_st[rows, 2 * p + e_, 0, i * 128:(i + 1) * 128],
                                                  rhs=kmT[rows, p, :], start=True, stop=True),
                         r=[r_qast, r_kmT], w=[bank[gb]])
                T.op("dve", lambda e: e.tensor_tensor(out=gm[:], in0=pf(gb, 384).rearrange("p (a b) -> p a b", a=6),
                                                      in1=bmask[:, lt:lt + 1, :].to_broadcast([128, 6, NBLK]),
                                                      op=ALU.add),
                     r=[r_const], w=[bank[gb], r_gm])
                for p in range(6):
                    T.op("dve", lambda e: e.max(out=m8[:, p, :], in_=gm[:, p, :]), r=[r_gm], w=[r_m8[p]])
                T.op("dve", lambda e: e.tensor_scalar(out=thr[:], in0=m8[:, :, 2], scalar1=-1e29, scalar2=None,
                                                      op0=ALU.max), r=r_m8, w=[r_gm])
                T.op("dve", lambda e: e.tensor_tensor(out=selb[:], in0=gm[:],
                                                      in1=thr[:].unsqueeze(2).to_broadcast([128, 6, NBLK]),
                                                      op=ALU.is_ge), r=[], w=[r_gm])
                cb = (64 if e_ == 0 else 0) + 6
                for v in range(2):
                    T.op("dve", lambda e: e.tensor_scalar(out=E[:, e_::2, v, cb:cb + 32],
                                                          in0=selb[:, :, v * 32:(v + 1) * 32],
                                                          scalar1=1.0, scalar2=BIG, op0=ALU.subtract, op1=ALU.mult),
                         r=[], w=[r_gm, r_E])

            def etr(i, e_):
                E = Et[i % 2]
                r_E = r_Et[i % 2]
                ext = slice(64, 128) if e_ == 0 else slice(0, 64)
                for hb in range(2):
                    tbk = 3 + hb
                    pv = pbf(tbk)
                    for j in range(3):
                        h = e_ + 2 * (3 * hb + j)
                        for v in range(2):
                            sl_ = (j * 2 + v) * 128
                            T.op("pe", lambda e: e.transpose(pv[:, sl_:sl_ + 128], E[:, h, v, :], ident[:]),
                                 r=[r_E, r_const], w=[bank[tbk]])
                    h0 = e_ + 6 * hb
                    evac(qa_st[ext, h0:h0 + 5:2, :, i * 128:(i + 1) * 128],
                         pv[ext, 0:768].rearrange("p (a b t) -> p a b t", a=3, b=2),
                         r=[], w=[bank[tbk], r_qast])

            def store():
                cols = slice(G * 512, (G + 1) * 512)
                for e_ in range(2):
                    dh = slice(0, 64) if e_ == 0 else slice(64, 128)
                    ex = slice(70, 128) if e_ == 0 else slice(6, 64)
                    rq = [r_QA[h] for h in range(e_, H, 2)]
                    for v in range(2):
                        T.dma("act", QA[e_::2, v, dh, cols].rearrange("h r c -> r h c"), qa_st[dh, e_::2, v, :],
                              r=[r_qast], w=rq)
                        T.dma("act", QA[e_::2, v, ex, cols].rearrange("h r c -> r h c"), qa_st[ex, e_::2, v, :],
                              r=[r_qast], w=rq)

            g_ = [[(lambda i=i, e_=e_: gate(i, e_)) for e_ in range(2)] for i in range(4)]
            t_ = [[(lambda i=i, e_=e_: etr(i, e_)) for e_ in range(2)] for i in range(4)]
            items = g_[0] + g_[1] + t_[0] + g_[2] + t_[1] + g_[3] + t_[2] + t_[3] + [store]
            return items

        def merge(a, b):
            out = []
            n, m_ = len(a), len(b)
            j = 0
            for i_, f in enumerate(a):
                out.append(f)
                while j < m_ and (j + 1) * n <= (i_ + 1) * m_:
                    out.append(b[j])
                    j += 1
            out.extend(b[j:])
            return out

        for f in norm_items(xm, 0, 4, 0, TB2):
            f()
        for G in range(NG2 + 1):
            main = a2_main_items(G) if G < NG2 else []
            side = []
            if G >= 1:
                side = a2_gate_items(G - 1)
            if G + 1 < NG2:
                side = merge(side, norm_items(xm, (G + 1) * 512, 4, (G + 1) % 2, TB2)) if side else \
                    norm_items(xm, (G + 1) * 512, 4, (G + 1) % 2, TB2)
            if main:
                run_interleaved(main, side)
            else:
                for f in side:
                    f()
        if stop_after in ("A2", "A2s"):
            T.barrier()
            return nc

        T.barrier()
        stA.close()
        stY = contextlib.ExitStack()
        es.enter_context(stY)
        Y = sb("Y", [128, NQT, D], BF16, stY)
        r_Y = Res("Y")
        stB = contextlib.ExitStack()
        es.enter_context(stB)
        cmt = sb("cmt", [128, 2, 256], BF16, stB)
        T.dma("sp", cmt[:].rearrange("p a b -> p (a b)"), cm_d, w=[r_const])
        QAs = [sb("QAs%d" % i, [128, 2, NLOC], BF16, stB) for i in range(2)]
        r_QAs = [Res(), Res()]
        KTos = [sb("KTos%d" % i, [128, NLOC], BF16, stB) for i in range(2)]
        VPos = [sb("VPos%d" % i, [128, NLT, 65], BF16, stB) for i in range(2)]
        r_own = [Res(), Res()]
        NKS = 3
        kring = [sb("kring%d" % i, [128, 1024], BF16, stB) for i in range(NKS)]
        vring = [sb("vring%d" % i, [128, 8, 65], BF16, stB) for i in range(NKS)]
        r_ring = [Res() for _ in range(NKS)]
        NPT = 3
        pT = [sb("pT%d" % i, [128, 2, 384], BF16, stB) for i in range(NPT)]
        r_pT = [Res() for _ in range(NPT)]
        rec = sb("rec", [128, 9], F32, stB)
        r_rec = Res()
        qmT = sb("qmT", [128, 2, NLOC], BF16, stB)
        r_qmT = Res()
        T.dma("sp", qmT[:], QM.rearrange("a p c -> p a c"), r=[r_QM], w=[r_qmT])

        heads = list(range(H))
        units = list(range(NU))
        if stop_after == "B1s":
            heads = [0, 1, 7]
            units = [0, 1]

        def load_head(h, hb):
            for v in range(2):
                T.dma("sp", QAs[hb][:, v, :], QA[h, v], r=[r_QA[h]], w=[r_QAs[hb]])
            T.dma("sp", KTos[hb][:], KTo[h], r=[r_KTo[h]], w=[r_own[hb]])
            T.dma("sp", VPos[hb][:], VPo[h], r=r_VPo, w=[r_own[hb]])

        chunks = []
        for h in heads:
            for m in units:
                nb = 16 * m + 15
                for c in range((nb + 3) // 4):
                    chunks.append((h, m, c))
        cstate = {"next": 0}

        def ensure_loaded(ci):
            while cstate["next"] <= min(ci, len(chunks) - 1):
                n = cstate["next"]
                h, m, c = chunks[n]
                sl = n % NKS
                T.dma("sp", kring[sl][:], KT[h, :, c * 1024:(c + 1) * 1024], r=[r_KT[h]], w=[r_ring[sl]])
                T.dma("sp", vring[sl][:], VP[h, :, c * 8:(c + 1) * 8, :], r=r_VP, w=[r_ring[sl]])
                cstate["next"] += 1

        sst = {"s": 0}
        LAG = 2
        ci = 0
        for hi, h in enumerate(heads):
            hb = hi % 2
            if hi == 0:
                load_head(h, 0)
            if hi + 1 < len(heads):
                load_head(heads[hi + 1], (hi + 1) % 2)
            for m in units:
                nb = 16 * m + 15
                cnts = (16 * m + 12, 16 * m + 14, 16 * m + 15)
                steps = []
                for c in range((nb + 3) // 4):
                    for j in range(4 * c, min(4 * c + 4, nb)):
                        for g in range(3):
                            if j < cnts[g]:
                                steps.append(("past", j, g, ci + c))
                for qb in range(5):
                    steps.append(("own", qb, 0, -1))
                first = {6: True, 7: True}
                nst = len(steps)
                info = [None] * nst
                for idx in range(nst + LAG):
                    if idx < nst:
                        kind, a, g, cidx = steps[idx]
                        sbuf_i = sst["s"] % 3
                        sst["s"] += 1
                        b0 = 2 * sbuf_i
                        if kind == "past":
                            j = a
                            ensure_loaded(cidx + 1)
                            sl = cidx % NKS
                            v = j // 32
                            qc = (UT * m + 1 + 3 * g) * 128
                            N = 384
                            for kt2 in range(2):
                                kc = (j % 4) * 256 + kt2 * 128
                                T.op("pe", lambda e: e.matmul(pf(b0 + kt2, N), lhsT=kring[sl][:, kc:kc + 128],
                                                              rhs=QAs[hb][:, v, qc:qc + N], start=True, stop=True),
                                     r=[r_ring[sl], r_QAs[hb]], w=[bank[b0 + kt2]])
                            tqs = [3 * g + i_ for i_ in range(3)]
                            vsrc = [(vring[sl], (j % 4) * 2 + kt2) for kt2 in range(2)]
                            rres = [r_ring[sl]]
                        else:
                            qb = a
                            if qb == 0:
                                qc, N, c0 = (UT * m + 1) * 128, 128, 128
                                tqs = [0]
                            else:
                                qc, N, c0 = (UT * m + 2 * qb) * 128, 256, 0
                                tqs = [2 * qb - 1, 2 * qb]
                            for kt2 in range(2):
                                kc = (UT * m + 2 * qb + kt2) * 128
                                T.op("pe", lambda e: e.matmul(pf(b0 + kt2, N), lhsT=KTos[hb][:, kc:kc + 128],
                                                              rhs=QAs[hb][:, 0, qc:qc + N], start=True, stop=False),
                                     r=[r_own[hb], r_QAs[hb]], w=[bank[b0 + kt2]])
                                T.op("pe", lambda e: e.matmul(pf(b0 + kt2, N), lhsT=ident[:],
                                                              rhs=cmt[:, kt2, c0:c0 + N], start=False, stop=True),
                                     r=[r_const], w=[bank[b0 + kt2]])
                            vsrc = [(VPos[hb], UT * m + 2 * qb + kt2) for kt2 in range(2)]
                            rres = [r_own[hb]]
                        ps = sst["s"] % NPT
                        sview = pbig[:, b0 * 512:(b0 + 2) * 512].rearrange("p (a b) -> p a b", a=2)[:, :, 0:N]
                        T.op("act", lambda e: e.activation(out=pT[ps][:, :, 0:N], in_=sview, func=AF.Exp,
                                                           bias=abias[:, h:h + 1], scale=1.0),
                             r=[r_const], w=[bank[b0], bank[b0 + 1], r_pT[ps]])
                        info[idx] = (ps, tqs, vsrc, rres)
                    k_ = idx - LAG
                    if k_ >= 0:
                        ps, tqs, vsrc, rres = info[k_]
                        for qi_, tq in enumerate(tqs):
                            bk = 6 if tq < 5 else 7
                            col = (tq if tq < 5 else tq - 5) * 65
                            for kt2 in range(2):
                                vt, vi = vsrc[kt2]
                                fl = first[bk]
                                first[bk] = False
                                T.op("pe", lambda e: e.matmul(pf(bk, 65, col), lhsT=pT[ps][:, kt2, qi_ * 128:(qi_ + 1) * 128],
                                                              rhs=vt[:, vi, :], start=fl, stop=True,
                                                              skip_group_check=True),
                                     r=[r_pT[ps]] + rres, w=[bank[bk]])
                ci += (nb + 3) // 4
                for bk, nt, t0_ in ((6, 5, 0), (7, 4, 5)):
                    av = pf(bk, nt * 65).rearrange("p (a b) -> p a b", b=65)
                    T.op("dve", lambda e: e.reciprocal(out=rec[:, 0:nt], in_=av[:, :, 64]), r=[], w=[bank[bk], r_rec])
                    T.op("dve", lambda e: e.tensor_tensor(out=Y[:, 9 * m + t0_: 9 * m + t0_ + nt, h * 64:(h + 1) * 64],
                                                          in0=av[:, :, 0:64],
                                                          in1=rec[:, 0:nt].unsqueeze(2).to_broadcast([128, nt, 64]),
                                                          op=ALU.mult),
                         r=[r_rec], w=[bank[bk], r_Y])
        if YD is not None:
            T.dma("sp", YD, Y[:].rearrange("p a b -> p (a b)"), r=[r_Y])
        if stop_after in ("B1", "B1s"):
            T.barrier()
            return nc

        def mem_attn(L, qsrc, r_qsrc, qcols, dst_fn, r_dst, ntile, sbanks=(0, 2, 4), abanks=(6, 7), heads_=range(4)):
            N = ntile * 128
            for hm in heads_:
                pr, e_ = divmod(hm, 2)
                rows = slice(e_ * 64, (e_ + 1) * 64)
                sbuf_i = sst["s"] % len(sbanks)
                sst["s"] += 1
                b0 = sbanks[sbuf_i]
                for kt2 in range(2):
                    T.op("pe", lambda e: e.matmul(pf(b0 + kt2, N), lhsT=kmem[rows, L, pr, kt2 * 128:(kt2 + 1) * 128],
                                                  rhs=qsrc[rows, pr, qcols:qcols + N], start=True, stop=True),
                         r=[r_kmem, r_qsrc], w=[bank[b0 + kt2]])
                ps = sst["s"] % len(pT)
                sview = pbig[:, b0 * 512:(b0 + 2) * 512].rearrange("p (a b) -> p a b", a=2)[:, :, 0:N]
                T.op("act", lambda e: e.activation(out=pT[ps][:, :, 0:N], in_=sview, func=AF.Exp),
                     r=[], w=[bank[b0], bank[b0 + 1], r_pT[ps]])
                bk = abanks[hm % len(abanks)]
                fl = True
                for qt in range(ntile):
                    for kt2 in range(2):
                        T.op("pe", lambda e: e.matmul(pf(bk, 65, qt * 65), lhsT=pT[ps][:, kt2, qt * 128:(qt + 1) * 128],
                                                      rhs=vmem[:, L, kt2, hm, :], start=fl, stop=True,
                                                      skip_group_check=True),
                             r=[r_pT[ps], r_vmem], w=[bank[bk]])
                        fl = False
                av = pf(bk, ntile * 65).rearrange("p (a b) -> p a b", b=65)
                T.op("dve", lambda e: e.reciprocal(out=rec[:, 0:ntile], in_=av[:, :, 64]), r=[], w=[bank[bk], r_rec])
                T.op("dve", lambda e: e.tensor_tensor(out=dst_fn(hm), in0=av[:, :, 0:64],
                                                      in1=rec[:, 0:ntile].unsqueeze(2).to_broadcast([128, ntile, 64]),
                                                      op=ALU.mult),
                     r=[r_rec], w=[bank[bk], r_dst])

        for m in units:
            for g in range(3):
                q0 = 9 * m + 3 * g
                mem_attn(0, qmT, r_qmT, (UT * m + 1 + 3 * g) * 128,
                         lambda hm: Y[:, q0:q0 + 3, 768 + hm * 64: 768 + (hm + 1) * 64], r_Y, 3)
        if YD is not None:
            T.dma("sp", YD, Y[:].rearrange("p a b -> p (a b)"), r=[r_Y])
        r_YS = Res()
        T.dma("act", YS, Y[:].rearrange("p a b -> p (a b)"), r=[r_Y], w=[r_YS])
        if stop_after in ("B2",):
            T.barrier()
            return nc

        T.barrier()
        stB.close()
        stY.close()
        stC = contextlib.ExitStack()
        es.enter_context(stC)
        wO0 = sb("wO0", [128, 8, D], BF16, stC)
        wO1 = sb("wO1", [128, 8, D], BF16, stC)
        wB = sb("wB", [128, 8, 2432], BF16, stC)
        r_wC = Res()
        stW = contextlib.ExitStack()
        stC.enter_context(stW)
        wst2 = [sb("wst2_%d" % i, [128, 1792], F32, stW) for i in range(2)]
        wst[0], wst[1] = wst2[0], wst2[1]
        r_wst[0], r_wst[1] = Res(), Res()
        for k in range(8):
            rows = slice(k * 128, (k + 1) * 128)
            wload(wO0[:, k, :], w_out[0, rows, :], D, None, r_wC)
            wload(wO1[:, k, :], w_out[1, rows, :], D, None, r_wC)
            g1c = gc[:, 8 + k: 9 + k]
            wload(wB[:, k, 0:768], w_in_b[rows, 0:768], 768, g1c, r_wC)
            for gk in range(2):
                for dup in range(2):
                    c0 = 768 + gk * 128 + dup * 64
                    wload(wB[:, k, c0:c0 + 64], w_in_b[rows, 768 + gk * 64: 768 + (gk + 1) * 64], 64, g1c, r_wC)
            wload(wB[:, k, 1024:2432], w_in_b[rows, 896:2304], 1408, g1c, r_wC)
        T.barrier()
        stW.close()
        swac = sb("swac", [128, 2, H, 128], F32, stC)
        fgt = sb("fgt", [128, D], F32, stC)
        esink = sb("esink", [128, H], F32, stC)
        halob = sb("halob", [128, NU], F32, stC)
        zero1 = sb("zero1", [128, 1], F32, stC)
        T.dma("sp", swac[:].rearrange("p a b c -> p (a b c)"), swac_d, w=[r_const])
        T.dma("sp", fgt[:], fgr, w=[r_const])
        T.dma("sp", halob[:], halob_d, w=[r_const])
        T.dma("sp", esink[:], sinkr, w=[r_const])
        T.op("act", lambda e: e.activation(out=esink[:], in_=esink[:], func=AF.Exp), r=[], w=[r_const])
        T.op("dve", lambda e: e.memset(zero1[:], 0.0), w=[r_const])

        yt = [sb("yt%d" % i, [128, D], BF16, stC) for i in range(2)]
        r_yt = [Res(), Res()]
        gtt = [sb("gtt0", [128, D], F32, stC)] * 2
        r_gtt = [Res()] * 2
        x1g = [sb("x1g%d" % i, [128, 3, D], F32, stC) for i in range(2)]
        r_x1g = [[Res() for _ in range(3)] for _ in range(2)]
        yg = [sb("yg%d" % i, [128, D], BF16, stC) for i in range(2)]
        r_yg = [Res(), Res()]
        ygT = [sb("ygT%d" % i, [128, 8, 128], BF16, stC) for i in range(2)]
        r_ygT = [Res(), Res()]
        ssc = [sb("ssc%d" % i, [128, 4], F32, stC) for i in range(4)]
        r_ssc = [Res() for _ in range(4)]
        xs1 = [sb("xs1_%d" % i, [128, D], BF16, stC) for i in range(2)]
        r_xs1 = [Res(), Res()]
        h1T = [sb("h1T%d" % i, [128, 8, 384], BF16, stC) for i in range(2)]
        r_h1T = [Res(), Res()]
        q1T = [sb("q1T%d" % i, [128, 6, 384], BF16, stC) for i in range(2)]
        r_q1T = [Res(), Res()]
        qm1T = [sb("qm1T%d" % i, [128, 2, 384], BF16, stC) for i in range(2)]
        r_qm1T = [Res(), Res()]
        k1T = [sb("k1T%d" % i, [128, 2, 9 * 128], BF16, stC) for i in range(2)]
        v1 = [sb("v1_%d" % i, [128, 9, 2, 65], BF16, stC) for i in range(2)]
        r_kv1 = [Res(), Res()]
        for i in range(2):
            T.op("dve", lambda e: e.memset(v1[i][:].rearrange("p a b c -> p (a b c)"), 1.0), w=[r_kv1[i]])
        y1 = [sb("y1_0", [128, 3, D], BF16, stC)] * 2
        r_y1 = [Res()] * 2
        sbs = [sb("sbs%d" % i, [128, 6, 128], F32, stC) for i in range(2)]
        r_sbs = [Res(), Res()]
        p1T = [sb("p1T%d" % i, [128, 6, 128], BF16, stC) for i in range(2)]
        r_p1T = [Res(), Res()]
        den = sb("den", [128, 6], F32, stC)
        r_den = Res()
        es1 = [sb("es1_0", [128, D], F32, stC)] * 2
        r_es1 = [Res()] * 2
        ot = [sb("ot0", [128, D], F32, stC)] * 2
        r_ot = [Res()] * 2
        pT = [sb("pTc%d" % i, [128, 2, 384], BF16, stC) for i in range(2)]
        r_pT = [Res() for _ in range(2)]
        yg2 = sb("yg2", [128, D], BF16, stC)
        r_yg2 = Res()
        ygT2 = sb("ygT2", [128, 8, 128], BF16, stC)
        r_ygT2 = Res()
        rec = sb("recc", [128, 9], F32, stC)
        r_rec = Res()
        r_out = Res()
        cc = {"t": 0, "n": 0, "pj": 0, "sw": 0, "z": 0, "o": 0}
        TBK, ABK = 0, 1

        def rms_scale(src, r_src, dst_bf, r_dst):
            s_ = cc["n"] % 4
            cc["n"] += 1
            T.op("act", lambda e: e.activation(out=junk[:], in_=src, func=AF.Square, accum_out=ssc[s_][:, 0:1]),
                 r=[r_src], w=[r_ssc[s_]])
            T.op("act", lambda e: e.activation(out=ssc[s_][:, 1:2], in_=ssc[s_][:, 0:1], func=AF.Ln,
                                               bias=epst[:, 0:1], scale=1.0 / D), r=[r_const], w=[r_ssc[s_]])
            T.op("act", lambda e: e.activation(out=ssc[s_][:, 2:3], in_=ssc[s_][:, 1:2], func=AF.Exp, scale=-0.5),
                 r=[], w=[r_ssc[s_]])
            if dst_bf is not None:
                T.op("dve", lambda e: e.tensor_scalar(out=dst_bf, in0=src, scalar1=ssc[s_][:, 2:3], scalar2=None,
                                                      op0=ALU.mult), r=[r_src, r_ssc[s_]], w=[r_dst])
            return s_

        def transpose8(src_bf, r_src, dst, r_dstT):
            pv = pbf(TBK)
            for k in range(8):
                T.op("pe", lambda e: e.transpose(pv[:, k * 128:(k + 1) * 128], src_bf[:, k * 128:(k + 1) * 128], ident[:]),
                     r=[r_src, r_const], w=[bank[TBK]])
            evac(dst, pv.rearrange("p (k t) -> p k t", k=8), r=[], w=[bank[TBK], r_dstT])

        def outproj(srcT, r_srcT, w_, xres, r_xres):
            for half in range(2):
                for k in range(8):
                    T.op("pe", lambda e: e.matmul(pf(2 + half), lhsT=srcT[:, k, :], rhs=w_[:, k, half * 512:(half + 1) * 512],
                                                  start=(k == 0), stop=(k == 7)),
                         r=[r_srcT, r_wC], w=[bank[2 + half]])
            T.op("dve", lambda e: e.tensor_tensor(out=xres, in0=pbig[:, 1024:2048], in1=xres, op=ALU.add),
                 r=[], w=[bank[2], bank[3], r_xres])

        def featproj(hT_, r_hT_, c0, dst, r_dst, scale, N=384):
            bk = 4 + cc["pj"] % 2
            cc["pj"] += 1
            for k in range(8):
                T.op("pe", lambda e: e.matmul(pf(bk, N), lhsT=wB[:, k, c0:c0 + 128], rhs=hT_[:, k, 0:N],
                                              start=(k == 0), stop=(k == 7)),
                     r=[r_wC, r_hT_], w=[bank[bk]])
            evac(dst, pf(bk, N), r=[], w=[bank[bk], r_dst], scale=scale)

        groups = [(m, g) for m in units for g in range(3)]

        def stageP(m, g, gb):
            ub = m % 2
            u0 = 1 + 3 * g
            items = []

            def t_a(i):
                u = u0 + i
                lt = UT * m + u
                qi = 9 * m + u - 1
                ys = i % 2
                T.dma("sp", yt[ys][:], YS[:, qi * D:(qi + 1) * D], r=[r_YS], w=[r_yt[ys]])
                T.dma("sp", gtt[ys][:], GT[lt * 128:(lt + 1) * 128, :], r=[r_GT], w=[r_gtt[ys]])
                T.dma("sp", x1g[gb][:, i, :], xm[lt * 128:(lt + 1) * 128, :], w=[r_x1g[gb][i]])
                T.op("dve", lambda e: e.tensor_tensor(out=yg[ys][:], in0=yt[ys][:], in1=gtt[ys][:], op=ALU.mult),
                     r=[r_yt[ys], r_gtt[ys]], w=[r_yg[ys]])

            def t_b(i):
                ys = i % 2
                transpose8(yg[ys], r_yg[ys], ygT[ys][:], r_ygT[ys])
                outproj(ygT[ys], r_ygT[ys], wO0, x1g[gb][:, i, :], r_x1g[gb][i])

            def t_c(i):
                ys = i % 2
                rms_scale(x1g[gb][:, i, :], r_x1g[gb][i], xs1[ys][:], r_xs1[ys])
                transpose8(xs1[ys], r_xs1[ys], h1T[gb][:, :, i * 128:(i + 1) * 128], r_h1T[gb])

            def vproj(i):
                bk = 4 + cc["pj"] % 2
                cc["pj"] += 1
                for k in range(8):
                    T.op("pe", lambda e: e.matmul(pf(bk, 128), lhsT=h1T[gb][:, k, i * 128:(i + 1) * 128],
                                                  rhs=wB[:, k, 1024:1152], start=(k == 0), stop=(k == 7)),
                         r=[r_wC, r_h1T[gb]], w=[bank[bk]])
                evac(v1[ub][:, u0 - 1 + i, :, 0:64], pf(bk, 128).rearrange("p (a b) -> p a b", a=2),
                     r=[], w=[bank[bk], r_kv1[ub]])

            items.append(lambda: t_a(0))
            items.append(lambda: t_a(1))
            items.append(lambda: t_b(0))
            items.append(lambda: t_c(0))
            items.append(lambda: t_b(1))
            items.append(lambda: t_a(2))
            items.append(lambda: t_c(1))
            items.append(lambda: t_b(2))
            items.append(lambda: t_c(2))
            for gk in range(2):
                items.append(lambda gk=gk: featproj(h1T[gb], r_h1T[gb], 768 + gk * 128,
                                                    k1T[ub][:, gk, (u0 - 1) * 128:(u0 + 2) * 128], r_kv1[ub], None))
            for i in range(3):
                items.append(lambda i=i: vproj(i))
            for p in range(6):
                items.append(lambda p=p: featproj(h1T[gb], r_h1T[gb], p * 128, q1T[gb][:, p, :], r_q1T[gb], 0.125))
            for p in range(2):
                items.append(lambda p=p: featproj(h1T[gb], r_h1T[gb], 1152 + p * 128, qm1T[gb][:, p, :],
                                                  r_qm1T[gb], 0.125))
            return items

        def stageQ(m, g, gb):
            ub = m % 2
            u0 = 1 + 3 * g
            items = []

            def swa(i, e_):
                u = u0 + i
                rows = slice(e_ * 64, (e_ + 1) * 64)
                first = True
                for kt in range(2):
                    kcol = (u - 2 + kt) * 128
                    for gk in range(2):
                        T.op("pe", lambda e: e.matmul(pf(6 + gk, 384).rearrange("p (a b) -> p a b", a=3),
                                                      lhsT=k1T[ub][rows, gk, kcol:kcol + 128],
                                                      rhs=q1T[gb][rows, 3 * gk:3 * gk + 3, i * 128:(i + 1) * 128],
                                                      start=True, stop=True),
                             r=[r_kv1[ub], r_q1T[gb]], w=[bank[6 + gk]])
                    sw = cc["sw"] % 2
                    cc["sw"] += 1
                    for gk in range(2):
                        T.op("dve", lambda e: e.tensor_tensor(
                            out=sbs[sw][:, 3 * gk:3 * gk + 3, :],
                            in0=pf(6 + gk, 384).rearrange("p (a b) -> p a b", a=3),
                            in1=swac[:, kt, e_ + 6 * gk: e_ + 6 * gk + 5:2, :], op=ALU.add),
                            r=[r_const], w=[bank[6 + gk], r_sbs[sw]])
                    bias_ap = halob[:, m:m + 1] if (kt == 0 and u == 2) else zero1[:, 0:1]
                    T.op("act", lambda e: e.activation(out=p1T[sw][:], in_=sbs[sw][:], func=AF.Exp,
                                                       bias=bias_ap, scale=1.0),
                         r=[r_sbs[sw], r_const], w=[r_p1T[sw]])
                    for hh in range(6):
                        gk = hh // 3
                        fl = first
                        first = False
                        T.op("pe", lambda e: e.matmul(pf(ABK, 65, hh * 65), lhsT=p1T[sw][:, hh, :],
                                                      rhs=v1[ub][:, u - 2 + kt, gk, :], start=fl, stop=True,
                                                      skip_group_check=True),
                             r=[r_p1T[sw], r_kv1[ub]], w=[bank[ABK]])
                av = pf(ABK, 390).rearrange("p (a b) -> p a b", b=65)
                T.op("dve", lambda e: e.tensor_tensor(out=den[:], in0=av[:, :, 64], in1=esink[:, e_::2], op=ALU.add),
                     r=[r_const], w=[bank[ABK], r_den])
                T.op("dve", lambda e: e.reciprocal(out=den[:], in_=den[:]), r=[], w=[r_den])
                T.op("dve", lambda e: e.tensor_tensor(
                    out=y1[gb][:, i, 0:768].rearrange("p (h c) -> p h c", c=64)[:, e_::2, :],
                    in0=av[:, :, 0:64], in1=den[:].unsqueeze(2).to_broadcast([128, 6, 64]), op=ALU.mult),
                    r=[r_den], w=[bank[ABK], r_y1[gb]])

            def memh(hm):
                mem_attn(1, qm1T[gb], r_qm1T[gb], 0,
                         lambda hm_: y1[gb][:, 0:3, 768 + hm_ * 64: 768 + (hm_ + 1) * 64], r_y1[gb], 3,
                         sbanks=(6,), abanks=(ABK,), heads_=[hm])

            def t_z1(i):
                for half in range(2):
                    for k in range(8):
                        T.op("pe", lambda e: e.matmul(pf(2 + half), lhsT=h1T[gb][:, k, i * 128:(i + 1) * 128],
                                                      rhs=wB[:, k, 1408 + half * 512: 1408 + (half + 1) * 512],
                                                      start=(k == 0), stop=(k == 7)),
                             r=[r_wC, r_h1T[gb]], w=[bank[2 + half]])
                zs = 0
                zps = pbig[:, 1024:2048]
                T.op("act", lambda e: e.activation(out=es1[zs][:], in_=zps, func=AF.Exp, scale=-1.0),
                     r=[], w=[bank[2], bank[3], r_es1[zs]])
                T.op("act", lambda e: e.copy(out=ot[zs][:], in_=zps), r=[], w=[bank[2], bank[3], r_ot[zs]])
                T.op("act", lambda e: e.activation(out=es1[zs][:], in_=es1[zs][:], func=AF.Ln, bias=1.0),
                     r=[], w=[r_es1[zs]])
                T.op("act", lambda e: e.activation(out=es1[zs][:], in_=es1[zs][:], func=AF.Exp, scale=-1.0),
                     r=[], w=[r_es1[zs]])
                T.op("dve", lambda e: e.tensor_tensor(out=es1[zs][:], in0=ot[zs][:], in1=es1[zs][:], op=ALU.mult),
                     r=[r_ot[zs]], w=[r_es1[zs]])
                T.op("dve", lambda e: e.tensor_tensor(out=yg2[:], in0=y1[gb][:, i, :], in1=es1[zs][:], op=ALU.mult),
                     r=[r_y1[gb], r_es1[zs]], w=[r_yg2])

            def t_z2(i):
                transpose8(yg2, r_yg2, ygT2[:], r_ygT2)

            def t_o(i):
                u = u0 + i
                outproj(ygT2, r_ygT2, wO1, x1g[gb][:, i, :], r_x1g[gb][i])
                s_ = rms_scale(x1g[gb][:, i, :], r_x1g[gb][i], None, None)
                T.op("dve", lambda e: e.scalar_tensor_tensor(out=ot[0][:], in0=x1g[gb][:, i, :], scalar=ssc[s_][:, 2:3],
                                                             in1=fgt[:], op0=ALU.mult, op1=ALU.mult),
                     r=[r_x1g[gb][i], r_ssc[s_], r_const], w=[r_ot[0]])
                orow = (m * 8 + u - 2) * 128
                T.dma("act", out_d[orow:orow + 128, :], ot[0][:], r=[r_ot[0]], w=[r_out])

            tiles = [i for i in range(3) if u0 + i >= 2]
            for i in tiles:
                items.append(lambda i=i: swa(i, 0))
                items.append(lambda i=i: swa(i, 1))
            for hm in range(4):
                items.append(lambda hm=hm: memh(hm))
            for i in tiles:
                items.append(lambda i=i: t_z1(i))
                items.append(lambda i=i: t_z2(i))
                items.append(lambda i=i: t_o(i))
            return items

        for gi_ in range(len(groups) + 1):
            main = stageP(groups[gi_][0], groups[gi_][1], gi_ % 2) if gi_ < len(groups) else []
            side = stageQ(groups[gi_ - 1][0], groups[gi_ - 1][1], (gi_ - 1) % 2) if gi_ >= 1 else []
            if main:
                run_interleaved(main, side)
            else:
                for f in side:
                    f()
        T.barrier()
    return nc


def host_consts(q):
    sl = _slopes()
    c = {}
    kx = np.zeros((64, S), dtype=np.float32)
    kx[0:3, :] = 1.0
    kt = (np.arange(S) // 128).astype(np.float32)
    kx[3:6, :] = kt[None, :]
    blk = np.arange(S) // 256
    kx[6 + (blk % 32), np.arange(S)] = 1.0
    c["kx"] = kx.astype(NPBF)
    tpos = np.zeros(NLOC, dtype=np.int64)
    for m in range(NU):
        base = (16 * m + 4 * q) * 256 - 256
        tpos[m * UT * 128:(m + 1) * UT * 128] = base + np.arange(UT * 128)
    kxo = np.zeros((64, NLOC), dtype=np.float32)
    kxo[0:3, :] = 1.0
    kto = np.floor_divide(tpos, 128).astype(np.float32)
    kxo[3:6, :] = kto[None, :]
    c["kxo"] = kxo.astype(NPBF)
    qac = np.zeros((H, 6, NLOC), dtype=NPBF)
    for h in range(H):
        cc = (-sl[h] * tpos.astype(np.float32)).astype(np.float32)
        hi, mid, lo = _split3(cc)
        qac[h, 0], qac[h, 1], qac[h, 2] = hi, mid, lo
        s1, s2, s3 = _split3(np.float32(sl[h]))
        qac[h, 3] = (np.float32(128.0) * s1.astype(np.float32)).astype(NPBF)
        qac[h, 4] = (np.float32(128.0) * s2.astype(np.float32)).astype(NPBF)
        qac[h, 5] = (np.float32(128.0) * s3.astype(np.float32)).astype(NPBF)
    c["qac"] = qac
    c["abias"] = (np.arange(128, dtype=np.float32)[:, None] * sl[None, :]).astype(np.float32)
    bm = np.zeros((NLT, NBLK), dtype=np.float32)
    for lt in range(NLT):
        m, u = divmod(lt, UT)
        i_own = 16 * m + 4 * q - 1 + u // 2
        bm[lt, :] = np.where(np.arange(NBLK) < i_own, 0.0, -1e30)
    c["bmask"] = np.ascontiguousarray(np.broadcast_to(bm.reshape(1, -1), (128, NLT * NBLK))).astype(np.float32)
    p = np.arange(128)[:, None, None]
    k2 = np.arange(2)[None, :, None]
    t = np.arange(256)[None, None, :]
    cm = np.where(t >= 128 * k2 + p, 0.0, -BIG).astype(np.float32)
    c["cm"] = cm.reshape(128, 512).astype(NPBF)
    c["ident"] = np.eye(128, dtype=np.float32).astype(NPBF)
    pk = np.arange(128, dtype=np.float32)[:, None, None]
    tt = np.arange(128, dtype=np.float32)[None, None, :]
    slh = sl[None, :, None]
    d_cur = tt - pk
    cur = np.where(d_cur >= 0, -slh * d_cur, NEG)
    d_prev = 128.0 + tt - pk
    prev = np.where(d_prev < 128, -slh * d_prev, NEG)
    c["swac"] = np.stack([prev, cur], axis=1).astype(np.float32).reshape(128, 2 * H * 128)
    hb = np.zeros((128, NU), dtype=np.float32)
    if q == 0:
        hb[:, 0] = NEG
    c["halob"] = hb
    return c


def make_in_maps(x, mem, norm_g, w_in_a, w_in_b, sinks_b, w_mem_kv, w_out, mem_norm_g, final_norm_g, cores):
    x = np.asarray(x, dtype=np.float32)
    in_maps = []
    gcols = np.concatenate([np.asarray(norm_g[0], np.float32).reshape(8, 128).T,
                            np.asarray(norm_g[1], np.float32).reshape(8, 128).T,
                            np.asarray(mem_norm_g, np.float32).reshape(8, 128).T], axis=1)
    fgr = np.ascontiguousarray(np.broadcast_to(np.asarray(final_norm_g, np.float32)[None, :], (128, D)))
    sinkr = np.ascontiguousarray(np.broadcast_to(np.asarray(sinks_b, np.float32).reshape(1, H), (128, H)))
    cc = {q: host_consts(q) for q in range(4)}
    for c in cores:
        b, q = divmod(c, 4)
        xmine = np.zeros((NLOC, D), dtype=np.float32)
        for m in range(NU):
            t0 = (16 * m + 4 * q) * 256 - 256
            lo = max(t0, 0)
            xmine[m * 1280 + (lo - t0):(m + 1) * 1280] = x[b, lo:t0 + 1280]
        d = {"xg": np.ascontiguousarray(x[b]), "xm": xmine, "memb": np.ascontiguousarray(np.asarray(mem, np.float32)[b]),
             "w_in_a": np.ascontiguousarray(np.asarray(w_in_a, np.float32)[0]),
             "w_in_b": np.ascontiguousarray(np.asarray(w_in_b, np.float32)[0]),
             "w_mem": np.ascontiguousarray(np.asarray(w_mem_kv, np.float32)),
             "w_out": np.ascontiguousarray(np.asarray(w_out, np.float32)),
             "gcols": np.ascontiguousarray(gcols), "fgr": fgr, "sinkr": sinkr}
        d.update(cc[q])
        in_maps.append(d)
    return in_maps


def kernel(x, mem, norm_g, w_in_a, w_in_b, sinks_b, w_mem_kv, w_out, mem_norm_g, final_norm_g):
    cores = list(range(8))
    in_maps = make_in_maps(x, mem, norm_g, w_in_a, w_in_b, sinks_b, w_mem_kv, w_out, mem_norm_g, final_norm_g, cores)
    nc = build()
    res = run_bass_kernel_spmd(nc, in_maps, core_ids=cores)
    out = np.zeros((2, S, D), dtype=np.float32)
    for c in cores:
        b, q = divmod(c, 4)
        o = res.results[c]["out"]
        for m in range(NU):
            t0 = (16 * m + 4 * q) * 256
            out[b, t0:t0 + 1024] = o[m * 1024:(m + 1) * 1024]
    return out
```

```python
import contextlib
import math
import numpy as np
import ml_dtypes
import concourse.bass as bass
import concourse.mybir as mybir
from concourse.bass_utils import run_bass_kernel_spmd

F32 = mybir.dt.float32
BF16 = mybir.dt.bfloat16
AF = mybir.ActivationFunctionType
ALU = mybir.AluOpType
AX = mybir.AxisListType
NPBF = ml_dtypes.bfloat16

D = 1024
S = 16384
H = 12
DH = 64
NBLK = 64
NTG = 128
UT = 10
NU = 4
NLT = NU * UT
NLOC = NLT * 128
NQT = 36
WA = 3584
EPS = 1e-6
BIG = 32768.0
NEG = -30000.0
_DBG = {}


def _slopes():
    def p2(n):
        st = 2.0 ** (-8.0 / n)
        return [st ** (i + 1) for i in range(n)]
    c = 2 ** math.floor(math.log2(H))
    vals = p2(c) + p2(2 * c)[0::2][: H - c]
    return np.array(vals, dtype=np.float32)


def _split3(x):
    x = np.asarray(x, dtype=np.float32)
    hi = x.astype(NPBF)
    r1 = (x - hi.astype(np.float32)).astype(np.float32)
    mid = r1.astype(NPBF)
    r2 = (r1 - mid.astype(np.float32)).astype(np.float32)
    lo = r2.astype(NPBF)
    return hi, mid, lo


class Res:
    __slots__ = ("w", "r", "name")

    def __init__(self, name=""):
        self.w = None
        self.r = {}
        self.name = name


class Trk:
    def __init__(self, nc, es):
        self.nc = nc
        self.eng = {"pe": nc.tensor, "act": nc.scalar, "dve": nc.vector,
                    "pool": nc.gpsimd, "sp": nc.sync}
        self.sem = {}
        self.cnt = {}
        for e in ("pe", "act", "dve", "pool"):
            self.sem[e] = es.enter_context(nc.semaphore("c_" + e))
            self.cnt[e] = 0
        self.seen = {e: {} for e in self.eng}
        self.dsem = {}
        self.dcnt = {}
        self.dnext = {}
        for q, n in (("sp", 16), ("act", 4), ("pool", 16)):
            self.dsem[q] = [es.enter_context(nc.semaphore("d_%s%d" % (q, i))) for i in range(n)]
            self.dcnt[q] = [0] * n
            self.dnext[q] = 0
        self.ninstr = 0

    def _wait(self, eng, ev):
        sem, val, key = ev
        if self.seen[eng].get(key, 0) >= val:
            return
        self.eng[eng].wait_ge(sem, val)
        self.seen[eng][key] = val

    def _deps(self, eng, r, w):
        evs = {}

        def add(ev):
            if ev is None:
                return
            k = ev[2]
            if k not in evs or evs[k][1] < ev[1]:
                evs[k] = ev
        for x in r:
            add(x.w)
        for x in w:
            add(x.w)
            for ev in x.r.values():
                add(ev)
        for ev in evs.values():
            if eng == "pe" and ev[2] == "c_pe":
                continue
            self._wait(eng, ev)

    def _upd(self, ev, r, w):
        k = ev[2]
        for x in r:
            x.r[k] = ev
        for x in w:
            x.w = ev
            x.r = {}

    def op(self, eng, fn, r=(), w=()):
        self._deps(eng, r, w)
        ins = fn(self.eng[eng])
        self.cnt[eng] += 1
        ins.then_inc(self.sem[eng], 1)
        ev = (self.sem[eng], self.cnt[eng], "c_" + eng)
        self._upd(ev, r, w)
        self.ninstr += 1
        return ev

    def dma(self, q, out, in_, r=(), w=()):
        self._deps(q, r, w)
        i = self.dnext[q]
        self.dnext[q] = (i + 1) % len(self.dsem[q])
        sem = self.dsem[q][i]
        key = "d_%s%d" % (q, i)
        if self.dcnt[q][i] > 0:
            self._wait(q, (sem, self.dcnt[q][i], key))
        self.eng[q].dma_start(out=out, in_=in_).then_inc(sem, 16)
        self.dcnt[q][i] += 16
        ev = (sem, self.dcnt[q][i], key)
        self._upd(ev, r, w)
        self.ninstr += 1
        return ev

    def barrier(self):
        evs = []
        for e in self.sem:
            if self.cnt[e] > 0:
                evs.append((self.sem[e], self.cnt[e], "c_" + e))
        for q in self.dsem:
            for i, sem in enumerate(self.dsem[q]):
                if self.dcnt[q][i] > 0:
                    evs.append((sem, self.dcnt[q][i], "d_%s%d" % (q, i)))
        for e in self.eng:
            for ev in evs:
                self._wait(e, ev)


def build(stop_after="all", debug=False):
    nc = bass.Bass("TRN2", target_bir_lowering=False)
    es = contextlib.ExitStack()

    def din(name, shape, dt=F32):
        return nc.dram_tensor(name, list(shape), dt, kind="ExternalInput").ap()

    dbg = set(debug) if debug else set()

    def dscr(name, shape, dt):
        kind = "ExternalOutput" if name in dbg else "Internal"
        return nc.dram_tensor(name, list(shape), dt, kind=kind).ap()

    xg = din("xg", [S, D])
    xm = din("xm", [NLOC, D])
    memb = din("memb", [256, D])
    w_in_a = din("w_in_a", [D, WA])
    w_in_b = din("w_in_b", [D, 2304])
    w_mem = din("w_mem", [2, D, 512])
    w_out = din("w_out", [2, D, D])
    gcols = din("gcols", [128, 24])
    fgr = din("fgr", [128, D])
    sinkr = din("sinkr", [128, H])
    kx = din("kx", [64, S], BF16)
    kxo = din("kxo", [64, NLOC], BF16)
    qac = din("qac", [H, 6, NLOC], BF16)
    abias_d = din("abias", [128, H])
    bmask_d = din("bmask", [128, NLT * NBLK])
    cm_d = din("cm", [128, 512], BF16)
    ident_d = din("ident", [128, 128], BF16)
    swac_d = din("swac", [128, 2 * H * 128])
    halob_d = din("halob", [128, NU])
    out_d = nc.dram_tensor("out", [NU * 1024, D], F32, kind="ExternalOutput").ap()

    KT = dscr("KT", [H, 128, S], BF16)
    VP = dscr("VP", [H, 128, NTG, 65], BF16)
    KTo = dscr("KTo", [H, 128, NLOC], BF16)
    VPo = dscr("VPo", [H, 128, NLT, 65], BF16)
    QA = dscr("QA", [H, 2, 128, NLOC], BF16)
    QM = dscr("QM", [2, 128, NLOC], BF16)
    GT = dscr("GT", [NLOC, D], F32)
    YS = dscr("YS", [128, NQT * D], BF16)
    KMD = dscr("KMD", [128, 6 * NBLK], BF16) if "KMD" in dbg else None
    YD = dscr("YD", [128, NQT * D], BF16) if "YD" in dbg else None

    with es:
        T = Trk(nc, es)

        def sb(name, shape, dt, st=None):
            return (st or es).enter_context(nc.sbuf_tensor("s_" + name, list(shape), dt))

        pbig = es.enter_context(nc.psum_tensor("pbig", [128, 4096], F32))
        bank = [Res("bank%d" % i) for i in range(8)]

        def pf(b, n=512, off=0):
            return pbig[:, b * 512 + off: b * 512 + off + n]

        def pbf(b):
            return pbig[:, b * 512:(b + 1) * 512].bitcast(BF16)

        ident = sb("ident", [128, 128], BF16)
        abias = sb("abias", [128, H], F32)
        gc = sb("gc", [128, 24], F32)
        epst = sb("epst", [128, 1], F32)
        junk = sb("junk", [128, 1024], BF16)
        r_const = Res("const")
        T.dma("sp", ident[:], ident_d, w=[r_const])
        T.dma("sp", abias[:], abias_d, w=[r_const])
        T.dma("sp", gc[:], gcols, w=[r_const])
        T.op("dve", lambda e: e.memset(epst[:], EPS), w=[r_const])

        r_KT = [Res("KT%d" % h) for h in range(H)]
        r_KTo = [Res("KTo%d" % h) for h in range(H)]
        r_QA = [Res("QA%d" % h) for h in range(H)]
        for h in range(H):
            eb = 64 if h % 2 == 0 else 0
            T.dma("sp", KT[h, eb:eb + 64, :], kx, w=[r_KT[h]])
            T.dma("sp", KTo[h, eb:eb + 64, :], kxo, w=[r_KTo[h]])
            for v in range(2):
                T.dma("sp", QA[h, v, eb:eb + 6, :], qac[h], w=[r_QA[h]])

        kmem = sb("kmem", [128, 2, 2, 256], BF16)
        vmem = sb("vmem", [128, 2, 2, 4, 65], BF16)
        kmT = sb("kmT", [128, 6, NBLK], BF16)

        stA = contextlib.ExitStack()
        es.enter_context(stA)
        NXS = 3
        xt = [sb("xt%d" % i, [128, D], F32, stA) for i in range(NXS)]
        r_xt = [Res() for _ in range(NXS)]
        xs = [sb("xs%d" % i, [128, D], BF16, stA) for i in range(2)]
        r_xs = [Res() for _ in range(2)]
        ssq = [sb("ssq%d" % i, [128, 4], F32, stA) for i in range(NXS)]
        r_ss = [Res() for _ in range(NXS)]
        hT = [sb("hT%d" % i, [128, 8, 512], BF16, stA) for i in range(2)]
        r_hT = [Res() for _ in range(2)]
        st = {"x": 0, "xs": 0, "tb": 0, "ev": 0}
        TB = (0, 1)

        def evac(out, in_, r, w, scale=None):
            st["ev"] += 1
            if st["ev"] % 2 == 0:
                if scale is None:
                    T.op("act", lambda e: e.copy(out=out, in_=in_), r=r, w=w)
                else:
                    T.op("act", lambda e: e.mul(out=out, in_=in_, mul=scale), r=r, w=w)
            else:
                if scale is None:
                    T.op("dve", lambda e: e.tensor_copy(out=out, in_=in_), r=r, w=w)
                else:
                    T.op("dve", lambda e: e.tensor_scalar(out=out, in0=in_, scalar1=scale, scalar2=None,
                                                          op0=ALU.mult), r=r, w=w)

        def norm_T(src, row0, ntiles, hslot):
            for i in range(ntiles):
                s = st["x"] % NXS
                st["x"] += 1
                T.dma("sp", xt[s][:], src[row0 + i * 128: row0 + (i + 1) * 128, :], w=[r_xt[s]])
                T.op("act", lambda e: e.activation(out=junk[:], in_=xt[s][:], func=AF.Square,
                                                   accum_out=ssq[s][:, 0:1]), r=[r_xt[s]], w=[r_ss[s]])
                T.op("act", lambda e: e.activation(out=ssq[s][:, 1:2], in_=ssq[s][:, 0:1], func=AF.Ln,
                                                   bias=epst[:, 0:1], scale=1.0 / D),
                     r=[r_const], w=[r_ss[s]])
                T.op("act", lambda e: e.activation(out=ssq[s][:, 2:3], in_=ssq[s][:, 1:2], func=AF.Exp, scale=-0.5),
                     r=[], w=[r_ss[s]])
                b = st["xs"] % 2
                st["xs"] += 1
                T.op("dve", lambda e: e.tensor_scalar(out=xs[b][:], in0=xt[s][:], scalar1=ssq[s][:, 2:3],
                                                      scalar2=None, op0=ALU.mult),
                     r=[r_xt[s], r_ss[s]], w=[r_xs[b]])
                tb = TB[st["tb"] % 2]
                st["tb"] += 1
                pv = pbf(tb)
                for k in range(8):
                    T.op("pe", lambda e: e.transpose(pv[:, k * 128:(k + 1) * 128], xs[b][:, k * 128:(k + 1) * 128],
                                                     ident[:]),
                         r=[r_xs[b], r_const], w=[bank[tb]])
                evac(hT[hslot][:, :, i * 128:(i + 1) * 128],
                     pv.rearrange("p (k t) -> p k t", k=8), r=[], w=[bank[tb], r_hT[hslot]])

        def run_interleaved(main, side):
            n, m_ = len(main), len(side)
            j = 0
            for i_, f in enumerate(main):
                f()
                while j < m_ and (j + 1) * n <= (i_ + 1) * m_:
                    side[j]()
                    j += 1
            while j < m_:
                side[j]()
                j += 1

        def norm_items(src, row0, ntiles, hslot, tbs):
            slots = []
            for i in range(ntiles):
                slots.append((st["x"] % NXS, st["xs"] % 2, tbs[st["tb"] % len(tbs)]))
                st["x"] += 1
                st["xs"] += 1
                st["tb"] += 1

            def p0(i):
                s = slots[i][0]
                T.dma("sp", xt[s][:], src[row0 + i * 128: row0 + (i + 1) * 128, :], w=[r_xt[s]])

            def p1(i):
                s, b, tb = slots[i]
                T.op("act", lambda e: e.activation(out=junk[:], in_=xt[s][:], func=AF.Square,
                                                   accum_out=ssq[s][:, 0:1]), r=[r_xt[s]], w=[r_ss[s]])
                T.op("act", lambda e: e.activation(out=ssq[s][:, 1:2], in_=ssq[s][:, 0:1], func=AF.Ln,
                                                   bias=epst[:, 0:1], scale=1.0 / D), r=[r_const], w=[r_ss[s]])
                T.op("act", lambda e: e.activation(out=ssq[s][:, 2:3], in_=ssq[s][:, 1:2], func=AF.Exp, scale=-0.5),
                     r=[], w=[r_ss[s]])
                T.op("dve", lambda e: e.tensor_scalar(out=xs[b][:], in0=xt[s][:], scalar1=ssq[s][:, 2:3],
                                                      scalar2=None, op0=ALU.mult),
                     r=[r_xt[s], r_ss[s]], w=[r_xs[b]])

            def p2(i):
                s, b, tb = slots[i]
                pv = pbf(tb)
                for k in range(8):
                    T.op("pe", lambda e: e.transpose(pv[:, k * 128:(k + 1) * 128], xs[b][:, k * 128:(k + 1) * 128],
                                                     ident[:]), r=[r_xs[b], r_const], w=[bank[tb]])
                evac(hT[hslot][:, :, i * 128:(i + 1) * 128],
                     pv.rearrange("p (k t) -> p k t", k=8), r=[], w=[bank[tb], r_hT[hslot]])

            items = []
            for k_ in range(-1, ntiles + 1):
                def f(k_=k_):
                    if 0 <= k_ + 1 < ntiles:
                        p0(k_ + 1)
                    if 0 <= k_ - 1 < ntiles:
                        p2(k_ - 1)
                    if 0 <= k_ < ntiles:
                        p1(k_)
                items.append(f)
            return items

        wA = sb("wA", [128, 8, WA], BF16, stA)
        r_wA = Res()
        r_wM = Res()
        r_kmT = Res()
        kms = sb("kms", [128, 2], F32, stA)
        r_kms = Res()
        kst = [sb("kst%d" % i, [128, 512], BF16, stA) for i in range(3)]
        r_kst = [Res() for _ in range(3)]
        vst = [sb("vst%d" % i, [128, H, 4, 65], BF16, stA) for i in range(2)]
        r_vst = [Res() for _ in range(2)]
        bmask = sb("bmask", [128, NLT, NBLK], F32, stA)
        Et = [sb("Et%d" % i, [128, H, 2, 128], BF16, stA) for i in range(2)]
        r_Et = [Res(), Res()]
        gm = sb("gm", [128, 6, NBLK], F32, stA)
        r_gm = Res()
        m8 = sb("m8", [128, 6, 8], F32, stA)
        r_m8 = [Res() for _ in range(6)]
        thr = sb("thr", [128, 6], F32, stA)
        selb = sb("selb", [128, 6, NBLK], F32, stA)
        qmst = sb("qmst", [128, 2, 512], BF16, stA)
        r_qmst = Res()
        esb = [sb("esb%d" % i, [128, D], F32, stA) for i in range(2)]
        r_esb = [Res(), Res()]
        gts = [sb("gts%d" % i, [128, D], F32, stA) for i in range(2)]
        r_gts = [Res(), Res()]
        qa_sts = [sb("qa_st0", [128, H, 2, 512], BF16, stA), None]
        r_qasts = [Res(), Res()]
        stW0 = contextlib.ExitStack()
        stA.enter_context(stW0)
        wM = sb("wM", [128, 2, 8, 512], BF16, stW0)
        wst = [sb("wst%d" % i, [128, 1792], F32, stW0) for i in range(2)]
        r_wst = [Res(), Res()]
        stw = {"i": 0}

        def wload(dst, src, ncols, gcol, rdst):
            for c0 in range(0, ncols, 1792):
                n = min(1792, ncols - c0)
                s = stw["i"] % 2
                stw["i"] += 1
                T.dma("sp", wst[s][:, 0:n], src[:, c0:c0 + n], w=[r_wst[s]])
                if s == 0:
                    if gcol is None:
                        T.op("dve", lambda e: e.tensor_copy(out=dst[:, c0:c0 + n], in_=wst[s][:, 0:n]),
                             r=[r_wst[s]], w=[rdst])
                    else:
                        T.op("dve", lambda e: e.tensor_scalar(out=dst[:, c0:c0 + n], in0=wst[s][:, 0:n],
                                                              scalar1=gcol, scalar2=None, op0=ALU.mult),
                             r=[r_wst[s], r_const], w=[rdst])
                else:
                    if gcol is None:
                        T.op("act", lambda e: e.copy(out=dst[:, c0:c0 + n], in_=wst[s][:, 0:n]),
                             r=[r_wst[s]], w=[rdst])
                    else:
                        T.op("act", lambda e: e.activation(out=dst[:, c0:c0 + n], in_=wst[s][:, 0:n], func=AF.Copy,
                                                           scale=gcol),
                             r=[r_wst[s], r_const], w=[rdst])

        for L in range(2):
            for k in range(8):
                wload(wM[:, L, k, :], w_mem[L, k * 128:(k + 1) * 128, :], 512, gc[:, 16 + k:17 + k], r_wM)
        for k in range(8):
            wload(wA[:, k, :], w_in_a[k * 128:(k + 1) * 128, :], WA, gc[:, k:k + 1], r_wA)
        T.dma("sp", bmask[:].rearrange("p a b -> p (a b)"), bmask_d, w=[r_const])
        for i in range(2):
            T.op("dve", lambda e: e.memset(vst[i][:].rearrange("p a b c -> p (a b c)"), 1.0), w=[r_vst[i]])
            T.op("dve", lambda e: e.memset(Et[i][:].rearrange("p a b c -> p (a b c)"), 0.0), w=[r_Et[i]])

        r_kmem = Res()
        r_vmem = Res()
        T.op("dve", lambda e: e.memset(vmem[:].rearrange("p a b c d -> p (a b c d)"), 1.0), w=[r_vmem])
        norm_T(memb, 0, 2, 0)
        for L in range(2):
            for pr in range(2):
                bk = 2 + (L * 2 + pr) % 2
                for k in range(8):
                    T.op("pe", lambda e: e.matmul(pf(bk, 256), lhsT=wM[:, L, k, pr * 128:(pr + 1) * 128],
                                                  rhs=hT[0][:, k, 0:256], start=(k == 0), stop=(k == 7)),
                         r=[r_wM, r_hT[0]], w=[bank[bk]])
                evac(kmem[:, L, pr, :], pf(bk, 256), r=[], w=[bank[bk], r_kmem])
            for mt in range(2):
                bk = 4 + mt
                for k in range(8):
                    T.op("pe", lambda e: e.matmul(pf(bk, 256), lhsT=hT[0][:, k, mt * 128:(mt + 1) * 128],
                                                  rhs=wM[:, L, k, 256:512], start=(k == 0), stop=(k == 7)),
                         r=[r_wM, r_hT[0]], w=[bank[bk]])
                evac(vmem[:, L, mt, :, 0:64], pf(bk, 256).rearrange("p (h c) -> p h c", h=4),
                     r=[], w=[bank[bk], r_vmem])
        T.barrier()
        stW0.close()
        qa_sts[1] = sb("qa_st1", [128, H, 2, 512], BF16, stA)

        sk = {"k": 0, "pb": 0, "PB": (2, 3, 4, 5, 6, 7)}

        def nextbank():
            PB = sk["PB"]
            b = PB[sk["pb"] % len(PB)]
            sk["pb"] += 1
            return b

        def proj_items(hslot, ktdst, r_ktdst, vpdst, r_vpdst, col0, gi, do_kmean):
            items = []

            def kpair(p):
                bk = nextbank()
                for k in range(8):
                    T.op("pe", lambda e: e.matmul(pf(bk), lhsT=wA[:, k, 768 + p * 128: 768 + (p + 1) * 128],
                                                  rhs=hT[hslot][:, k, :], start=(k == 0), stop=(k == 7)),
                         r=[r_wA, r_hT[hslot]], w=[bank[bk]])
                s = sk["k"] % 3
                sk["k"] += 1
                if do_kmean:
                    T.op("dve", lambda e: e.tensor_reduce(out=kms[:], in_=pf(bk).rearrange("p (b t) -> p b t", b=2),
                                                          axis=AX.X, op=ALU.add),
                         r=[], w=[bank[bk], r_kms])
                    T.op("dve", lambda e: e.tensor_scalar(out=kmT[:, p, 2 * (gi // 4): 2 * (gi // 4) + 2], in0=kms[:],
                                                          scalar1=1.0 / 256.0, scalar2=None, op0=ALU.mult),
                         r=[r_kms], w=[r_kmT])
                evac(kst[s][:], pf(bk), r=[], w=[bank[bk], r_kst[s]])
                if not _DBG.get("nokst"):
                    T.dma("pool", ktdst[2 * p, 0:64, col0:col0 + 512], kst[s][0:64, :], r=[r_kst[s]], w=[r_ktdst[2 * p]])
                    T.dma("pool", ktdst[2 * p + 1, 64:128, col0:col0 + 512], kst[s][64:128, :], r=[r_kst[s]],
                          w=[r_ktdst[2 * p + 1]])

            vs = (gi // 4) % 2

            def vgrp(i, c0, n, h0, last):
                bk = nextbank()
                for k in range(8):
                    T.op("pe", lambda e: e.matmul(pf(bk, n), lhsT=hT[hslot][:, k, i * 128:(i + 1) * 128],
                                                  rhs=wA[:, k, 1536 + c0: 1536 + c0 + n],
                                                  start=(k == 0), stop=(k == 7)),
                         r=[r_wA, r_hT[hslot]], w=[bank[bk]])
                evac(vst[vs][:, h0:h0 + n // 64, i, 0:64], pf(bk, n).rearrange("p (h c) -> p h c", c=64),
                     r=[], w=[bank[bk], r_vst[vs]])
                if last and not _DBG.get("novst"):
                    T.dma("pool", vpdst[:, :, gi:gi + 4, :].rearrange("h p t c -> p h (t c)"),
                          vst[vs][:].rearrange("p h t c -> p h (t c)"), r=[r_vst[vs]], w=r_vpdst)

            for p in range(6):
                items.append(lambda p=p: kpair(p))
            for i in range(4):
                items.append(lambda i=i: vgrp(i, 0, 512, 0, False))
                items.append(lambda i=i: vgrp(i, 512, 256, 8, i == 3))
            return items

        r_VP = [Res("VP")]
        r_VPo = [Res("VPo")]
        NG1 = NTG // 4
        if stop_after == "A1s":
            NG1 = 2
        for f in norm_items(xg, 0, 4, 0, TB):
            f()
        for G in range(NG1):
            main = proj_items(G % 2, KT, r_KT, VP, r_VP, G * 512, G * 4, True)
            side = norm_items(xg, (G + 1) * 512, 4, (G + 1) % 2, TB) if G + 1 < NG1 else []
            run_interleaved(main, side)
        if KMD is not None:
            T.dma("sp", KMD, kmT[:].rearrange("p a b -> p (a b)"), r=[r_kmT])
        if stop_after in ("A1", "A1s"):
            T.barrier()
            return nc

        r_QM = Res()
        r_GT = Res()
        NG2 = NLT // 4
        if stop_after == "A2s":
            NG2 = 1
        cnt2 = {"e": 0, "z": 0}
        sk["PB"] = (5, 6, 7)
        TB2 = (0,)

        def a2_main_items(G):
            hs = G % 2
            qa_st = qa_sts[G % 2]
            r_qast = r_qasts[G % 2]
            items = proj_items(hs, KTo, r_KTo, VPo, r_VPo, G * 512, G * 4, False)

            def qpair(p):
                bk = nextbank()
                for k in range(8):
                    T.op("pe", lambda e: e.matmul(pf(bk), lhsT=wA[:, k, p * 128:(p + 1) * 128],
                                                  rhs=hT[hs][:, k, :], start=(k == 0), stop=(k == 7)),
                         r=[r_wA, r_hT[hs]], w=[bank[bk]])
                for e_ in range(2):
                    rows = slice(e_ * 64, (e_ + 1) * 64)
                    for v in range(2):
                        evac(qa_st[rows, 2 * p + e_, v, :], pbig[rows, bk * 512:(bk + 1) * 512], r=[],
                             w=[bank[bk], r_qast], scale=0.125)

            def qmpair(p):
                bk = nextbank()
                for k in range(8):
                    T.op("pe", lambda e: e.matmul(pf(bk), lhsT=wA[:, k, 2304 + p * 128: 2304 + (p + 1) * 128],
                                                  rhs=hT[hs][:, k, :], start=(k == 0), stop=(k == 7)),
                         r=[r_wA, r_hT[hs]], w=[bank[bk]])
                evac(qmst[:, p, :], pf(bk), r=[], w=[bank[bk], r_qmst], scale=0.125)
                if p == 1:
                    T.dma("pool", QM[:, :, G * 512:(G + 1) * 512].rearrange("a p c -> p a c"), qmst[:],
                          r=[r_qmst], w=[r_QM])

            def ztile(i):
                zb = 6
                for half in range(2):
                    for k in range(8):
                        T.op("pe", lambda e: e.matmul(pf(zb + half), lhsT=hT[hs][:, k, i * 128:(i + 1) * 128],
                                                      rhs=wA[:, k, 2560 + half * 512: 2560 + (half + 1) * 512],
                                                      start=(k == 0), stop=(k == 7)),
                             r=[r_wA, r_hT[hs]], w=[bank[zb + half]])
                zs = cnt2["z"] % 2
                cnt2["z"] += 1
                zps = pbig[:, zb * 512:(zb + 2) * 512]
                T.op("act", lambda e: e.activation(out=esb[zs][:], in_=zps, func=AF.Exp, scale=-1.0),
                     r=[], w=[bank[zb], bank[zb + 1], r_esb[zs]])
                T.op("act", lambda e: e.copy(out=gts[zs][:], in_=zps), r=[], w=[bank[zb], bank[zb + 1], r_gts[zs]])
                T.op("act", lambda e: e.activation(out=esb[zs][:], in_=esb[zs][:], func=AF.Ln, bias=1.0),
                     r=[], w=[r_esb[zs]])
                T.op("act", lambda e: e.activation(out=esb[zs][:], in_=esb[zs][:], func=AF.Exp, scale=-1.0),
                     r=[], w=[r_esb[zs]])
                T.op("dve", lambda e: e.tensor_tensor(out=gts[zs][:], in0=gts[zs][:], in1=esb[zs][:], op=ALU.mult),
                     r=[r_esb[zs]], w=[r_gts[zs]])
                lt = G * 4 + i
                T.dma("pool", GT[lt * 128:(lt + 1) * 128, :], gts[zs][:], r=[r_gts[zs]], w=[r_GT])

            for p in range(6):
                items.append(lambda p=p: qpair(p))
            for p in range(2):
                items.append(lambda p=p: qmpair(p))
            for i in range(4):
                items.append(lambda i=i: ztile(i))
            return items

        def a2_gate_items(G):
            qa_st = qa_sts[G % 2]
            r_qast = r_qasts[G % 2]
            items = []

            def gate(i, e_):
                lt = G * 4 + i
                E = Et[i % 2]
                r_E = r_Et[i % 2]
                gb = 1 + e_
                rows = slice(e_ * 64, (e_ + 1) * 64)
                for p in range(6):
                    T.op("pe", lambda e: e.matmul(pf(gb, 64, p * 64), lhsT=qa_st[rows, 2 * p + e_, 0, i * 128:(i + 1) * 128],
                                                  rhs=kmT[rows, p, :], start=True, stop=True),
                         r=[r_qast, r_kmT], w=[bank[gb]])
                T.op("dve", lambda e: e.tensor_tensor(out=gm[:], in0=pf(gb, 384).rearrange("p (a b) -> p a b", a=6),
                                                      in1=bmask[:, lt:lt + 1, :].to_broadcast([128, 6, NBLK]),
                                                      op=ALU.add),
                     r=[r_const], w=[bank[gb], r_gm])
                for p in range(6):
                    T.op("dve", lambda e: e.max(out=m8[:, p, :], in_=gm[:, p, :]), r=[r_gm], w=[r_m8[p]])
                T.op("dve", lambda e: e.tensor_scalar(out=thr[:], in0=m8[:, :, 2], scalar1=-1e29, scalar2=None,
                                                      op0=ALU.max), r=r_m8, w=[r_gm])
                T.op("dve", lambda e: e.tensor_tensor(out=selb[:], in0=gm[:],
                                                      in1=thr[:].unsqueeze(2).to_broadcast([128, 6, NBLK]),
                                                      op=ALU.is_ge), r=[], w=[r_gm])
                cb = (64 if e_ == 0 else 0) + 6
                for v in range(2):
                    T.op("dve", lambda e: e.tensor_scalar(out=E[:, e_::2, v, cb:cb + 32],
                                                          in0=selb[:, :, v * 32:(v + 1) * 32],
                                                          scalar1=1.0, scalar2=BIG, op0=ALU.subtract, op1=ALU.mult),
                         r=[], w=[r_gm, r_E])

            def etr(i, e_):
                E = Et[i % 2]
                r_E = r_Et[i % 2]
                ext = slice(64, 128) if e_ == 0 else slice(0, 64)
                for hb in range(2):
                    tbk = 3 + hb
                    pv = pbf(tbk)
                    for j in range(3):
                        h = e_ + 2 * (3 * hb + j)
                        for v in range(2):
                            sl_ = (j * 2 + v) * 128
                            T.op("pe", lambda e: e.transpose(pv[:, sl_:sl_ + 128], E[:, h, v, :], ident[:]),
                                 r=[r_E, r_const], w=[bank[tbk]])
                    h0 = e_ + 6 * hb
                    evac(qa_st[ext, h0:h0 + 5:2, :, i * 128:(i + 1) * 128],
                         pv[ext, 0:768].rearrange("p (a b t) -> p a b t", a=3, b=2),
                         r=[], w=[bank[tbk], r_qast])

            def store():
                cols = slice(G * 512, (G + 1) * 512)
                for e_ in range(2):
                    dh = slice(0, 64) if e_ == 0 else slice(64, 128)
                    ex = slice(70, 128) if e_ == 0 else slice(6, 64)
                    rq = [r_QA[h] for h in range(e_, H, 2)]
                    for v in range(2):
                        T.dma("pool", QA[e_::2, v, dh, cols].rearrange("h r c -> r h c"), qa_st[dh, e_::2, v, :],
                              r=[r_qast], w=rq)
                        T.dma("pool", QA[e_::2, v, ex, cols].rearrange("h r c -> r h c"), qa_st[ex, e_::2, v, :],
                              r=[r_qast], w=rq)

            g_ = [[(lambda i=i, e_=e_: gate(i, e_)) for e_ in range(2)] for i in range(4)]
            t_ = [[(lambda i=i, e_=e_: etr(i, e_)) for e_ in range(2)] for i in range(4)]
            items = g_[0] + g_[1] + t_[0] + g_[2] + t_[1] + g_[3] + t_[2] + t_[3] + [store]
            return items

        def merge(a, b):
            out = []
            n, m_ = len(a), len(b)
            j = 0
            for i_, f in enumerate(a):
                out.append(f)
                while j < m_ and (j + 1) * n <= (i_ + 1) * m_:
                    out.append(b[j])
                    j += 1
            out.extend(b[j:])
            return out

        for f in norm_items(xm, 0, 4, 0, TB2):
            f()
        for G in range(NG2 + 1):
            main = a2_main_items(G) if G < NG2 else []
            side = []
            if G >= 1:
                side = a2_gate_items(G - 1)
            if G + 1 < NG2:
                side = merge(side, norm_items(xm, (G + 1) * 512, 4, (G + 1) % 2, TB2)) if side else \
                    norm_items(xm, (G + 1) * 512, 4, (G + 1) % 2, TB2)
            if main:
                run_interleaved(main, side)
            else:
                for f in side:
                    f()
        if stop_after in ("A2", "A2s"):
            T.barrier()
            return nc

        T.barrier()
        stA.close()
        stY = contextlib.ExitStack()
        es.enter_context(stY)
        Y = sb("Y", [128, NQT, D], BF16, stY)
        r_Y = Res("Y")
        stB = contextlib.ExitStack()
        es.enter_context(stB)
        cmt = sb("cmt", [128, 2, 256], BF16, stB)
        T.dma("sp", cmt[:].rearrange("p a b -> p (a b)"), cm_d, w=[r_const])
        QAs = [sb("QAs%d" % i, [128, 2, NLOC], BF16, stB) for i in range(2)]
        r_QAs = [Res(), Res()]
        KTos = [sb("KTos%d" % i, [128, NLOC], BF16, stB) for i in range(2)]
        VPos = [sb("VPos%d" % i, [128, NLT, 65], BF16, stB) for i in range(2)]
        r_own = [Res(), Res()]
        NKS = 3
        kring = [sb("kring%d" % i, [128, 1024], BF16, stB) for i in range(NKS)]
        vring = [sb("vring%d" % i, [128, 8, 65], BF16, stB) for i in range(NKS)]
        r_ring = [Res() for _ in range(NKS)]
        NPT = 3
        pT = [sb("pT%d" % i, [128, 2, 384], BF16, stB) for i in range(NPT)]
        r_pT = [Res() for _ in range(NPT)]
        rec = sb("rec", [128, 9], F32, stB)
        r_rec = Res()
        qmT = sb("qmT", [128, 2, NLOC], BF16, stB)
        r_qmT = Res()
        T.dma("sp", qmT[:], QM.rearrange("a p c -> p a c"), r=[r_QM], w=[r_qmT])

        heads = list(range(H))
        units = list(range(NU))
        if stop_after == "B1s":
            heads = [0, 1, 7]
            units = [0, 1]

        def load_head(h, hb):
            for v in range(2):
                T.dma("sp", QAs[hb][:, v, :], QA[h, v], r=[r_QA[h]], w=[r_QAs[hb]])
            T.dma("sp", KTos[hb][:], KTo[h], r=[r_KTo[h]], w=[r_own[hb]])
            T.dma("sp", VPos[hb][:], VPo[h], r=r_VPo, w=[r_own[hb]])

        chunks = []
        for h in heads:
            for m in units:
                nb = 16 * m + 15
                for c in range((nb + 3) // 4):
                    chunks.append((h, m, c))
        cstate = {"next": 0}

        def ensure_loaded(ci):
            while cstate["next"] <= min(ci, len(chunks) - 1):
                n = cstate["next"]
                h, m, c = chunks[n]
                sl = n % NKS
                T.dma("sp", kring[sl][:], KT[h, :, c * 1024:(c + 1) * 1024], r=[r_KT[h]], w=[r_ring[sl]])
                T.dma("sp", vring[sl][:], VP[h, :, c * 8:(c + 1) * 8, :], r=r_VP, w=[r_ring[sl]])
                cstate["next"] += 1

        sst = {"s": 0}
        LAG = 2
        ci = 0
        for hi, h in enumerate(heads):
            hb = hi % 2
            if hi == 0:
                load_head(h, 0)
            if hi + 1 < len(heads):
                load_head(heads[hi + 1], (hi + 1) % 2)
            for m in units:
                nb = 16 * m + 15
                cnts = (16 * m + 12, 16 * m + 14, 16 * m + 15)
                steps = []
                for c in range((nb + 3) // 4):
                    for j in range(4 * c, min(4 * c + 4, nb)):
                        for g in range(3):
                            if j < cnts[g]:
                                steps.append(("past", j, g, ci + c))
                for qb in range(5):
                    steps.append(("own", qb, 0, -1))
                first = {6: True, 7: True}
                nst = len(steps)
                info = [None] * nst
                for idx in range(nst + LAG):
                    if idx < nst:
                        kind, a, g, cidx = steps[idx]
                        sbuf_i = sst["s"] % 3
                        sst["s"] += 1
                        b0 = 2 * sbuf_i
                        if kind == "past":
                            j = a
                            ensure_loaded(cidx + 1)
                            sl = cidx % NKS
                            v = j // 32
                            qc = (UT * m + 1 + 3 * g) * 128
                            N = 384
                            for kt2 in range(2):
                                kc = (j % 4) * 256 + kt2 * 128
                                T.op("pe", lambda e: e.matmul(pf(b0 + kt2, N), lhsT=kring[sl][:, kc:kc + 128],
                                                              rhs=QAs[hb][:, v, qc:qc + N], start=True, stop=True),
                                     r=[r_ring[sl], r_QAs[hb]], w=[bank[b0 + kt2]])
                            tqs = [3 * g + i_ for i_ in range(3)]
                            vsrc = [(vring[sl], (j % 4) * 2 + kt2) for kt2 in range(2)]
                            rres = [r_ring[sl]]
                        else:
                            qb = a
                            if qb == 0:
                                qc, N, c0 = (UT * m + 1) * 128, 128, 128
                                tqs = [0]
                            else:
                                qc, N, c0 = (UT * m + 2 * qb) * 128, 256, 0
                                tqs = [2 * qb - 1, 2 * qb]
                            for kt2 in range(2):
                                kc = (UT * m + 2 * qb + kt2) * 128
                                T.op("pe", lambda e: e.matmul(pf(b0 + kt2, N), lhsT=KTos[hb][:, kc:kc + 128],
                                                              rhs=QAs[hb][:, 0, qc:qc + N], start=True, stop=False),
                                     r=[r_own[hb], r_QAs[hb]], w=[bank[b0 + kt2]])
                                T.op("pe", lambda e: e.matmul(pf(b0 + kt2, N), lhsT=ident[:],
                                                              rhs=cmt[:, kt2, c0:c0 + N], start=False, stop=True),
                                     r=[r_const], w=[bank[b0 + kt2]])
                            vsrc = [(VPos[hb], UT * m + 2 * qb + kt2) for kt2 in range(2)]
                            rres = [r_own[hb]]
                        ps = sst["s"] % NPT
                        sview = pbig[:, b0 * 512:(b0 + 2) * 512].rearrange("p (a b) -> p a b", a=2)[:, :, 0:N]
                        T.op("act", lambda e: e.activation(out=pT[ps][:, :, 0:N], in_=sview, func=AF.Exp,
                                                           bias=abias[:, h:h + 1], scale=1.0),
                             r=[r_const], w=[bank[b0], bank[b0 + 1], r_pT[ps]])
                        info[idx] = (ps, tqs, vsrc, rres)
                    k_ = idx - LAG
                    if k_ >= 0:
                        ps, tqs, vsrc, rres = info[k_]
                        for qi_, tq in enumerate(tqs):
                            bk = 6 if tq < 5 else 7
                            col = (tq if tq < 5 else tq - 5) * 65
                            for kt2 in range(2):
                                vt, vi = vsrc[kt2]
                                fl = first[bk]
                                first[bk] = False
                                T.op("pe", lambda e: e.matmul(pf(bk, 65, col), lhsT=pT[ps][:, kt2, qi_ * 128:(qi_ + 1) * 128],
                                                              rhs=vt[:, vi, :], start=fl, stop=True,
                                                              skip_group_check=True),
                                     r=[r_pT[ps]] + rres, w=[bank[bk]])
                ci += (nb + 3) // 4
                for bk, nt, t0_ in ((6, 5, 0), (7, 4, 5)):
                    av = pf(bk, nt * 65).rearrange("p (a b) -> p a b", b=65)
                    T.op("dve", lambda e: e.reciprocal(out=rec[:, 0:nt], in_=av[:, :, 64]), r=[], w=[bank[bk], r_rec])
                    T.op("dve", lambda e: e.tensor_tensor(out=Y[:, 9 * m + t0_: 9 * m + t0_ + nt, h * 64:(h + 1) * 64],
                                                          in0=av[:, :, 0:64],
                                                          in1=rec[:, 0:nt].unsqueeze(2).to_broadcast([128, nt, 64]),
                                                          op=ALU.mult),
                         r=[r_rec], w=[bank[bk], r_Y])
        if YD is not None:
            T.dma("sp", YD, Y[:].rearrange("p a b -> p (a b)"), r=[r_Y])
        if stop_after in ("B1", "B1s"):
            T.barrier()
            return nc

        def mem_attn(L, qsrc, r_qsrc, qcols, dst_fn, r_dst, ntile, sbanks=(0, 2, 4), abanks=(6, 7), heads_=range(4)):
            N = ntile * 128
            for hm in heads_:
                pr, e_ = divmod(hm, 2)
                rows = slice(e_ * 64, (e_ + 1) * 64)
                sbuf_i = sst["s"] % len(sbanks)
                sst["s"] += 1
                b0 = sbanks[sbuf_i]
                for kt2 in range(2):
                    T.op("pe", lambda e: e.matmul(pf(b0 + kt2, N), lhsT=kmem[rows, L, pr, kt2 * 128:(kt2 + 1) * 128],
                                                  rhs=qsrc[rows, pr, qcols:qcols + N], start=True, stop=True),
                         r=[r_kmem, r_qsrc], w=[bank[b0 + kt2]])
                ps = sst["s"] % len(pT)
                sview = pbig[:, b0 * 512:(b0 + 2) * 512].rearrange("p (a b) -> p a b", a=2)[:, :, 0:N]
                T.op("act", lambda e: e.activation(out=pT[ps][:, :, 0:N], in_=sview, func=AF.Exp),
                     r=[], w=[bank[b0], bank[b0 + 1], r_pT[ps]])
                bk = abanks[hm % len(abanks)]
                fl = True
                for qt in range(ntile):
                    for kt2 in range(2):
                        T.op("pe", lambda e: e.matmul(pf(bk, 65, qt * 65), lhsT=pT[ps][:, kt2, qt * 128:(qt + 1) * 128],
                                                      rhs=vmem[:, L, kt2, hm, :], start=fl, stop=True,
                                                      skip_group_check=True),
                             r=[r_pT[ps], r_vmem], w=[bank[bk]])
                        fl = False
                av = pf(bk, ntile * 65).rearrange("p (a b) -> p a b", b=65)
                T.op("dve", lambda e: e.reciprocal(out=rec[:, 0:ntile], in_=av[:, :, 64]), r=[], w=[bank[bk], r_rec])
                T.op("dve", lambda e: e.tensor_tensor(out=dst_fn(hm), in0=av[:, :, 0:64],
                                                      in1=rec[:, 0:ntile].unsqueeze(2).to_broadcast([128, ntile, 64]),
                                                      op=ALU.mult),
                     r=[r_rec], w=[bank[bk], r_dst])

        for m in units:
            for g in range(3):
                q0 = 9 * m + 3 * g
                mem_attn(0, qmT, r_qmT, (UT * m + 1 + 3 * g) * 128,
                         lambda hm: Y[:, q0:q0 + 3, 768 + hm * 64: 768 + (hm + 1) * 64], r_Y, 3)
        if YD is not None:
            T.dma("sp", YD, Y[:].rearrange("p a b -> p (a b)"), r=[r_Y])
        r_YS = Res()
        T.dma("pool", YS, Y[:].rearrange("p a b -> p (a b)"), r=[r_Y], w=[r_YS])
        if stop_after in ("B2",):
            T.barrier()
            return nc

        T.barrier()
        stB.close()
        stY.close()
        stC = contextlib.ExitStack()
        es.enter_context(stC)
        wO0 = sb("wO0", [128, 8, D], BF16, stC)
        wO1 = sb("wO1", [128, 8, D], BF16, stC)
        wB = sb("wB", [128, 8, 2432], BF16, stC)
        r_wC = Res()
        stW = contextlib.ExitStack()
        stC.enter_context(stW)
        wst2 = [sb("wst2_%d" % i, [128, 1792], F32, stW) for i in range(2)]
        wst[0], wst[1] = wst2[0], wst2[1]
        r_wst[0], r_wst[1] = Res(), Res()
        for k in range(8):
            rows = slice(k * 128, (k + 1) * 128)
            wload(wO0[:, k, :], w_out[0, rows, :], D, None, r_wC)
            wload(wO1[:, k, :], w_out[1, rows, :], D, None, r_wC)
            g1c = gc[:, 8 + k: 9 + k]
            wload(wB[:, k, 0:768], w_in_b[rows, 0:768], 768, g1c, r_wC)
            for gk in range(2):
                for dup in range(2):
                    c0 = 768 + gk * 128 + dup * 64
                    wload(wB[:, k, c0:c0 + 64], w_in_b[rows, 768 + gk * 64: 768 + (gk + 1) * 64], 64, g1c, r_wC)
            wload(wB[:, k, 1024:2432], w_in_b[rows, 896:2304], 1408, g1c, r_wC)
        T.barrier()
        stW.close()
        swac = sb("swac", [128, 2, H, 128], F32, stC)
        fgt = sb("fgt", [128, D], F32, stC)
        esink = sb("esink", [128, H], F32, stC)
        halob = sb("halob", [128, NU], F32, stC)
        zero1 = sb("zero1", [128, 1], F32, stC)
        T.dma("sp", swac[:].rearrange("p a b c -> p (a b c)"), swac_d, w=[r_const])
        T.dma("sp", fgt[:], fgr, w=[r_const])
        T.dma("sp", halob[:], halob_d, w=[r_const])
        T.dma("sp", esink[:], sinkr, w=[r_const])
        T.op("act", lambda e: e.activation(out=esink[:], in_=esink[:], func=AF.Exp), r=[], w=[r_const])
        T.op("dve", lambda e: e.memset(zero1[:], 0.0), w=[r_const])

        yt = [sb("yt%d" % i, [128, D], BF16, stC) for i in range(2)]
        r_yt = [Res(), Res()]
        gtt = [sb("gtt0", [128, D], F32, stC)] * 2
        r_gtt = [Res()] * 2
        x1g = [sb("x1g%d" % i, [128, 3, D], F32, stC) for i in range(2)]
        r_x1g = [[Res() for _ in range(3)] for _ in range(2)]
        yg = [sb("yg%d" % i, [128, D], BF16, stC) for i in range(2)]
        r_yg = [Res(), Res()]
        ygT = [sb("ygT%d" % i, [128, 8, 128], BF16, stC) for i in range(2)]
        r_ygT = [Res(), Res()]
        ssc = [sb("ssc%d" % i, [128, 4], F32, stC) for i in range(4)]
        r_ssc = [Res() for _ in range(4)]
        xs1 = [sb("xs1_%d" % i, [128, D], BF16, stC) for i in range(2)]
        r_xs1 = [Res(), Res()]
        h1T = [sb("h1T%d" % i, [128, 8, 384], BF16, stC) for i in range(2)]
        r_h1T = [Res(), Res()]
        q1T = [sb("q1T%d" % i, [128, 6, 384], BF16, stC) for i in range(2)]
        r_q1T = [Res(), Res()]
        qm1T = [sb("qm1T%d" % i, [128, 2, 384], BF16, stC) for i in range(2)]
        r_qm1T = [Res(), Res()]
        k1T = [sb("k1T%d" % i, [128, 2, 9 * 128], BF16, stC) for i in range(2)]
        v1 = [sb("v1_%d" % i, [128, 9, 2, 65], BF16, stC) for i in range(2)]
        r_kv1 = [Res(), Res()]
        for i in range(2):
            T.op("dve", lambda e: e.memset(v1[i][:].rearrange("p a b c -> p (a b c)"), 1.0), w=[r_kv1[i]])
        y1 = [sb("y1_0", [128, 3, D], BF16, stC)] * 2
        r_y1 = [Res()] * 2
        sbs = [sb("sbs%d" % i, [128, 6, 128], F32, stC) for i in range(2)]
        r_sbs = [Res(), Res()]
        p1T = [sb("p1T%d" % i, [128, 6, 128], BF16, stC) for i in range(2)]
        r_p1T = [Res(), Res()]
        den = sb("den", [128, 6], F32, stC)
        r_den = Res()
        es1 = [sb("es1_0", [128, D], F32, stC)] * 2
        r_es1 = [Res()] * 2
        ot = [sb("ot0", [128, D], F32, stC)] * 2
        r_ot = [Res()] * 2
        pT = [sb("pTc%d" % i, [128, 2, 384], BF16, stC) for i in range(2)]
        r_pT = [Res() for _ in range(2)]
        yg2 = sb("yg2", [128, D], BF16, stC)
        r_yg2 = Res()
        ygT2 = sb("ygT2", [128, 8, 128], BF16, stC)
        r_ygT2 = Res()
        rec = sb("recc", [128, 9], F32, stC)
        r_rec = Res()
        r_out = Res()
        cc = {"t": 0, "n": 0, "pj": 0, "sw": 0, "z": 0, "o": 0}
        TBK, ABK = 0, 1

        def rms_scale(src, r_src, dst_bf, r_dst):
            s_ = cc["n"] % 4
            cc["n"] += 1
            T.op("act", lambda e: e.activation(out=junk[:], in_=src, func=AF.Square, accum_out=ssc[s_][:, 0:1]),
                 r=[r_src], w=[r_ssc[s_]])
            T.op("act", lambda e: e.activation(out=ssc[s_][:, 1:2], in_=ssc[s_][:, 0:1], func=AF.Ln,
                                               bias=epst[:, 0:1], scale=1.0 / D), r=[r_const], w=[r_ssc[s_]])
            T.op("act", lambda e: e.activation(out=ssc[s_][:, 2:3], in_=ssc[s_][:, 1:2], func=AF.Exp, scale=-0.5),
                 r=[], w=[r_ssc[s_]])
            if dst_bf is not None:
                T.op("dve", lambda e: e.tensor_scalar(out=dst_bf, in0=src, scalar1=ssc[s_][:, 2:3], scalar2=None,
                                                      op0=ALU.mult), r=[r_src, r_ssc[s_]], w=[r_dst])
            return s_

        def transpose8(src_bf, r_src, dst, r_dstT):
            pv = pbf(TBK)
            for k in range(8):
                T.op("pe", lambda e: e.transpose(pv[:, k * 128:(k + 1) * 128], src_bf[:, k * 128:(k + 1) * 128], ident[:]),
                     r=[r_src, r_const], w=[bank[TBK]])
            evac(dst, pv.rearrange("p (k t) -> p k t", k=8), r=[], w=[bank[TBK], r_dstT])

        def outproj(srcT, r_srcT, w_, xres, r_xres):
            for half in range(2):
                for k in range(8):
                    T.op("pe", lambda e: e.matmul(pf(2 + half), lhsT=srcT[:, k, :], rhs=w_[:, k, half * 512:(half + 1) * 512],
                                                  start=(k == 0), stop=(k == 7)),
                         r=[r_srcT, r_wC], w=[bank[2 + half]])
            T.op("dve", lambda e: e.tensor_tensor(out=xres, in0=pbig[:, 1024:2048], in1=xres, op=ALU.add),
                 r=[], w=[bank[2], bank[3], r_xres])

        def featproj(hT_, r_hT_, c0, dst, r_dst, scale, N=384):
            bk = 4 + cc["pj"] % 2
            cc["pj"] += 1
            for k in range(8):
                T.op("pe", lambda e: e.matmul(pf(bk, N), lhsT=wB[:, k, c0:c0 + 128], rhs=hT_[:, k, 0:N],
                                              start=(k == 0), stop=(k == 7)),
                     r=[r_wC, r_hT_], w=[bank[bk]])
            evac(dst, pf(bk, N), r=[], w=[bank[bk], r_dst], scale=scale)

        groups = [(m, g) for m in units for g in range(3)]

        def stageP(m, g, gb):
            ub = m % 2
            u0 = 1 + 3 * g
            items = []

            def t_a(i):
                u = u0 + i
                lt = UT * m + u
                qi = 9 * m + u - 1
                ys = i % 2
                T.dma("sp", yt[ys][:], YS[:, qi * D:(qi + 1) * D], r=[r_YS], w=[r_yt[ys]])
                T.dma("sp", gtt[ys][:], GT[lt * 128:(lt + 1) * 128, :], r=[r_GT], w=[r_gtt[ys]])
                T.dma("sp", x1g[gb][:, i, :], xm[lt * 128:(lt + 1) * 128, :], w=[r_x1g[gb][i]])
                T.op("dve", lambda e: e.tensor_tensor(out=yg[ys][:], in0=yt[ys][:], in1=gtt[ys][:], op=ALU.mult),
                     r=[r_yt[ys], r_gtt[ys]], w=[r_yg[ys]])

            def t_b(i):
                ys = i % 2
                transpose8(yg[ys], r_yg[ys], ygT[ys][:], r_ygT[ys])
                outproj(ygT[ys], r_ygT[ys], wO0, x1g[gb][:, i, :], r_x1g[gb][i])

            def t_c(i):
                ys = i % 2
                rms_scale(x1g[gb][:, i, :], r_x1g[gb][i], xs1[ys][:], r_xs1[ys])
                transpose8(xs1[ys], r_xs1[ys], h1T[gb][:, :, i * 128:(i + 1) * 128], r_h1T[gb])

            def vproj(i):
                bk = 4 + cc["pj"] % 2
                cc["pj"] += 1
                for k in range(8):
                    T.op("pe", lambda e: e.matmul(pf(bk, 128), lhsT=h1T[gb][:, k, i * 128:(i + 1) * 128],
                                                  rhs=wB[:, k, 1024:1152], start=(k == 0), stop=(k == 7)),
                         r=[r_wC, r_h1T[gb]], w=[bank[bk]])
                evac(v1[ub][:, u0 - 1 + i, :, 0:64], pf(bk, 128).rearrange("p (a b) -> p a b", a=2),
                     r=[], w=[bank[bk], r_kv1[ub]])

            items.append(lambda: t_a(0))
            items.append(lambda: t_a(1))
            items.append(lambda: t_b(0))
            items.append(lambda: t_b(1))
            items.append(lambda: t_a(2))
            items.append(lambda: t_c(0))
            items.append(lambda: t_b(2))
            items.append(lambda: t_c(1))
            items.append(lambda: t_c(2))
            for gk in range(2):
                items.append(lambda gk=gk: featproj(h1T[gb], r_h1T[gb], 768 + gk * 128,
                                                    k1T[ub][:, gk, (u0 - 1) * 128:(u0 + 2) * 128], r_kv1[ub], None))
            for i in range(3):
                items.append(lambda i=i: vproj(i))
            for p in range(6):
                items.append(lambda p=p: featproj(h1T[gb], r_h1T[gb], p * 128, q1T[gb][:, p, :], r_q1T[gb], 0.125))
            for p in range(2):
                items.append(lambda p=p: featproj(h1T[gb], r_h1T[gb], 1152 + p * 128, qm1T[gb][:, p, :],
                                                  r_qm1T[gb], 0.125))
            return items

        def stageQ(m, g, gb):
            ub = m % 2
            u0 = 1 + 3 * g
            items = []

            tiles = [i for i in range(3) if u0 + i >= 2]
            steps = []

            def swa_s(i, e_, kt, sw):
                u = u0 + i
                rows = slice(e_ * 64, (e_ + 1) * 64)
                kcol = (u - 2 + kt) * 128
                for gk in range(2):
                    T.op("pe", lambda e: e.matmul(pf(6 + gk, 384).rearrange("p (a b) -> p a b", a=3),
                                                  lhsT=k1T[ub][rows, gk, kcol:kcol + 128],
                                                  rhs=q1T[gb][rows, 3 * gk:3 * gk + 3, i * 128:(i + 1) * 128],
                                                  start=True, stop=True),
                         r=[r_kv1[ub], r_q1T[gb]], w=[bank[6 + gk]])
                for gk in range(2):
                    T.op("dve", lambda e: e.tensor_tensor(
                        out=sbs[sw][:, 3 * gk:3 * gk + 3, :],
                        in0=pf(6 + gk, 384).rearrange("p (a b) -> p a b", a=3),
                        in1=swac[:, kt, e_ + 6 * gk: e_ + 6 * gk + 5:2, :], op=ALU.add),
                        r=[r_const], w=[bank[6 + gk], r_sbs[sw]])
                bias_ap = halob[:, m:m + 1] if (kt == 0 and u == 2) else zero1[:, 0:1]
                T.op("act", lambda e: e.activation(out=p1T[sw][:], in_=sbs[sw][:], func=AF.Exp,
                                                   bias=bias_ap, scale=1.0),
                     r=[r_sbs[sw], r_const], w=[r_p1T[sw]])

            def swa_pv(i, e_, kt, sw):
                u = u0 + i
                for hh in range(6):
                    gk = hh // 3
                    T.op("pe", lambda e: e.matmul(pf(ABK, 65, hh * 65), lhsT=p1T[sw][:, hh, :],
                                                  rhs=v1[ub][:, u - 2 + kt, gk, :], start=(kt == 0 and hh == 0),
                                                  stop=True, skip_group_check=True),
                         r=[r_p1T[sw], r_kv1[ub]], w=[bank[ABK]])
                if kt == 1:
                    av = pf(ABK, 390).rearrange("p (a b) -> p a b", b=65)
                    T.op("dve", lambda e: e.tensor_tensor(out=den[:], in0=av[:, :, 64], in1=esink[:, e_::2], op=ALU.add),
                         r=[r_const], w=[bank[ABK], r_den])
                    T.op("dve", lambda e: e.reciprocal(out=den[:], in_=den[:]), r=[], w=[r_den])
                    T.op("dve", lambda e: e.tensor_tensor(
                        out=y1[gb][:, i, 0:768].rearrange("p (h c) -> p h c", c=64)[:, e_::2, :],
                        in0=av[:, :, 0:64], in1=den[:].unsqueeze(2).to_broadcast([128, 6, 64]), op=ALU.mult),
                        r=[r_den], w=[bank[ABK], r_y1[gb]])

            def mem_s(hm, ps):
                pr, e_ = divmod(hm, 2)
                rows = slice(e_ * 64, (e_ + 1) * 64)
                for kt2 in range(2):
                    T.op("pe", lambda e: e.matmul(pf(6 + kt2, 384), lhsT=kmem[rows, 1, pr, kt2 * 128:(kt2 + 1) * 128],
                                                  rhs=qm1T[gb][rows, pr, 0:384], start=True, stop=True),
                         r=[r_kmem, r_qm1T[gb]], w=[bank[6 + kt2]])
                sview = pbig[:, 6 * 512:8 * 512].rearrange("p (a b) -> p a b", a=2)[:, :, 0:384]
                T.op("act", lambda e: e.activation(out=pT[ps][:], in_=sview, func=AF.Exp),
                     r=[], w=[bank[6], bank[7], r_pT[ps]])

            def mem_pv(hm, ps):
                fl = True
                for qt in range(3):
                    for kt2 in range(2):
                        T.op("pe", lambda e: e.matmul(pf(ABK, 65, qt * 65), lhsT=pT[ps][:, kt2, qt * 128:(qt + 1) * 128],
                                                      rhs=vmem[:, 1, kt2, hm, :], start=fl, stop=True,
                                                      skip_group_check=True),
                             r=[r_pT[ps], r_vmem], w=[bank[ABK]])
                        fl = False
                av = pf(ABK, 195).rearrange("p (a b) -> p a b", b=65)
                T.op("dve", lambda e: e.reciprocal(out=rec[:, 0:3], in_=av[:, :, 64]), r=[], w=[bank[ABK], r_rec])
                T.op("dve", lambda e: e.tensor_tensor(out=y1[gb][:, 0:3, 768 + hm * 64: 768 + (hm + 1) * 64],
                                                      in0=av[:, :, 0:64],
                                                      in1=rec[:, 0:3].unsqueeze(2).to_broadcast([128, 3, 64]),
                                                      op=ALU.mult),
                     r=[r_rec], w=[bank[ABK], r_y1[gb]])

            k_ = 0
            for i in tiles:
                for e_ in range(2):
                    for kt in range(2):
                        sw = k_ % 2
                        steps.append((lambda i=i, e_=e_, kt=kt, sw=sw: swa_s(i, e_, kt, sw),
                                      lambda i=i, e_=e_, kt=kt, sw=sw: swa_pv(i, e_, kt, sw)))
                        k_ += 1
            for hm in range(4):
                steps.append((lambda hm=hm: mem_s(hm, hm % 2), lambda hm=hm: mem_pv(hm, hm % 2)))
            for k in range(len(steps) + 1):
                def f(k=k):
                    if k < len(steps):
                        steps[k][0]()
                    if k >= 1:
                        steps[k - 1][1]()
                items.append(f)

            def t_z1(i):
                for half in range(2):
                    for k in range(8):
                        T.op("pe", lambda e: e.matmul(pf(2 + half), lhsT=h1T[gb][:, k, i * 128:(i + 1) * 128],
                                                      rhs=wB[:, k, 1408 + half * 512: 1408 + (half + 1) * 512],
                                                      start=(k == 0), stop=(k == 7)),
                             r=[r_wC, r_h1T[gb]], w=[bank[2 + half]])
                zs = 0
                zps = pbig[:, 1024:2048]
                T.op("act", lambda e: e.activation(out=es1[zs][:], in_=zps, func=AF.Exp, scale=-1.0),
                     r=[], w=[bank[2], bank[3], r_es1[zs]])
                T.op("act", lambda e: e.copy(out=ot[zs][:], in_=zps), r=[], w=[bank[2], bank[3], r_ot[zs]])
                T.op("act", lambda e: e.activation(out=es1[zs][:], in_=es1[zs][:], func=AF.Ln, bias=1.0),
                     r=[], w=[r_es1[zs]])
                T.op("act", lambda e: e.activation(out=es1[zs][:], in_=es1[zs][:], func=AF.Exp, scale=-1.0),
                     r=[], w=[r_es1[zs]])
                T.op("dve", lambda e: e.tensor_tensor(out=es1[zs][:], in0=ot[zs][:], in1=es1[zs][:], op=ALU.mult),
                     r=[r_ot[zs]], w=[r_es1[zs]])
                T.op("dve", lambda e: e.tensor_tensor(out=yg2[:], in0=y1[gb][:, i, :], in1=es1[zs][:], op=ALU.mult),
                     r=[r_y1[gb], r_es1[zs]], w=[r_yg2])

            def t_z2(i):
                transpose8(yg2, r_yg2, ygT2[:], r_ygT2)

            def t_o(i):
                u = u0 + i
                outproj(ygT2, r_ygT2, wO1, x1g[gb][:, i, :], r_x1g[gb][i])
                s_ = rms_scale(x1g[gb][:, i, :], r_x1g[gb][i], None, None)
                T.op("dve", lambda e: e.scalar_tensor_tensor(out=ot[0][:], in0=x1g[gb][:, i, :], scalar=ssc[s_][:, 2:3],
                                                             in1=fgt[:], op0=ALU.mult, op1=ALU.mult),
                     r=[r_x1g[gb][i], r_ssc[s_], r_const], w=[r_ot[0]])
                orow = (m * 8 + u - 2) * 128
                T.dma("pool", out_d[orow:orow + 128, :], ot[0][:], r=[r_ot[0]], w=[r_out])

            for i in tiles:
                items.append(lambda i=i: t_z1(i))
                items.append(lambda i=i: t_z2(i))
                items.append(lambda i=i: t_o(i))
            return items

        for gi_ in range(len(groups) + 1):
            main = stageP(groups[gi_][0], groups[gi_][1], gi_ % 2) if gi_ < len(groups) else []
            side = stageQ(groups[gi_ - 1][0], groups[gi_ - 1][1], (gi_ - 1) % 2) if gi_ >= 1 else []
            if main:
                run_interleaved(main, side)
            else:
                for f in side:
                    f()
        T.barrier()
    return nc


def host_consts(q):
    sl = _slopes()
    c = {}
    kx = np.zeros((64, S), dtype=np.float32)
    kx[0:3, :] = 1.0
    kt = (np.arange(S) // 128).astype(np.float32)
    kx[3:6, :] = kt[None, :]
    blk = np.arange(S) // 256
    kx[6 + (blk % 32), np.arange(S)] = 1.0
    c["kx"] = kx.astype(NPBF)
    tpos = np.zeros(NLOC, dtype=np.int64)
    for m in range(NU):
        base = (16 * m + 4 * q) * 256 - 256
        tpos[m * UT * 128:(m + 1) * UT * 128] = base + np.arange(UT * 128)
    kxo = np.zeros((64, NLOC), dtype=np.float32)
    kxo[0:3, :] = 1.0
    kto = np.floor_divide(tpos, 128).astype(np.float32)
    kxo[3:6, :] = kto[None, :]
    c["kxo"] = kxo.astype(NPBF)
    qac = np.zeros((H, 6, NLOC), dtype=NPBF)
    for h in range(H):
        cc = (-sl[h] * tpos.astype(np.float32)).astype(np.float32)
        hi, mid, lo = _split3(cc)
        qac[h, 0], qac[h, 1], qac[h, 2] = hi, mid, lo
        s1, s2, s3 = _split3(np.float32(sl[h]))
        qac[h, 3] = (np.float32(128.0) * s1.astype(np.float32)).astype(NPBF)
        qac[h, 4] = (np.float32(128.0) * s2.astype(np.float32)).astype(NPBF)
        qac[h, 5] = (np.float32(128.0) * s3.astype(np.float32)).astype(NPBF)
    c["qac"] = qac
    c["abias"] = (np.arange(128, dtype=np.float32)[:, None] * sl[None, :]).astype(np.float32)
    bm = np.zeros((NLT, NBLK), dtype=np.float32)
    for lt in range(NLT):
        m, u = divmod(lt, UT)
        i_own = 16 * m + 4 * q - 1 + u // 2
        bm[lt, :] = np.where(np.arange(NBLK) < i_own, 0.0, -1e30)
    c["bmask"] = np.ascontiguousarray(np.broadcast_to(bm.reshape(1, -1), (128, NLT * NBLK))).astype(np.float32)
    p = np.arange(128)[:, None, None]
    k2 = np.arange(2)[None, :, None]
    t = np.arange(256)[None, None, :]
    cm = np.where(t >= 128 * k2 + p, 0.0, -BIG).astype(np.float32)
    c["cm"] = cm.reshape(128, 512).astype(NPBF)
    c["ident"] = np.eye(128, dtype=np.float32).astype(NPBF)
    pk = np.arange(128, dtype=np.float32)[:, None, None]
    tt = np.arange(128, dtype=np.float32)[None, None, :]
    slh = sl[None, :, None]
    d_cur = tt - pk
    cur = np.where(d_cur >= 0, -slh * d_cur, NEG)
    d_prev = 128.0 + tt - pk
    prev = np.where(d_prev < 128, -slh * d_prev, NEG)
    c["swac"] = np.stack([prev, cur], axis=1).astype(np.float32).reshape(128, 2 * H * 128)
    hb = np.zeros((128, NU), dtype=np.float32)
    if q == 0:
        hb[:, 0] = NEG
    c["halob"] = hb
    return c


def make_in_maps(x, mem, norm_g, w_in_a, w_in_b, sinks_b, w_mem_kv, w_out, mem_norm_g, final_norm_g, cores):
    x = np.asarray(x, dtype=np.float32)
    in_maps = []
    gcols = np.concatenate([np.asarray(norm_g[0], np.float32).reshape(8, 128).T,
                            np.asarray(norm_g[1], np.float32).reshape(8, 128).T,
                            np.asarray(mem_norm_g, np.float32).reshape(8, 128).T], axis=1)
    fgr = np.ascontiguousarray(np.broadcast_to(np.asarray(final_norm_g, np.float32)[None, :], (128, D)))
    sinkr = np.ascontiguousarray(np.broadcast_to(np.asarray(sinks_b, np.float32).reshape(1, H), (128, H)))
    cc = {q: host_consts(q) for q in range(4)}
    for c in cores:
        b, q = divmod(c, 4)
        xmine = np.zeros((NLOC, D), dtype=np.float32)
        for m in range(NU):
            t0 = (16 * m + 4 * q) * 256 - 256
            lo = max(t0, 0)
            xmine[m * 1280 + (lo - t0):(m + 1) * 1280] = x[b, lo:t0 + 1280]
        d = {"xg": np.ascontiguousarray(x[b]), "xm": xmine, "memb": np.ascontiguousarray(np.asarray(mem, np.float32)[b]),
             "w_in_a": np.ascontiguousarray(np.asarray(w_in_a, np.float32)[0]),
             "w_in_b": np.ascontiguousarray(np.asarray(w_in_b, np.float32)[0]),
             "w_mem": np.ascontiguousarray(np.asarray(w_mem_kv, np.float32)),
             "w_out": np.ascontiguousarray(np.asarray(w_out, np.float32)),
             "gcols": np.ascontiguousarray(gcols), "fgr": fgr, "sinkr": sinkr}
        d.update(cc[q])
        in_maps.append(d)
    return in_maps


def kernel(x, mem, norm_g, w_in_a, w_in_b, sinks_b, w_mem_kv, w_out, mem_norm_g, final_norm_g):
    cores = list(range(8))
    in_maps = make_in_maps(x, mem, norm_g, w_in_a, w_in_b, sinks_b, w_mem_kv, w_out, mem_norm_g, final_norm_g, cores)
    nc = build()
    res = run_bass_kernel_spmd(nc, in_maps, core_ids=cores)
    out = np.zeros((2, S, D), dtype=np.float32)
    for c in cores:
        b, q = divmod(c, 4)
        o = res.results[c]["out"]
        for m in range(NU):
            t0 = (16 * m + 4 * q) * 256
            out[b, t0:t0 + 1024] = o[m * 1024:(m + 1) * 1024]
    return out
```

```python
import contextlib
import math
import numpy as np
import ml_dtypes
import concourse.bass as bass
import concourse.mybir as mybir
from concourse.bass_utils import run_bass_kernel_spmd

F32 = mybir.dt.float32
BF16 = mybir.dt.bfloat16
AF = mybir.ActivationFunctionType
ALU = mybir.AluOpType
AX = mybir.AxisListType
NPBF = ml_dtypes.bfloat16

D = 1024
S = 16384
H = 12
DH = 64
NBLK = 64
NTG = 128
UT = 10
NU = 4
NLT = NU * UT
NLOC = NLT * 128
NQT = 36
WA = 3584
EPS = 1e-6
BIG = 32768.0
NEG = -30000.0
_DBG = {}


def _slopes():
    def p2(n):
        st = 2.0 ** (-8.0 / n)
        return [st ** (i + 1) for i in range(n)]
    c = 2 ** math.floor(math.log2(H))
    vals = p2(c) + p2(2 * c)[0::2][: H - c]
    return np.array(vals, dtype=np.float32)


def _split3(x):
    x = np.asarray(x, dtype=np.float32)
    hi = x.astype(NPBF)
    r1 = (x - hi.astype(np.float32)).astype(np.float32)
    mid = r1.astype(NPBF)
    r2 = (r1 - mid.astype(np.float32)).astype(np.float32)
    lo = r2.astype(NPBF)
    return hi, mid, lo


class Res:
    __slots__ = ("w", "r", "name")

    def __init__(self, name=""):
        self.w = None
        self.r = {}
        self.name = name


class Trk:
    def __init__(self, nc, es):
        self.nc = nc
        self.eng = {"pe": nc.tensor, "act": nc.scalar, "dve": nc.vector,
                    "pool": nc.gpsimd, "sp": nc.sync}
        self.sem = {}
        self.cnt = {}
        for e in ("pe", "act", "dve", "pool"):
            self.sem[e] = es.enter_context(nc.semaphore("c_" + e))
            self.cnt[e] = 0
        self.seen = {e: {} for e in self.eng}
        self.dsem = {}
        self.dcnt = {}
        self.dnext = {}
        for q, n in (("sp", 16), ("act", 4), ("pool", 16)):
            self.dsem[q] = [es.enter_context(nc.semaphore("d_%s%d" % (q, i))) for i in range(n)]
            self.dcnt[q] = [0] * n
            self.dnext[q] = 0
        self.ninstr = 0

    def _wait(self, eng, ev):
        sem, val, key = ev
        if self.seen[eng].get(key, 0) >= val:
            return
        self.eng[eng].wait_ge(sem, val)
        self.seen[eng][key] = val

    def _deps(self, eng, r, w):
        evs = {}

        def add(ev):
            if ev is None:
                return
            k = ev[2]
            if k not in evs or evs[k][1] < ev[1]:
                evs[k] = ev
        for x in r:
            add(x.w)
        for x in w:
            add(x.w)
            for ev in x.r.values():
                add(ev)
        for ev in evs.values():
            if eng == "pe" and ev[2] == "c_pe":
                continue
            self._wait(eng, ev)

    def _upd(self, ev, r, w):
        k = ev[2]
        for x in r:
            x.r[k] = ev
        for x in w:
            x.w = ev
            x.r = {}

    def op(self, eng, fn, r=(), w=()):
        self._deps(eng, r, w)
        ins = fn(self.eng[eng])
        self.cnt[eng] += 1
        ins.then_inc(self.sem[eng], 1)
        ev = (self.sem[eng], self.cnt[eng], "c_" + eng)
        self._upd(ev, r, w)
        self.ninstr += 1
        return ev

    def dma(self, q, out, in_, r=(), w=()):
        self._deps(q, r, w)
        i = self.dnext[q]
        self.dnext[q] = (i + 1) % len(self.dsem[q])
        sem = self.dsem[q][i]
        key = "d_%s%d" % (q, i)
        if self.dcnt[q][i] > 0:
            self._wait(q, (sem, self.dcnt[q][i], key))
        self.eng[q].dma_start(out=out, in_=in_).then_inc(sem, 16)
        self.dcnt[q][i] += 16
        ev = (sem, self.dcnt[q][i], key)
        self._upd(ev, r, w)
        self.ninstr += 1
        return ev

    def barrier(self):
        evs = []
        for e in self.sem:
            if self.cnt[e] > 0:
                evs.append((self.sem[e], self.cnt[e], "c_" + e))
        for q in self.dsem:
            for i, sem in enumerate(self.dsem[q]):
                if self.dcnt[q][i] > 0:
                    evs.append((sem, self.dcnt[q][i], "d_%s%d" % (q, i)))
        for e in self.eng:
            for ev in evs:
                self._wait(e, ev)


def build(stop_after="all", debug=False):
    nc = bass.Bass("TRN2", target_bir_lowering=False)
    es = contextlib.ExitStack()

    def din(name, shape, dt=F32):
        return nc.dram_tensor(name, list(shape), dt, kind="ExternalInput").ap()

    dbg = set(debug) if debug else set()

    def dscr(name, shape, dt):
        kind = "ExternalOutput" if name in dbg else "Internal"
        return nc.dram_tensor(name, list(shape), dt, kind=kind).ap()

    xg = din("xg", [S, D])
    xm = din("xm", [NLOC, D])
    memb = din("memb", [256, D])
    w_in_a = din("w_in_a", [D, WA])
    w_in_b = din("w_in_b", [D, 2304])
    w_mem = din("w_mem", [2, D, 512])
    w_out = din("w_out", [2, D, D])
    gcols = din("gcols", [128, 24])
    fgr = din("fgr", [128, D])
    sinkr = din("sinkr", [128, H])
    kx = din("kx", [64, S], BF16)
    kxo = din("kxo", [64, NLOC], BF16)
    qac = din("qac", [H, 6, NLOC], BF16)
    abias_d = din("abias", [128, H])
    bmask_d = din("bmask", [128, NLT * NBLK])
    cm_d = din("cm", [128, 512], BF16)
    ident_d = din("ident", [128, 128], BF16)
    swac_d = din("swac", [128, 2 * H * 128])
    halob_d = din("halob", [128, NU])
    out_d = nc.dram_tensor("out", [NU * 1024, D], F32, kind="ExternalOutput").ap()

    KT = dscr("KT", [H, 128, S], BF16)
    VP = dscr("VP", [H, 128, NTG, 65], BF16)
    KTo = dscr("KTo", [H, 128, NLOC], BF16)
    VPo = dscr("VPo", [H, 128, NLT, 65], BF16)
    QA = dscr("QA", [H, 2, 128, NLOC], BF16)
    QM = dscr("QM", [2, 128, NLOC], BF16)
    GT = dscr("GT", [NLOC, D], F32)
    YS = dscr("YS", [128, NQT * D], BF16)
    KMD = dscr("KMD", [128, 6 * NBLK], BF16) if "KMD" in dbg else None
    YD = dscr("YD", [128, NQT * D], BF16) if "YD" in dbg else None

    with es:
        T = Trk(nc, es)

        def sb(name, shape, dt, st=None):
            return (st or es).enter_context(nc.sbuf_tensor("s_" + name, list(shape), dt))

        pbig = es.enter_context(nc.psum_tensor("pbig", [128, 4096], F32))
        bank = [Res("bank%d" % i) for i in range(8)]

        def pf(b, n=512, off=0):
            return pbig[:, b * 512 + off: b * 512 + off + n]

        def pbf(b):
            return pbig[:, b * 512:(b + 1) * 512].bitcast(BF16)

        ident = sb("ident", [128, 128], BF16)
        abias = sb("abias", [128, H], F32)
        gc = sb("gc", [128, 24], F32)
        epst = sb("epst", [128, 1], F32)
        junk = sb("junk", [128, 1024], BF16)
        r_const = Res("const")
        T.dma("sp", ident[:], ident_d, w=[r_const])
        T.dma("sp", abias[:], abias_d, w=[r_const])
        T.dma("sp", gc[:], gcols, w=[r_const])
        T.op("dve", lambda e: e.memset(epst[:], EPS), w=[r_const])

        r_KT = [Res("KT%d" % h) for h in range(H)]
        r_KTo = [Res("KTo%d" % h) for h in range(H)]
        r_QA = [Res("QA%d" % h) for h in range(H)]
        for h in range(H):
            eb = 64 if h % 2 == 0 else 0
            T.dma("sp", KT[h, eb:eb + 64, :], kx, w=[r_KT[h]])
            T.dma("sp", KTo[h, eb:eb + 64, :], kxo, w=[r_KTo[h]])
            for v in range(2):
                T.dma("sp", QA[h, v, eb:eb + 6, :], qac[h], w=[r_QA[h]])

        kmem = sb("kmem", [128, 2, 2, 256], BF16)
        vmem = sb("vmem", [128, 2, 2, 4, 65], BF16)
        kmT = sb("kmT", [128, 6, NBLK], BF16)

        stA = contextlib.ExitStack()
        es.enter_context(stA)
        NXS = 3
        xt = [sb("xt%d" % i, [128, D], F32, stA) for i in range(NXS)]
        r_xt = [Res() for _ in range(NXS)]
        xs = [sb("xs%d" % i, [128, D], BF16, stA) for i in range(2)]
        r_xs = [Res() for _ in range(2)]
        ssq = [sb("ssq%d" % i, [128, 4], F32, stA) for i in range(NXS)]
        r_ss = [Res() for _ in range(NXS)]
        hT = [sb("hT%d" % i, [128, 8, 512], BF16, stA) for i in range(2)]
        r_hT = [Res() for _ in range(2)]
        st = {"x": 0, "xs": 0, "tb": 0, "ev": 0}
        TB = (0, 1)

        def evac(out, in_, r, w, scale=None):
            st["ev"] += 1
            if st["ev"] % 2 == 0:
                if scale is None:
                    T.op("act", lambda e: e.copy(out=out, in_=in_), r=r, w=w)
                else:
                    T.op("act", lambda e: e.mul(out=out, in_=in_, mul=scale), r=r, w=w)
            else:
                if scale is None:
                    T.op("dve", lambda e: e.tensor_copy(out=out, in_=in_), r=r, w=w)
                else:
                    T.op("dve", lambda e: e.tensor_scalar(out=out, in0=in_, scalar1=scale, scalar2=None,
                                                          op0=ALU.mult), r=r, w=w)

        def norm_T(src, row0, ntiles, hslot):
            for i in range(ntiles):
                s = st["x"] % NXS
                st["x"] += 1
                T.dma("sp", xt[s][:], src[row0 + i * 128: row0 + (i + 1) * 128, :], w=[r_xt[s]])
                T.op("act", lambda e: e.activation(out=junk[:], in_=xt[s][:], func=AF.Square,
                                                   accum_out=ssq[s][:, 0:1]), r=[r_xt[s]], w=[r_ss[s]])
                T.op("act", lambda e: e.activation(out=ssq[s][:, 1:2], in_=ssq[s][:, 0:1], func=AF.Ln,
                                                   bias=epst[:, 0:1], scale=1.0 / D),
                     r=[r_const], w=[r_ss[s]])
                T.op("act", lambda e: e.activation(out=ssq[s][:, 2:3], in_=ssq[s][:, 1:2], func=AF.Exp, scale=-0.5),
                     r=[], w=[r_ss[s]])
                b = st["xs"] % 2
                st["xs"] += 1
                T.op("dve", lambda e: e.tensor_scalar(out=xs[b][:], in0=xt[s][:], scalar1=ssq[s][:, 2:3],
                                                      scalar2=None, op0=ALU.mult),
                     r=[r_xt[s], r_ss[s]], w=[r_xs[b]])
                tb = TB[st["tb"] % 2]
                st["tb"] += 1
                pv = pbf(tb)
                for k in range(8):
                    T.op("pe", lambda e: e.transpose(pv[:, k * 128:(k + 1) * 128], xs[b][:, k * 128:(k + 1) * 128],
                                                     ident[:]),
                         r=[r_xs[b], r_const], w=[bank[tb]])
                evac(hT[hslot][:, :, i * 128:(i + 1) * 128],
                     pv.rearrange("p (k t) -> p k t", k=8), r=[], w=[bank[tb], r_hT[hslot]])

        def run_interleaved(main, side):
            n, m_ = len(main), len(side)
            j = 0
            for i_, f in enumerate(main):
                f()
                while j < m_ and (j + 1) * n <= (i_ + 1) * m_:
                    side[j]()
                    j += 1
            while j < m_:
                side[j]()
                j += 1

        def norm_items(src, row0, ntiles, hslot, tbs):
            slots = []
            for i in range(ntiles):
                slots.append((st["x"] % NXS, st["xs"] % 2, tbs[st["tb"] % len(tbs)]))
                st["x"] += 1
                st["xs"] += 1
                st["tb"] += 1

            def p0(i):
                s = slots[i][0]
                T.dma("sp", xt[s][:], src[row0 + i * 128: row0 + (i + 1) * 128, :], w=[r_xt[s]])

            def p1(i):
                s, b, tb = slots[i]
                T.op("act", lambda e: e.activation(out=junk[:], in_=xt[s][:], func=AF.Square,
                                                   accum_out=ssq[s][:, 0:1]), r=[r_xt[s]], w=[r_ss[s]])
                T.op("act", lambda e: e.activation(out=ssq[s][:, 1:2], in_=ssq[s][:, 0:1], func=AF.Ln,
                                                   bias=epst[:, 0:1], scale=1.0 / D), r=[r_const], w=[r_ss[s]])
                T.op("act", lambda e: e.activation(out=ssq[s][:, 2:3], in_=ssq[s][:, 1:2], func=AF.Exp, scale=-0.5),
                     r=[], w=[r_ss[s]])
                T.op("dve", lambda e: e.tensor_scalar(out=xs[b][:], in0=xt[s][:], scalar1=ssq[s][:, 2:3],
                                                      scalar2=None, op0=ALU.mult),
                     r=[r_xt[s], r_ss[s]], w=[r_xs[b]])

            def p2(i):
                s, b, tb = slots[i]
                pv = pbf(tb)
                for k in range(8):
                    T.op("pe", lambda e: e.transpose(pv[:, k * 128:(k + 1) * 128], xs[b][:, k * 128:(k + 1) * 128],
                                                     ident[:]), r=[r_xs[b], r_const], w=[bank[tb]])
                evac(hT[hslot][:, :, i * 128:(i + 1) * 128],
                     pv.rearrange("p (k t) -> p k t", k=8), r=[], w=[bank[tb], r_hT[hslot]])

            items = []
            for k_ in range(-1, ntiles + 1):
                def f(k_=k_):
                    if 0 <= k_ + 1 < ntiles:
                        p0(k_ + 1)
                    if 0 <= k_ - 1 < ntiles:
                        p2(k_ - 1)
                    if 0 <= k_ < ntiles:
                        p1(k_)
                items.append(f)
            return items

        wA = sb("wA", [128, 8, WA], BF16, stA)
        r_wA = Res()
        r_wM = Res()
        r_kmT = Res()
        kms = sb("kms", [128, 2], F32, stA)
        r_kms = Res()
        kst = [sb("kst%d" % i, [128, 512], BF16, stA) for i in range(3)]
        r_kst = [Res() for _ in range(3)]
        vst = [sb("vst%d" % i, [128, H, 4, 65], BF16, stA) for i in range(2)]
        r_vst = [Res() for _ in range(2)]
        bmask = sb("bmask", [128, NLT, NBLK], F32, stA)
        Et = [sb("Et%d" % i, [128, H, 2, 128], BF16, stA) for i in range(2)]
        r_Et = [Res(), Res()]
        gm = sb("gm", [128, 6, NBLK], F32, stA)
        r_gm = Res()
        m8 = sb("m8", [128, 6, 8], F32, stA)
        r_m8 = [Res() for _ in range(6)]
        thr = sb("thr", [128, 6], F32, stA)
        selb = sb("selb", [128, 6, NBLK], F32, stA)
        qmst = sb("qmst", [128, 2, 512], BF16, stA)
        r_qmst = Res()
        esb = [sb("esb%d" % i, [128, D], F32, stA) for i in range(2)]
        r_esb = [Res(), Res()]
        gts = [sb("gts%d" % i, [128, D], F32, stA) for i in range(2)]
        r_gts = [Res(), Res()]
        qa_sts = [sb("qa_st0", [128, H, 2, 512], BF16, stA), None]
        r_qasts = [Res(), Res()]
        stW0 = contextlib.ExitStack()
        stA.enter_context(stW0)
        wM = sb("wM", [128, 2, 8, 512], BF16, stW0)
        wst = [sb("wst%d" % i, [128, 1792], F32, stW0) for i in range(2)]
        r_wst = [Res(), Res()]
        stw = {"i": 0}

        def wload(dst, src, ncols, gcol, rdst):
            for c0 in range(0, ncols, 1792):
                n = min(1792, ncols - c0)
                s = stw["i"] % 2
                stw["i"] += 1
                T.dma("sp", wst[s][:, 0:n], src[:, c0:c0 + n], w=[r_wst[s]])
                if s == 0:
                    if gcol is None:
                        T.op("dve", lambda e: e.tensor_copy(out=dst[:, c0:c0 + n], in_=wst[s][:, 0:n]),
                             r=[r_wst[s]], w=[rdst])
                    else:
                        T.op("dve", lambda e: e.tensor_scalar(out=dst[:, c0:c0 + n], in0=wst[s][:, 0:n],
                                                              scalar1=gcol, scalar2=None, op0=ALU.mult),
                             r=[r_wst[s], r_const], w=[rdst])
                else:
                    if gcol is None:
                        T.op("act", lambda e: e.copy(out=dst[:, c0:c0 + n], in_=wst[s][:, 0:n]),
                             r=[r_wst[s]], w=[rdst])
                    else:
                        T.op("act", lambda e: e.activation(out=dst[:, c0:c0 + n], in_=wst[s][:, 0:n], func=AF.Copy,
                                                           scale=gcol),
                             r=[r_wst[s], r_const], w=[rdst])

        for L in range(2):
            for k in range(8):
                wload(wM[:, L, k, :], w_mem[L, k * 128:(k + 1) * 128, :], 512, gc[:, 16 + k:17 + k], r_wM)
        for k in range(8):
            wload(wA[:, k, :], w_in_a[k * 128:(k + 1) * 128, :], WA, gc[:, k:k + 1], r_wA)
        T.dma("sp", bmask[:].rearrange("p a b -> p (a b)"), bmask_d, w=[r_const])
        for i in range(2):
            T.op("dve", lambda e: e.memset(vst[i][:].rearrange("p a b c -> p (a b c)"), 1.0), w=[r_vst[i]])
            T.op("dve", lambda e: e.memset(Et[i][:].rearrange("p a b c -> p (a b c)"), 0.0), w=[r_Et[i]])

        r_kmem = Res()
        r_vmem = Res()
        T.op("dve", lambda e: e.memset(vmem[:].rearrange("p a b c d -> p (a b c d)"), 1.0), w=[r_vmem])
        norm_T(memb, 0, 2, 0)
        for L in range(2):
            for pr in range(2):
                bk = 2 + (L * 2 + pr) % 2
                for k in range(8):
                    T.op("pe", lambda e: e.matmul(pf(bk, 256), lhsT=wM[:, L, k, pr * 128:(pr + 1) * 128],
                                                  rhs=hT[0][:, k, 0:256], start=(k == 0), stop=(k == 7)),
                         r=[r_wM, r_hT[0]], w=[bank[bk]])
                evac(kmem[:, L, pr, :], pf(bk, 256), r=[], w=[bank[bk], r_kmem])
            for mt in range(2):
                bk = 4 + mt
                for k in range(8):
                    T.op("pe", lambda e: e.matmul(pf(bk, 256), lhsT=hT[0][:, k, mt * 128:(mt + 1) * 128],
                                                  rhs=wM[:, L, k, 256:512], start=(k == 0), stop=(k == 7)),
                         r=[r_wM, r_hT[0]], w=[bank[bk]])
                evac(vmem[:, L, mt, :, 0:64], pf(bk, 256).rearrange("p (h c) -> p h c", h=4),
                     r=[], w=[bank[bk], r_vmem])
        T.barrier()
        stW0.close()
        qa_sts[1] = sb("qa_st1", [128, H, 2, 512], BF16, stA)

        sk = {"k": 0, "pb": 0, "PB": (2, 3, 4, 5, 6, 7)}

        def nextbank():
            PB = sk["PB"]
            b = PB[sk["pb"] % len(PB)]
            sk["pb"] += 1
            return b

        def proj_items(hslot, ktdst, r_ktdst, vpdst, r_vpdst, col0, gi, do_kmean):
            items = []

            def kpair(p):
                bk = nextbank()
                for k in range(8):
                    T.op("pe", lambda e: e.matmul(pf(bk), lhsT=wA[:, k, 768 + p * 128: 768 + (p + 1) * 128],
                                                  rhs=hT[hslot][:, k, :], start=(k == 0), stop=(k == 7)),
                         r=[r_wA, r_hT[hslot]], w=[bank[bk]])
                s = sk["k"] % 3
                sk["k"] += 1
                if do_kmean:
                    T.op("dve", lambda e: e.tensor_reduce(out=kms[:], in_=pf(bk).rearrange("p (b t) -> p b t", b=2),
                                                          axis=AX.X, op=ALU.add),
                         r=[], w=[bank[bk], r_kms])
                    T.op("dve", lambda e: e.tensor_scalar(out=kmT[:, p, 2 * (gi // 4): 2 * (gi // 4) + 2], in0=kms[:],
                                                          scalar1=1.0 / 256.0, scalar2=None, op0=ALU.mult),
                         r=[r_kms], w=[r_kmT])
                evac(kst[s][:], pf(bk), r=[], w=[bank[bk], r_kst[s]])
                if not _DBG.get("nokst"):
                    T.dma("pool", ktdst[2 * p, 0:64, col0:col0 + 512], kst[s][0:64, :], r=[r_kst[s]], w=[r_ktdst[2 * p]])
                    T.dma("pool", ktdst[2 * p + 1, 64:128, col0:col0 + 512], kst[s][64:128, :], r=[r_kst[s]],
                          w=[r_ktdst[2 * p + 1]])

            vs = (gi // 4) % 2

            def vgrp(i, c0, n, h0, last):
                bk = nextbank()
                for k in range(8):
                    T.op("pe", lambda e: e.matmul(pf(bk, n), lhsT=hT[hslot][:, k, i * 128:(i + 1) * 128],
                                                  rhs=wA[:, k, 1536 + c0: 1536 + c0 + n],
                                                  start=(k == 0), stop=(k == 7)),
                         r=[r_wA, r_hT[hslot]], w=[bank[bk]])
                evac(vst[vs][:, h0:h0 + n // 64, i, 0:64], pf(bk, n).rearrange("p (h c) -> p h c", c=64),
                     r=[], w=[bank[bk], r_vst[vs]])
                if last and not _DBG.get("novst"):
                    T.dma("pool", vpdst[:, :, gi:gi + 4, :].rearrange("h p t c -> p h (t c)"),
                          vst[vs][:].rearrange("p h t c -> p h (t c)"), r=[r_vst[vs]], w=r_vpdst)

            for p in range(6):
                items.append(lambda p=p: kpair(p))
            for i in range(4):
                items.append(lambda i=i: vgrp(i, 0, 512, 0, False))
                items.append(lambda i=i: vgrp(i, 512, 256, 8, i == 3))
            return items

        r_VP = [Res("VP")]
        r_VPo = [Res("VPo")]
        NG1 = NTG // 4
        if stop_after == "A1s":
            NG1 = 2
        for f in norm_items(xg, 0, 4, 0, TB):
            f()
        for G in range(NG1):
            main = proj_items(G % 2, KT, r_KT, VP, r_VP, G * 512, G * 4, True)
            side = norm_items(xg, (G + 1) * 512, 4, (G + 1) % 2, TB) if G + 1 < NG1 else []
            run_interleaved(main, side)
        if KMD is not None:
            T.dma("sp", KMD, kmT[:].rearrange("p a b -> p (a b)"), r=[r_kmT])
        if stop_after in ("A1", "A1s"):
            T.barrier()
            return nc

        r_QM = Res()
        r_GT = Res()
        NG2 = NLT // 4
        if stop_after == "A2s":
            NG2 = 1
        cnt2 = {"e": 0, "z": 0}
        sk["PB"] = (5, 6, 7)
        TB2 = (0,)

        def a2_main_items(G):
            hs = G % 2
            qa_st = qa_sts[G % 2]
            r_qast = r_qasts[G % 2]
            items = proj_items(hs, KTo, r_KTo, VPo, r_VPo, G * 512, G * 4, False)

            def qpair(p):
                bk = nextbank()
                for k in range(8):
                    T.op("pe", lambda e: e.matmul(pf(bk), lhsT=wA[:, k, p * 128:(p + 1) * 128],
                                                  rhs=hT[hs][:, k, :], start=(k == 0), stop=(k == 7)),
                         r=[r_wA, r_hT[hs]], w=[bank[bk]])
                for e_ in range(2):
                    rows = slice(e_ * 64, (e_ + 1) * 64)
                    for v in range(2):
                        evac(qa_st[rows, 2 * p + e_, v, :], pbig[rows, bk * 512:(bk + 1) * 512], r=[],
                             w=[bank[bk], r_qast], scale=0.125)

            def qmpair(p):
                bk = nextbank()
                for k in range(8):
                    T.op("pe", lambda e: e.matmul(pf(bk), lhsT=wA[:, k, 2304 + p * 128: 2304 + (p + 1) * 128],
                                                  rhs=hT[hs][:, k, :], start=(k == 0), stop=(k == 7)),
                         r=[r_wA, r_hT[hs]], w=[bank[bk]])
                evac(qmst[:, p, :], pf(bk), r=[], w=[bank[bk], r_qmst], scale=0.125)
                if p == 1:
                    T.dma("pool", QM[:, :, G * 512:(G + 1) * 512].rearrange("a p c -> p a c"), qmst[:],
                          r=[r_qmst], w=[r_QM])

            def ztile(i):
                zb = 6
                for half in range(2):
                    for k in range(8):
                        T.op("pe", lambda e: e.matmul(pf(zb + half), lhsT=hT[hs][:, k, i * 128:(i + 1) * 128],
                                                      rhs=wA[:, k, 2560 + half * 512: 2560 + (half + 1) * 512],
                                                      start=(k == 0), stop=(k == 7)),
                             r=[r_wA, r_hT[hs]], w=[bank[zb + half]])
                zs = cnt2["z"] % 2
                cnt2["z"] += 1
                zps = pbig[:, zb * 512:(zb + 2) * 512]
                T.op("act", lambda e: e.activation(out=esb[zs][:], in_=zps, func=AF.Exp, scale=-1.0),
                     r=[], w=[bank[zb], bank[zb + 1], r_esb[zs]])
                T.op("act", lambda e: e.copy(out=gts[zs][:], in_=zps), r=[], w=[bank[zb], bank[zb + 1], r_gts[zs]])
                T.op("act", lambda e: e.activation(out=esb[zs][:], in_=esb[zs][:], func=AF.Ln, bias=1.0),
                     r=[], w=[r_esb[zs]])
                T.op("act", lambda e: e.activation(out=esb[zs][:], in_=esb[zs][:], func=AF.Exp, scale=-1.0),
                     r=[], w=[r_esb[zs]])
                T.op("dve", lambda e: e.tensor_tensor(out=gts[zs][:], in0=gts[zs][:], in1=esb[zs][:], op=ALU.mult),
                     r=[r_esb[zs]], w=[r_gts[zs]])
                lt = G * 4 + i
                T.dma("pool", GT[lt * 128:(lt + 1) * 128, :], gts[zs][:], r=[r_gts[zs]], w=[r_GT])

            for p in range(6):
                items.append(lambda p=p: qpair(p))
            for p in range(2):
                items.append(lambda p=p: qmpair(p))
            for i in range(4):
                items.append(lambda i=i: ztile(i))
            return items

        def a2_gate_items(G):
            qa_st = qa_sts[G % 2]
            r_qast = r_qasts[G % 2]
            items = []

            def gate(i, e_):
                lt = G * 4 + i
                E = Et[i % 2]
                r_E = r_Et[i % 2]
                gb = 1 + e_
                rows = slice(e_ * 64, (e_ + 1) * 64)
                for p in range(6):
                    T.op("pe", lambda e: e.matmul(pf(gb, 64, p * 64), lhsT=qa_st[rows, 2 * p + e_, 0, i * 128:(i + 1) * 128],
                                                  rhs=kmT[rows, p, :], start=True, stop=True),
                         r=[r_qast, r_kmT], w=[bank[gb]])
                T.op("dve", lambda e: e.tensor_tensor(out=gm[:], in0=pf(gb, 384).rearrange("p (a b) -> p a b", a=6),
                                                      in1=bmask[:, lt:lt + 1, :].to_broadcast([128, 6, NBLK]),
                                                      op=ALU.add),
                     r=[r_const], w=[bank[gb], r_gm])
                for p in range(6):
                    T.op("dve", lambda e: e.max(out=m8[:, p, :], in_=gm[:, p, :]), r=[r_gm], w=[r_m8[p]])
                T.op("dve", lambda e: e.tensor_scalar(out=thr[:], in0=m8[:, :, 2], scalar1=-1e29, scalar2=None,
                                                      op0=ALU.max), r=r_m8, w=[r_gm])
                T.op("dve", lambda e: e.tensor_tensor(out=selb[:], in0=gm[:],
                                                      in1=thr[:].unsqueeze(2).to_broadcast([128, 6, NBLK]),
                                                      op=ALU.is_ge), r=[], w=[r_gm])
                cb = (64 if e_ == 0 else 0) + 6
                for v in range(2):
                    T.op("dve", lambda e: e.tensor_scalar(out=E[:, e_::2, v, cb:cb + 32],
                                                          in0=selb[:, :, v * 32:(v + 1) * 32],
                                                          scalar1=1.0, scalar2=BIG, op0=ALU.subtract, op1=ALU.mult),
                         r=[], w=[r_gm, r_E])

            def etr(i, e_):
                E = Et[i % 2]
                r_E = r_Et[i % 2]
                ext = slice(64, 128) if e_ == 0 else slice(0, 64)
                for hb in range(2):
                    tbk = 3 + hb
                    pv = pbf(tbk)
                    for j in range(3):
                        h = e_ + 2 * (3 * hb + j)
                        for v in range(2):
                            sl_ = (j * 2 + v) * 128
                            T.op("pe", lambda e: e.transpose(pv[:, sl_:sl_ + 128], E[:, h, v, :], ident[:]),
                                 r=[r_E, r_const], w=[bank[tbk]])
                    h0 = e_ + 6 * hb
                    evac(qa_st[ext, h0:h0 + 5:2, :, i * 128:(i + 1) * 128],
                         pv[ext, 0:768].rearrange("p (a b t) -> p a b t", a=3, b=2),
                         r=[], w=[bank[tbk], r_qast])

            def store():
                cols = slice(G * 512, (G + 1) * 512)
                for e_ in range(2):
                    dh = slice(0, 64) if e_ == 0 else slice(64, 128)
                    ex = slice(70, 128) if e_ == 0 else slice(6, 64)
                    rq = [r_QA[h] for h in range(e_, H, 2)]
                    for v in range(2):
                        T.dma("sp", QA[e_::2, v, dh, cols].rearrange("h r c -> r h c"), qa_st[dh, e_::2, v, :],
                              r=[r_qast], w=rq)
                        T.dma("sp", QA[e_::2, v, ex, cols].rearrange("h r c -> r h c"), qa_st[ex, e_::2, v, :],
                              r=[r_qast], w=rq)

            g_ = [[(lambda i=i, e_=e_: gate(i, e_)) for e_ in range(2)] for i in range(4)]
            t_ = [[(lambda i=i, e_=e_: etr(i, e_)) for e_ in range(2)] for i in range(4)]
            items = g_[0] + g_[1] + t_[0] + g_[2] + t_[1] + g_[3] + t_[2] + t_[3] + [store]
            return items

        def merge(a, b):
            out = []
            n, m_ = len(a), len(b)
            j = 0
            for i_, f in enumerate(a):
                out.append(f)
                while j < m_ and (j + 1) * n <= (i_ + 1) * m_:
                    out.append(b[j])
                    j += 1
            out.extend(b[j:])
            return out

        for f in norm_items(xm, 0, 4, 0, TB2):
            f()
        for G in range(NG2 + 1):
            main = a2_main_items(G) if G < NG2 else []
            side = []
            if G >= 1:
                side = a2_gate_items(G - 1)
            if G + 1 < NG2:
                side = merge(side, norm_items(xm, (G + 1) * 512, 4, (G + 1) % 2, TB2)) if side else \
                    norm_items(xm, (G + 1) * 512, 4, (G + 1) % 2, TB2)
            if main:
                run_interleaved(main, side)
            else:
                for f in side:
                    f()
        if stop_after in ("A2", "A2s"):
            T.barrier()
            return nc

        T.barrier()
        stA.close()
        stY = contextlib.ExitStack()
        es.enter_context(stY)
        Y = sb("Y", [128, NQT, D], BF16, stY)
        r_Y = Res("Y")
        stB = contextlib.ExitStack()
        es.enter_context(stB)
        cmt = sb("cmt", [128, 2, 256], BF16, stB)
        T.dma("sp", cmt[:].rearrange("p a b -> p (a b)"), cm_d, w=[r_const])
        QAs = [sb("QAs%d" % i, [128, 2, NLOC], BF16, stB) for i in range(2)]
        r_QAs = [Res(), Res()]
        KTos = [sb("KTos%d" % i, [128, NLOC], BF16, stB) for i in range(2)]
        VPos = [sb("VPos%d" % i, [128, NLT, 65], BF16, stB) for i in range(2)]
        r_own = [Res(), Res()]
        NKS = 3
        kring = [sb("kring%d" % i, [128, 1024], BF16, stB) for i in range(NKS)]
        vring = [sb("vring%d" % i, [128, 8, 65], BF16, stB) for i in range(NKS)]
        r_ring = [Res() for _ in range(NKS)]
        NPT = 3
        pT = [sb("pT%d" % i, [128, 2, 384], BF16, stB) for i in range(NPT)]
        r_pT = [Res() for _ in range(NPT)]
        rec = sb("rec", [128, 9], F32, stB)
        r_rec = Res()
        qmT = sb("qmT", [128, 2, NLOC], BF16, stB)
        r_qmT = Res()
        T.dma("sp", qmT[:], QM.rearrange("a p c -> p a c"), r=[r_QM], w=[r_qmT])

        heads = list(range(H))
        units = list(range(NU))
        if stop_after == "B1s":
            heads = [0, 1, 7]
            units = [0, 1]

        def load_head(h, hb):
            for v in range(2):
                T.dma("sp", QAs[hb][:, v, :], QA[h, v], r=[r_QA[h]], w=[r_QAs[hb]])
            T.dma("sp", KTos[hb][:], KTo[h], r=[r_KTo[h]], w=[r_own[hb]])
            T.dma("sp", VPos[hb][:], VPo[h], r=r_VPo, w=[r_own[hb]])

        chunks = []
        for h in heads:
            for m in units:
                nb = 16 * m + 15
                for c in range((nb + 3) // 4):
                    chunks.append((h, m, c))
        cstate = {"next": 0}

        def ensure_loaded(ci):
            while cstate["next"] <= min(ci, len(chunks) - 1):
                n = cstate["next"]
                h, m, c = chunks[n]
                sl = n % NKS
                T.dma("sp", kring[sl][:], KT[h, :, c * 1024:(c + 1) * 1024], r=[r_KT[h]], w=[r_ring[sl]])
                T.dma("sp", vring[sl][:], VP[h, :, c * 8:(c + 1) * 8, :], r=r_VP, w=[r_ring[sl]])
                cstate["next"] += 1

        sst = {"s": 0}
        LAG = 2
        ci = 0
        for hi, h in enumerate(heads):
            hb = hi % 2
            if hi == 0:
                load_head(h, 0)
            if hi + 1 < len(heads):
                load_head(heads[hi + 1], (hi + 1) % 2)
            for m in units:
                nb = 16 * m + 15
                cnts = (16 * m + 12, 16 * m + 14, 16 * m + 15)
                steps = []
                for c in range((nb + 3) // 4):
                    for j in range(4 * c, min(4 * c + 4, nb)):
                        for g in range(3):
                            if j < cnts[g]:
                                steps.append(("past", j, g, ci + c))
                for qb in range(5):
                    steps.append(("own", qb, 0, -1))
                first = {6: True, 7: True}
                nst = len(steps)
                info = [None] * nst
                for idx in range(nst + LAG):
                    if idx < nst:
                        kind, a, g, cidx = steps[idx]
                        sbuf_i = sst["s"] % 3
                        sst["s"] += 1
                        b0 = 2 * sbuf_i
                        if kind == "past":
                            j = a
                            ensure_loaded(cidx + 1)
                            sl = cidx % NKS
                            v = j // 32
                            qc = (UT * m + 1 + 3 * g) * 128
                            N = 384
                            for kt2 in range(2):
                                kc = (j % 4) * 256 + kt2 * 128
                                T.op("pe", lambda e: e.matmul(pf(b0 + kt2, N), lhsT=kring[sl][:, kc:kc + 128],
                                                              rhs=QAs[hb][:, v, qc:qc + N], start=True, stop=True),
                                     r=[r_ring[sl], r_QAs[hb]], w=[bank[b0 + kt2]])
                            tqs = [3 * g + i_ for i_ in range(3)]
                            vsrc = [(vring[sl], (j % 4) * 2 + kt2) for kt2 in range(2)]
                            rres = [r_ring[sl]]
                        else:
                            qb = a
                            if qb == 0:
                                qc, N, c0 = (UT * m + 1) * 128, 128, 128
                                tqs = [0]
                            else:
                                qc, N, c0 = (UT * m + 2 * qb) * 128, 256, 0
                                tqs = [2 * qb - 1, 2 * qb]
                            for kt2 in range(2):
                                kc = (UT * m + 2 * qb + kt2) * 128
                                T.op("pe", lambda e: e.matmul(pf(b0 + kt2, N), lhsT=KTos[hb][:, kc:kc + 128],
                                                              rhs=QAs[hb][:, 0, qc:qc + N], start=True, stop=False),
                                     r=[r_own[hb], r_QAs[hb]], w=[bank[b0 + kt2]])
                                T.op("pe", lambda e: e.matmul(pf(b0 + kt2, N), lhsT=ident[:],
                                                              rhs=cmt[:, kt2, c0:c0 + N], start=False, stop=True),
                                     r=[r_const], w=[bank[b0 + kt2]])
                            vsrc = [(VPos[hb], UT * m + 2 * qb + kt2) for kt2 in range(2)]
                            rres = [r_own[hb]]
                        ps = sst["s"] % NPT
                        sview = pbig[:, b0 * 512:(b0 + 2) * 512].rearrange("p (a b) -> p a b", a=2)[:, :, 0:N]
                        T.op("act", lambda e: e.activation(out=pT[ps][:, :, 0:N], in_=sview, func=AF.Exp,
                                                           bias=abias[:, h:h + 1], scale=1.0),
                             r=[r_const], w=[bank[b0], bank[b0 + 1], r_pT[ps]])
                        info[idx] = (ps, tqs, vsrc, rres)
                    k_ = idx - LAG
                    if k_ >= 0:
                        ps, tqs, vsrc, rres = info[k_]
                        for qi_, tq in enumerate(tqs):
                            bk = 6 if tq < 5 else 7
                            col = (tq if tq < 5 else tq - 5) * 65
                            for kt2 in range(2):
                                vt, vi = vsrc[kt2]
                                fl = first[bk]
                                first[bk] = False
                                T.op("pe", lambda e: e.matmul(pf(bk, 65, col), lhsT=pT[ps][:, kt2, qi_ * 128:(qi_ + 1) * 128],
                                                              rhs=vt[:, vi, :], start=fl, stop=True,
                                                              skip_group_check=True),
                                     r=[r_pT[ps]] + rres, w=[bank[bk]])
                ci += (nb + 3) // 4
                for bk, nt, t0_ in ((6, 5, 0), (7, 4, 5)):
                    av = pf(bk, nt * 65).rearrange("p (a b) -> p a b", b=65)
                    T.op("dve", lambda e: e.reciprocal(out=rec[:, 0:nt], in_=av[:, :, 64]), r=[], w=[bank[bk], r_rec])
                    T.op("dve", lambda e: e.tensor_tensor(out=Y[:, 9 * m + t0_: 9 * m + t0_ + nt, h * 64:(h + 1) * 64],
                                                          in0=av[:, :, 0:64],
                                                          in1=rec[:, 0:nt].unsqueeze(2).to_broadcast([128, nt, 64]),
                                                          op=ALU.mult),
                         r=[r_rec], w=[bank[bk], r_Y])
        if YD is not None:
            T.dma("sp", YD, Y[:].rearrange("p a b -> p (a b)"), r=[r_Y])
        if stop_after in ("B1", "B1s"):
            T.barrier()
            return nc

        def mem_attn(L, qsrc, r_qsrc, qcols, dst_fn, r_dst, ntile, sbanks=(0, 2, 4), abanks=(6, 7), heads_=range(4)):
            N = ntile * 128
            for hm in heads_:
                pr, e_ = divmod(hm, 2)
                rows = slice(e_ * 64, (e_ + 1) * 64)
                sbuf_i = sst["s"] % len(sbanks)
                sst["s"] += 1
                b0 = sbanks[sbuf_i]
                for kt2 in range(2):
                    T.op("pe", lambda e: e.matmul(pf(b0 + kt2, N), lhsT=kmem[rows, L, pr, kt2 * 128:(kt2 + 1) * 128],
                                                  rhs=qsrc[rows, pr, qcols:qcols + N], start=True, stop=True),
                         r=[r_kmem, r_qsrc], w=[bank[b0 + kt2]])
                ps = sst["s"] % len(pT)
                sview = pbig[:, b0 * 512:(b0 + 2) * 512].rearrange("p (a b) -> p a b", a=2)[:, :, 0:N]
                T.op("act", lambda e: e.activation(out=pT[ps][:, :, 0:N], in_=sview, func=AF.Exp),
                     r=[], w=[bank[b0], bank[b0 + 1], r_pT[ps]])
                bk = abanks[hm % len(abanks)]
                fl = True
                for qt in range(ntile):
                    for kt2 in range(2):
                        T.op("pe", lambda e: e.matmul(pf(bk, 65, qt * 65), lhsT=pT[ps][:, kt2, qt * 128:(qt + 1) * 128],
                                                      rhs=vmem[:, L, kt2, hm, :], start=fl, stop=True,
                                                      skip_group_check=True),
                             r=[r_pT[ps], r_vmem], w=[bank[bk]])
                        fl = False
                av = pf(bk, ntile * 65).rearrange("p (a b) -> p a b", b=65)
                T.op("dve", lambda e: e.reciprocal(out=rec[:, 0:ntile], in_=av[:, :, 64]), r=[], w=[bank[bk], r_rec])
                T.op("dve", lambda e: e.tensor_tensor(out=dst_fn(hm), in0=av[:, :, 0:64],
                                                      in1=rec[:, 0:ntile].unsqueeze(2).to_broadcast([128, ntile, 64]),
                                                      op=ALU.mult),
                     r=[r_rec], w=[bank[bk], r_dst])

        for m in units:
            for g in range(3):
                q0 = 9 * m + 3 * g
                mem_attn(0, qmT, r_qmT, (UT * m + 1 + 3 * g) * 128,
                         lambda hm: Y[:, q0:q0 + 3, 768 + hm * 64: 768 + (hm + 1) * 64], r_Y, 3)
        if YD is not None:
            T.dma("sp", YD, Y[:].rearrange("p a b -> p (a b)"), r=[r_Y])
        r_YS = Res()
        T.dma("pool", YS, Y[:].rearrange("p a b -> p (a b)"), r=[r_Y], w=[r_YS])
        if stop_after in ("B2",):
            T.barrier()
            return nc

        T.barrier()
        stB.close()
        stY.close()
        stC = contextlib.ExitStack()
        es.enter_context(stC)
        wO0 = sb("wO0", [128, 8, D], BF16, stC)
        wO1 = sb("wO1", [128, 8, D], BF16, stC)
        wB = sb("wB", [128, 8, 2432], BF16, stC)
        r_wC = Res()
        stW = contextlib.ExitStack()
        stC.enter_context(stW)
        wst2 = [sb("wst2_%d" % i, [128, 1792], F32, stW) for i in range(2)]
        wst[0], wst[1] = wst2[0], wst2[1]
        r_wst[0], r_wst[1] = Res(), Res()
        for k in range(8):
            rows = slice(k * 128, (k + 1) * 128)
            wload(wO0[:, k, :], w_out[0, rows, :], D, None, r_wC)
            wload(wO1[:, k, :], w_out[1, rows, :], D, None, r_wC)
            g1c = gc[:, 8 + k: 9 + k]
            wload(wB[:, k, 0:768], w_in_b[rows, 0:768], 768, g1c, r_wC)
            for gk in range(2):
                for dup in range(2):
                    c0 = 768 + gk * 128 + dup * 64
                    wload(wB[:, k, c0:c0 + 64], w_in_b[rows, 768 + gk * 64: 768 + (gk + 1) * 64], 64, g1c, r_wC)
            wload(wB[:, k, 1024:2432], w_in_b[rows, 896:2304], 1408, g1c, r_wC)
        T.barrier()
        stW.close()
        swac = sb("swac", [128, 2, H, 128], F32, stC)
        fgt = sb("fgt", [128, D], F32, stC)
        esink = sb("esink", [128, H], F32, stC)
        halob = sb("halob", [128, NU], F32, stC)
        zero1 = sb("zero1", [128, 1], F32, stC)
        T.dma("sp", swac[:].rearrange("p a b c -> p (a b c)"), swac_d, w=[r_const])
        T.dma("sp", fgt[:], fgr, w=[r_const])
        T.dma("sp", halob[:], halob_d, w=[r_const])
        T.dma("sp", esink[:], sinkr, w=[r_const])
        T.op("act", lambda e: e.activation(out=esink[:], in_=esink[:], func=AF.Exp), r=[], w=[r_const])
        T.op("dve", lambda e: e.memset(zero1[:], 0.0), w=[r_const])

        yt = [sb("yt%d" % i, [128, D], BF16, stC) for i in range(2)]
        r_yt = [Res(), Res()]
        gtt = [sb("gtt0", [128, D], F32, stC)] * 2
        r_gtt = [Res()] * 2
        x1g = [sb("x1g%d" % i, [128, 3, D], F32, stC) for i in range(2)]
        r_x1g = [[Res() for _ in range(3)] for _ in range(2)]
        yg = [sb("yg%d" % i, [128, D], BF16, stC) for i in range(2)]
        r_yg = [Res(), Res()]
        ygT = [sb("ygT%d" % i, [128, 8, 128], BF16, stC) for i in range(2)]
        r_ygT = [Res(), Res()]
        ssc = [sb("ssc%d" % i, [128, 4], F32, stC) for i in range(4)]
        r_ssc = [Res() for _ in range(4)]
        xs1 = [sb("xs1_%d" % i, [128, D], BF16, stC) for i in range(2)]
        r_xs1 = [Res(), Res()]
        h1T = [sb("h1T%d" % i, [128, 8, 384], BF16, stC) for i in range(2)]
        r_h1T = [Res(), Res()]
        q1T = [sb("q1T%d" % i, [128, 6, 384], BF16, stC) for i in range(2)]
        r_q1T = [Res(), Res()]
        qm1T = [sb("qm1T%d" % i, [128, 2, 384], BF16, stC) for i in range(2)]
        r_qm1T = [Res(), Res()]
        k1T = [sb("k1T%d" % i, [128, 2, 9 * 128], BF16, stC) for i in range(2)]
        v1 = [sb("v1_%d" % i, [128, 9, 2, 65], BF16, stC) for i in range(2)]
        r_kv1 = [Res(), Res()]
        for i in range(2):
            T.op("dve", lambda e: e.memset(v1[i][:].rearrange("p a b c -> p (a b c)"), 1.0), w=[r_kv1[i]])
        y1 = [sb("y1_0", [128, 3, D], BF16, stC)] * 2
        r_y1 = [Res()] * 2
        sbs = [sb("sbs%d" % i, [128, 6, 128], F32, stC) for i in range(2)]
        r_sbs = [Res(), Res()]
        p1T = [sb("p1T%d" % i, [128, 6, 128], BF16, stC) for i in range(2)]
        r_p1T = [Res(), Res()]
        den = sb("den", [128, 6], F32, stC)
        r_den = Res()
        es1 = [sb("es1_0", [128, D], F32, stC)] * 2
        r_es1 = [Res()] * 2
        ot = [sb("ot0", [128, D], F32, stC)] * 2
        r_ot = [Res()] * 2
        pT = [sb("pTc%d" % i, [128, 2, 384], BF16, stC) for i in range(2)]
        r_pT = [Res() for _ in range(2)]
        yg2 = sb("yg2", [128, D], BF16, stC)
        r_yg2 = Res()
        ygT2 = sb("ygT2", [128, 8, 128], BF16, stC)
        r_ygT2 = Res()
        rec = sb("recc", [128, 9], F32, stC)
        r_rec = Res()
        r_out = Res()
        cc = {"t": 0, "n": 0, "pj": 0, "sw": 0, "z": 0, "o": 0}
        TBK, ABK = 0, 1

        def rms_scale(src, r_src, dst_bf, r_dst):
            s_ = cc["n"] % 4
            cc["n"] += 1
            T.op("act", lambda e: e.activation(out=junk[:], in_=src, func=AF.Square, accum_out=ssc[s_][:, 0:1]),
                 r=[r_src], w=[r_ssc[s_]])
            T.op("act", lambda e: e.activation(out=ssc[s_][:, 1:2], in_=ssc[s_][:, 0:1], func=AF.Ln,
                                               bias=epst[:, 0:1], scale=1.0 / D), r=[r_const], w=[r_ssc[s_]])
            T.op("act", lambda e: e.activation(out=ssc[s_][:, 2:3], in_=ssc[s_][:, 1:2], func=AF.Exp, scale=-0.5),
                 r=[], w=[r_ssc[s_]])
            if dst_bf is not None:
                T.op("dve", lambda e: e.tensor_scalar(out=dst_bf, in0=src, scalar1=ssc[s_][:, 2:3], scalar2=None,
                                                      op0=ALU.mult), r=[r_src, r_ssc[s_]], w=[r_dst])
            return s_

        def transpose8(src_bf, r_src, dst, r_dstT):
            pv = pbf(TBK)
            for k in range(8):
                T.op("pe", lambda e: e.transpose(pv[:, k * 128:(k + 1) * 128], src_bf[:, k * 128:(k + 1) * 128], ident[:]),
                     r=[r_src, r_const], w=[bank[TBK]])
            evac(dst, pv.rearrange("p (k t) -> p k t", k=8), r=[], w=[bank[TBK], r_dstT])

        def outproj(srcT, r_srcT, w_, xres, r_xres):
            for half in range(2):
                for k in range(8):
                    T.op("pe", lambda e: e.matmul(pf(2 + half), lhsT=srcT[:, k, :], rhs=w_[:, k, half * 512:(half + 1) * 512],
                                                  start=(k == 0), stop=(k == 7)),
                         r=[r_srcT, r_wC], w=[bank[2 + half]])
            T.op("dve", lambda e: e.tensor_tensor(out=xres, in0=pbig[:, 1024:2048], in1=xres, op=ALU.add),
                 r=[], w=[bank[2], bank[3], r_xres])

        def featproj(hT_, r_hT_, c0, dst, r_dst, scale, N=384):
            bk = 4 + cc["pj"] % 2
            cc["pj"] += 1
            for k in range(8):
                T.op("pe", lambda e: e.matmul(pf(bk, N), lhsT=wB[:, k, c0:c0 + 128], rhs=hT_[:, k, 0:N],
                                              start=(k == 0), stop=(k == 7)),
                     r=[r_wC, r_hT_], w=[bank[bk]])
            evac(dst, pf(bk, N), r=[], w=[bank[bk], r_dst], scale=scale)

        groups = [(m, g) for m in units for g in range(3)]

        def stageP(m, g, gb):
            ub = m % 2
            u0 = 1 + 3 * g
            items = []

            def t_a(i):
                u = u0 + i
                lt = UT * m + u
                qi = 9 * m + u - 1
                ys = i % 2
                T.dma("sp", yt[ys][:], YS[:, qi * D:(qi + 1) * D], r=[r_YS], w=[r_yt[ys]])
                T.dma("sp", gtt[ys][:], GT[lt * 128:(lt + 1) * 128, :], r=[r_GT], w=[r_gtt[ys]])
                T.dma("sp", x1g[gb][:, i, :], xm[lt * 128:(lt + 1) * 128, :], w=[r_x1g[gb][i]])
                T.op("dve", lambda e: e.tensor_tensor(out=yg[ys][:], in0=yt[ys][:], in1=gtt[ys][:], op=ALU.mult),
                     r=[r_yt[ys], r_gtt[ys]], w=[r_yg[ys]])

            def t_b(i):
                ys = i % 2
                transpose8(yg[ys], r_yg[ys], ygT[ys][:], r_ygT[ys])
                outproj(ygT[ys], r_ygT[ys], wO0, x1g[gb][:, i, :], r_x1g[gb][i])

            def t_c(i):
                ys = i % 2
                rms_scale(x1g[gb][:, i, :], r_x1g[gb][i], xs1[ys][:], r_xs1[ys])
                transpose8(xs1[ys], r_xs1[ys], h1T[gb][:, :, i * 128:(i + 1) * 128], r_h1T[gb])

            def vproj(i):
                bk = 4 + cc["pj"] % 2
                cc["pj"] += 1
                for k in range(8):
                    T.op("pe", lambda e: e.matmul(pf(bk, 128), lhsT=h1T[gb][:, k, i * 128:(i + 1) * 128],
                                                  rhs=wB[:, k, 1024:1152], start=(k == 0), stop=(k == 7)),
                         r=[r_wC, r_h1T[gb]], w=[bank[bk]])
                evac(v1[ub][:, u0 - 1 + i, :, 0:64], pf(bk, 128).rearrange("p (a b) -> p a b", a=2),
                     r=[], w=[bank[bk], r_kv1[ub]])

            items.append(lambda: t_a(0))
            items.append(lambda: t_a(1))
            items.append(lambda: t_b(0))
            items.append(lambda: t_b(1))
            items.append(lambda: t_a(2))
            items.append(lambda: t_c(0))
            items.append(lambda: t_b(2))
            items.append(lambda: t_c(1))
            items.append(lambda: t_c(2))
            for gk in range(2):
                items.append(lambda gk=gk: featproj(h1T[gb], r_h1T[gb], 768 + gk * 128,
                                                    k1T[ub][:, gk, (u0 - 1) * 128:(u0 + 2) * 128], r_kv1[ub], None))
            for i in range(3):
                items.append(lambda i=i: vproj(i))
            for p in range(6):
                items.append(lambda p=p: featproj(h1T[gb], r_h1T[gb], p * 128, q1T[gb][:, p, :], r_q1T[gb], 0.125))
            for p in range(2):
                items.append(lambda p=p: featproj(h1T[gb], r_h1T[gb], 1152 + p * 128, qm1T[gb][:, p, :],
                                                  r_qm1T[gb], 0.125))
            return items

        def stageQ(m, g, gb):
            ub = m % 2
            u0 = 1 + 3 * g
            items = []

            tiles = [i for i in range(3) if u0 + i >= 2]
            steps = []

            def swa_s(i, e_, kt, sw):
                u = u0 + i
                rows = slice(e_ * 64, (e_ + 1) * 64)
                kcol = (u - 2 + kt) * 128
                for gk in range(2):
                    T.op("pe", lambda e: e.matmul(pf(6 + gk, 384).rearrange("p (a b) -> p a b", a=3),
                                                  lhsT=k1T[ub][rows, gk, kcol:kcol + 128],
                                                  rhs=q1T[gb][rows, 3 * gk:3 * gk + 3, i * 128:(i + 1) * 128],
                                                  start=True, stop=True),
                         r=[r_kv1[ub], r_q1T[gb]], w=[bank[6 + gk]])
                for gk in range(2):
                    T.op("dve", lambda e: e.tensor_tensor(
                        out=sbs[sw][:, 3 * gk:3 * gk + 3, :],
                        in0=pf(6 + gk, 384).rearrange("p (a b) -> p a b", a=3),
                        in1=swac[:, kt, e_ + 6 * gk: e_ + 6 * gk + 5:2, :], op=ALU.add),
                        r=[r_const], w=[bank[6 + gk], r_sbs[sw]])
                bias_ap = halob[:, m:m + 1] if (kt == 0 and u == 2) else zero1[:, 0:1]
                T.op("act", lambda e: e.activation(out=p1T[sw][:], in_=sbs[sw][:], func=AF.Exp,
                                                   bias=bias_ap, scale=1.0),
                     r=[r_sbs[sw], r_const], w=[r_p1T[sw]])

            def swa_pv(i, e_, kt, sw):
                u = u0 + i
                for hh in range(6):
                    gk = hh // 3
                    T.op("pe", lambda e: e.matmul(pf(ABK, 65, hh * 65), lhsT=p1T[sw][:, hh, :],
                                                  rhs=v1[ub][:, u - 2 + kt, gk, :], start=(kt == 0 and hh == 0),
                                                  stop=True, skip_group_check=True),
                         r=[r_p1T[sw], r_kv1[ub]], w=[bank[ABK]])
                if kt == 1:
                    av = pf(ABK, 390).rearrange("p (a b) -> p a b", b=65)
                    T.op("dve", lambda e: e.tensor_tensor(out=den[:], in0=av[:, :, 64], in1=esink[:, e_::2], op=ALU.add),
                         r=[r_const], w=[bank[ABK], r_den])
                    T.op("dve", lambda e: e.reciprocal(out=den[:], in_=den[:]), r=[], w=[r_den])
                    T.op("dve", lambda e: e.tensor_tensor(
                        out=y1[gb][:, i, 0:768].rearrange("p (h c) -> p h c", c=64)[:, e_::2, :],
                        in0=av[:, :, 0:64], in1=den[:].unsqueeze(2).to_broadcast([128, 6, 64]), op=ALU.mult),
                        r=[r_den], w=[bank[ABK], r_y1[gb]])

            def mem_s(hm, ps):
                pr, e_ = divmod(hm, 2)
                rows = slice(e_ * 64, (e_ + 1) * 64)
                for kt2 in range(2):
                    T.op("pe", lambda e: e.matmul(pf(6 + kt2, 384), lhsT=kmem[rows, 1, pr, kt2 * 128:(kt2 + 1) * 128],
                                                  rhs=qm1T[gb][rows, pr, 0:384], start=True, stop=True),
                         r=[r_kmem, r_qm1T[gb]], w=[bank[6 + kt2]])
                sview = pbig[:, 6 * 512:8 * 512].rearrange("p (a b) -> p a b", a=2)[:, :, 0:384]
                T.op("act", lambda e: e.activation(out=pT[ps][:], in_=sview, func=AF.Exp),
                     r=[], w=[bank[6], bank[7], r_pT[ps]])

            def mem_pv(hm, ps):
                fl = True
                for qt in range(3):
                    for kt2 in range(2):
                        T.op("pe", lambda e: e.matmul(pf(ABK, 65, qt * 65), lhsT=pT[ps][:, kt2, qt * 128:(qt + 1) * 128],
                                                      rhs=vmem[:, 1, kt2, hm, :], start=fl, stop=True,
                                                      skip_group_check=True),
                             r=[r_pT[ps], r_vmem], w=[bank[ABK]])
                        fl = False
                av = pf(ABK, 195).rearrange("p (a b) -> p a b", b=65)
                T.op("dve", lambda e: e.reciprocal(out=rec[:, 0:3], in_=av[:, :, 64]), r=[], w=[bank[ABK], r_rec])
                T.op("dve", lambda e: e.tensor_tensor(out=y1[gb][:, 0:3, 768 + hm * 64: 768 + (hm + 1) * 64],
                                                      in0=av[:, :, 0:64],
                                                      in1=rec[:, 0:3].unsqueeze(2).to_broadcast([128, 3, 64]),
                                                      op=ALU.mult),
                     r=[r_rec], w=[bank[ABK], r_y1[gb]])

            k_ = 0
            for i in tiles:
                for e_ in range(2):
                    for kt in range(2):
                        sw = k_ % 2
                        steps.append((lambda i=i, e_=e_, kt=kt, sw=sw: swa_s(i, e_, kt, sw),
                                      lambda i=i, e_=e_, kt=kt, sw=sw: swa_pv(i, e_, kt, sw)))
                        k_ += 1
            for hm in range(4):
                steps.append((lambda hm=hm: mem_s(hm, hm % 2), lambda hm=hm: mem_pv(hm, hm % 2)))
            for k in range(len(steps) + 1):
                def f(k=k):
                    if k < len(steps):
                        steps[k][0]()
                    if k >= 1:
                        steps[k - 1][1]()
                items.append(f)

            def t_z1(i):
                for half in range(2):
                    for k in range(8):
                        T.op("pe", lambda e: e.matmul(pf(2 + half), lhsT=h1T[gb][:, k, i * 128:(i + 1) * 128],
                                                      rhs=wB[:, k, 1408 + half * 512: 1408 + (half + 1) * 512],
                                                      start=(k == 0), stop=(k == 7)),
                             r=[r_wC, r_h1T[gb]], w=[bank[2 + half]])
                zs = 0
                zps = pbig[:, 1024:2048]
                T.op("act", lambda e: e.activation(out=es1[zs][:], in_=zps, func=AF.Exp, scale=-1.0),
                     r=[], w=[bank[2], bank[3], r_es1[zs]])
                T.op("act", lambda e: e.copy(out=ot[zs][:], in_=zps), r=[], w=[bank[2], bank[3], r_ot[zs]])
                T.op("act", lambda e: e.activation(out=es1[zs][:], in_=es1[zs][:], func=AF.Ln, bias=1.0),
                     r=[], w=[r_es1[zs]])
                T.op("act", lambda e: e.activation(out=es1[zs][:], in_=es1[zs][:], func=AF.Exp, scale=-1.0),
                     r=[], w=[r_es1[zs]])
                T.op("dve", lambda e: e.tensor_tensor(out=es1[zs][:], in0=ot[zs][:], in1=es1[zs][:], op=ALU.mult),
                     r=[r_ot[zs]], w=[r_es1[zs]])
                T.op("dve", lambda e: e.tensor_tensor(out=yg2[:], in0=y1[gb][:, i, :], in1=es1[zs][:], op=ALU.mult),
                     r=[r_y1[gb], r_es1[zs]], w=[r_yg2])

            def t_z2(i):
                transpose8(yg2, r_yg2, ygT2[:], r_ygT2)

            def t_o(i):
                u = u0 + i
                outproj(ygT2, r_ygT2, wO1, x1g[gb][:, i, :], r_x1g[gb][i])
                s_ = rms_scale(x1g[gb][:, i, :], r_x1g[gb][i], None, None)
                T.op("dve", lambda e: e.scalar_tensor_tensor(out=ot[0][:], in0=x1g[gb][:, i, :], scalar=ssc[s_][:, 2:3],
                                                             in1=fgt[:], op0=ALU.mult, op1=ALU.mult),
                     r=[r_x1g[gb][i], r_ssc[s_], r_const], w=[r_ot[0]])
                orow = (m * 8 + u - 2) * 128
                T.dma("pool", out_d[orow:orow + 128, :], ot[0][:], r=[r_ot[0]], w=[r_out])

            for i in tiles:
                items.append(lambda i=i: t_z1(i))
                items.append(lambda i=i: t_z2(i))
                items.append(lambda i=i: t_o(i))
            return items

        for gi_ in range(len(groups) + 1):
            main = stageP(groups[gi_][0], groups[gi_][1], gi_ % 2) if gi_ < len(groups) else []
            side = stageQ(groups[gi_ - 1][0], groups[gi_ - 1][1], (gi_ - 1) % 2) if gi_ >= 1 else []
            if main:
                run_interleaved(main, side)
            else:
                for f in side:
                    f()
        T.barrier()
    return nc


def host_consts(q):
    sl = _slopes()
    c = {}
    kx = np.zeros((64, S), dtype=np.float32)
    kx[0:3, :] = 1.0
    kt = (np.arange(S) // 128).astype(np.float32)
    kx[3:6, :] = kt[None, :]
    blk = np.arange(S) // 256
    kx[6 + (blk % 32), np.arange(S)] = 1.0
    c["kx"] = kx.astype(NPBF)
    tpos = np.zeros(NLOC, dtype=np.int64)
    for m in range(NU):
        base = (16 * m + 4 * q) * 256 - 256
        tpos[m * UT * 128:(m + 1) * UT * 128] = base + np.arange(UT * 128)
    kxo = np.zeros((64, NLOC), dtype=np.float32)
    kxo[0:3, :] = 1.0
    kto = np.floor_divide(tpos, 128).astype(np.float32)
    kxo[3:6, :] = kto[None, :]
    c["kxo"] = kxo.astype(NPBF)
    qac = np.zeros((H, 6, NLOC), dtype=NPBF)
    for h in range(H):
        cc = (-sl[h] * tpos.astype(np.float32)).astype(np.float32)
        hi, mid, lo = _split3(cc)
        qac[h, 0], qac[h, 1], qac[h, 2] = hi, mid, lo
        s1, s2, s3 = _split3(np.float32(sl[h]))
        qac[h, 3] = (np.float32(128.0) * s1.astype(np.float32)).astype(NPBF)
        qac[h, 4] = (np.float32(128.0) * s2.astype(np.float32)).astype(NPBF)
        qac[h, 5] = (np.float32(128.0) * s3.astype(np.float32)).astype(NPBF)
    c["qac"] = qac
    c["abias"] = (np.arange(128, dtype=np.float32)[:, None] * sl[None, :]).astype(np.float32)
    bm = np.zeros((NLT, NBLK), dtype=np.float32)
    for lt in range(NLT):
        m, u = divmod(lt, UT)
        i_own = 16 * m + 4 * q - 1 + u // 2
        bm[lt, :] = np.where(np.arange(NBLK) < i_own, 0.0, -1e30)
    c["bmask"] = np.ascontiguousarray(np.broadcast_to(bm.reshape(1, -1), (128, NLT * NBLK))).astype(np.float32)
    p = np.arange(128)[:, None, None]
    k2 = np.arange(2)[None, :, None]
    t = np.arange(256)[None, None, :]
    cm = np.where(t >= 128 * k2 + p, 0.0, -BIG).astype(np.float32)
    c["cm"] = cm.reshape(128, 512).astype(NPBF)
    c["ident"] = np.eye(128, dtype=np.float32).astype(NPBF)
    pk = np.arange(128, dtype=np.float32)[:, None, None]
    tt = np.arange(128, dtype=np.float32)[None, None, :]
    slh = sl[None, :, None]
    d_cur = tt - pk
    cur = np.where(d_cur >= 0, -slh * d_cur, NEG)
    d_prev = 128.0 + tt - pk
    prev = np.where(d_prev < 128, -slh * d_prev, NEG)
    c["swac"] = np.stack([prev, cur], axis=1).astype(np.float32).reshape(128, 2 * H * 128)
    hb = np.zeros((128, NU), dtype=np.float32)
    if q == 0:
        hb[:, 0] = NEG
    c["halob"] = hb
    return c


def make_in_maps(x, mem, norm_g, w_in_a, w_in_b, sinks_b, w_mem_kv, w_out, mem_norm_g, final_norm_g, cores):
    x = np.asarray(x, dtype=np.float32)
    in_maps = []
    gcols = np.concatenate([np.asarray(norm_g[0], np.float32).reshape(8, 128).T,
                            np.asarray(norm_g[1], np.float32).reshape(8, 128).T,
                            np.asarray(mem_norm_g, np.float32).reshape(8, 128).T], axis=1)
    fgr = np.ascontiguousarray(np.broadcast_to(np.asarray(final_norm_g, np.float32)[None, :], (128, D)))
    sinkr = np.ascontiguousarray(np.broadcast_to(np.asarray(sinks_b, np.float32).reshape(1, H), (128, H)))
    cc = {q: host_consts(q) for q in range(4)}
    for c in cores:
        b, q = divmod(c, 4)
        xmine = np.zeros((NLOC, D), dtype=np.float32)
        for m in range(NU):
            t0 = (16 * m + 4 * q) * 256 - 256
            lo = max(t0, 0)
            xmine[m * 1280 + (lo - t0):(m + 1) * 1280] = x[b, lo:t0 + 1280]
        d = {"xg": np.ascontiguousarray(x[b]), "xm": xmine, "memb": np.ascontiguousarray(np.asarray(mem, np.float32)[b]),
             "w_in_a": np.ascontiguousarray(np.asarray(w_in_a, np.float32)[0]),
             "w_in_b": np.ascontiguousarray(np.asarray(w_in_b, np.float32)[0]),
             "w_mem": np.ascontiguousarray(np.asarray(w_mem_kv, np.float32)),
             "w_out": np.ascontiguousarray(np.asarray(w_out, np.float32)),
             "gcols": np.ascontiguousarray(gcols), "fgr": fgr, "sinkr": sinkr}
        d.update(cc[q])
        in_maps.append(d)
    return in_maps


def kernel(x, mem, norm_g, w_in_a, w_in_b, sinks_b, w_mem_kv, w_out, mem_norm_g, final_norm_g):
    cores = list(range(8))
    in_maps = make_in_maps(x, mem, norm_g, w_in_a, w_in_b, sinks_b, w_mem_kv, w_out, mem_norm_g, final_norm_g, cores)
    nc = build()
    res = run_bass_kernel_spmd(nc, in_maps, core_ids=cores)
    out = np.zeros((2, S, D), dtype=np.float32)
    for c in cores:
        b, q = divmod(c, 4)
        o = res.results[c]["out"]
        for m in range(NU):
            t0 = (16 * m + 4 * q) * 256
            out[b, t0:t0 + 1024] = o[m * 1024:(m + 1) * 1024]
    return out
```

```python
import contextlib
import math
import numpy as np
import ml_dtypes
import concourse.bass as bass
import concourse.mybir as mybir
from concourse.bass_utils import run_bass_kernel_spmd

F32 = mybir.dt.float32
BF16 = mybir.dt.bfloat16
AF = mybir.ActivationFunctionType
ALU = mybir.AluOpType
AX = mybir.AxisListType
NPBF = ml_dtypes.bfloat16

D = 1024
S = 16384
H = 12
DH = 64
NBLK = 64
NTG = 128
UT = 10
NU = 4
NLT = NU * UT
NLOC = NLT * 128
NQT = 36
WA = 3584
EPS = 1e-6
BIG = 32768.0
NEG = -30000.0
_DBG = {}


def _slopes():
    def p2(n):
        st = 2.0 ** (-8.0 / n)
        return [st ** (i + 1) for i in range(n)]
    c = 2 ** math.floor(math.log2(H))
    vals = p2(c) + p2(2 * c)[0::2][: H - c]
    return np.array(vals, dtype=np.float32)


def _split3(x):
    x = np.asarray(x, dtype=np.float32)
    hi = x.astype(NPBF)
    r1 = (x - hi.astype(np.float32)).astype(np.float32)
    mid = r1.astype(NPBF)
    r2 = (r1 - mid.astype(np.float32)).astype(np.float32)
    lo = r2.astype(NPBF)
    return hi, mid, lo


class Res:
    __slots__ = ("w", "r", "name")

    def __init__(self, name=""):
        self.w = None
        self.r = {}
        self.name = name


class Trk:
    def __init__(self, nc, es):
        self.nc = nc
        self.eng = {"pe": nc.tensor, "act": nc.scalar, "dve": nc.vector,
                    "pool": nc.gpsimd, "sp": nc.sync}
        self.sem = {}
        self.cnt = {}
        for e in ("pe", "act", "dve", "pool"):
            self.sem[e] = es.enter_context(nc.semaphore("c_" + e))
            self.cnt[e] = 0
        self.seen = {e: {} for e in self.eng}
        self.dsem = {}
        self.dcnt = {}
        self.dnext = {}
        for q, n in (("sp", 16), ("act", 4), ("pool", 16)):
            self.dsem[q] = [es.enter_context(nc.semaphore("d_%s%d" % (q, i))) for i in range(n)]
            self.dcnt[q] = [0] * n
            self.dnext[q] = 0
        self.ninstr = 0

    def _wait(self, eng, ev):
        sem, val, key = ev
        if self.seen[eng].get(key, 0) >= val:
            return
        self.eng[eng].wait_ge(sem, val)
        self.seen[eng][key] = val

    def _deps(self, eng, r, w):
        evs = {}

        def add(ev):
            if ev is None:
                return
            k = ev[2]
            if k not in evs or evs[k][1] < ev[1]:
                evs[k] = ev
        for x in r:
            add(x.w)
        for x in w:
            add(x.w)
            for ev in x.r.values():
                add(ev)
        for ev in evs.values():
            if eng == "pe" and ev[2] == "c_pe":
                continue
            self._wait(eng, ev)

    def _upd(self, ev, r, w):
        k = ev[2]
        for x in r:
            x.r[k] = ev
        for x in w:
            x.w = ev
            x.r = {}

    def op(self, eng, fn, r=(), w=()):
        self._deps(eng, r, w)
        ins = fn(self.eng[eng])
        self.cnt[eng] += 1
        ins.then_inc(self.sem[eng], 1)
        ev = (self.sem[eng], self.cnt[eng], "c_" + eng)
        self._upd(ev, r, w)
        self.ninstr += 1
        return ev

    def dma(self, q, out, in_, r=(), w=()):
        self._deps(q, r, w)
        i = self.dnext[q]
        self.dnext[q] = (i + 1) % len(self.dsem[q])
        sem = self.dsem[q][i]
        key = "d_%s%d" % (q, i)
        if self.dcnt[q][i] > 0:
            self._wait(q, (sem, self.dcnt[q][i], key))
        self.eng[q].dma_start(out=out, in_=in_).then_inc(sem, 16)
        self.dcnt[q][i] += 16
        ev = (sem, self.dcnt[q][i], key)
        self._upd(ev, r, w)
        self.ninstr += 1
        return ev

    def barrier(self):
        evs = []
        for e in self.sem:
            if self.cnt[e] > 0:
                evs.append((self.sem[e], self.cnt[e], "c_" + e))
        for q in self.dsem:
            for i, sem in enumerate(self.dsem[q]):
                if self.dcnt[q][i] > 0:
                    evs.append((sem, self.dcnt[q][i], "d_%s%d" % (q, i)))
        for e in self.eng:
            for ev in evs:
                self._wait(e, ev)


def build(stop_after="all", debug=False):
    nc = bass.Bass("TRN2", target_bir_lowering=False)
    es = contextlib.ExitStack()

    def din(name, shape, dt=F32):
        return nc.dram_tensor(name, list(shape), dt, kind="ExternalInput").ap()

    dbg = set(debug) if debug else set()

    def dscr(name, shape, dt):
        kind = "ExternalOutput" if name in dbg else "Internal"
        return nc.dram_tensor(name, list(shape), dt, kind=kind).ap()

    xg = din("xg", [S, D])
    xm = din("xm", [NLOC, D])
    memb = din("memb", [256, D])
    w_in_a = din("w_in_a", [D, WA])
    w_in_b = din("w_in_b", [D, 2304])
    w_mem = din("w_mem", [2, D, 512])
    w_out = din("w_out", [2, D, D])
    gcols = din("gcols", [128, 24])
    fgr = din("fgr", [128, D])
    sinkr = din("sinkr", [128, H])
    kx = din("kx", [64, S], BF16)
    kxo = din("kxo", [64, NLOC], BF16)
    qac = din("qac", [H, 6, NLOC], BF16)
    abias_d = din("abias", [128, H])
    bmask_d = din("bmask", [128, NLT * NBLK])
    cm_d = din("cm", [128, 512], BF16)
    ident_d = din("ident", [128, 128], BF16)
    swac_d = din("swac", [128, 2 * H * 128])
    halob_d = din("halob", [128, NU])
    out_d = nc.dram_tensor("out", [NU * 1024, D], F32, kind="ExternalOutput").ap()

    KT = dscr("KT", [H, 128, S], BF16)
    VP = dscr("VP", [H, 128, NTG, 65], BF16)
    KTo = dscr("KTo", [H, 128, NLOC], BF16)
    VPo = dscr("VPo", [H, 128, NLT, 65], BF16)
    QA = dscr("QA", [H, 2, 128, NLOC], BF16)
    QM = dscr("QM", [2, 128, NLOC], BF16)
    GT = dscr("GT", [NLOC, D], F32)
    YS = dscr("YS", [128, NQT * D], BF16)
    KMD = dscr("KMD", [128, 6 * NBLK], BF16) if "KMD" in dbg else None
    YD = dscr("YD", [128, NQT * D], BF16) if "YD" in dbg else None

    with es:
        T = Trk(nc, es)

        def sb(name, shape, dt, st=None):
            return (st or es).enter_context(nc.sbuf_tensor("s_" + name, list(shape), dt))

        pbig = es.enter_context(nc.psum_tensor("pbig", [128, 4096], F32))
        bank = [Res("bank%d" % i) for i in range(8)]

        def pf(b, n=512, off=0):
            return pbig[:, b * 512 + off: b * 512 + off + n]

        def pbf(b):
            return pbig[:, b * 512:(b + 1) * 512].bitcast(BF16)

        ident = sb("ident", [128, 128], BF16)
        abias = sb("abias", [128, H], F32)
        gc = sb("gc", [128, 24], F32)
        epst = sb("epst", [128, 1], F32)
        junk = sb("junk", [128, 1024], BF16)
        r_const = Res("const")
        T.dma("sp", ident[:], ident_d, w=[r_const])
        T.dma("sp", abias[:], abias_d, w=[r_const])
        T.dma("sp", gc[:], gcols, w=[r_const])
        T.op("dve", lambda e: e.memset(epst[:], EPS), w=[r_const])

        r_KT = [Res("KT%d" % h) for h in range(H)]
        r_KTo = [Res("KTo%d" % h) for h in range(H)]
        r_QA = [Res("QA%d" % h) for h in range(H)]
        for h in range(H):
            eb = 64 if h % 2 == 0 else 0
            T.dma("sp", KT[h, eb:eb + 64, :], kx, w=[r_KT[h]])
            T.dma("sp", KTo[h, eb:eb + 64, :], kxo, w=[r_KTo[h]])
            for v in range(2):
                T.dma("sp", QA[h, v, eb:eb + 6, :], qac[h], w=[r_QA[h]])

        kmem = sb("kmem", [128, 2, 2, 256], BF16)
        vmem = sb("vmem", [128, 2, 2, 4, 65], BF16)
        kmT = sb("kmT", [128, 6, NBLK], BF16)

        stA = contextlib.ExitStack()
        es.enter_context(stA)
        NXS = 3
        xt = [sb("xt%d" % i, [128, D], F32, stA) for i in range(NXS)]
        r_xt = [Res() for _ in range(NXS)]
        xs = [sb("xs%d" % i, [128, D], BF16, stA) for i in range(2)]
        r_xs = [Res() for _ in range(2)]
        ssq = [sb("ssq%d" % i, [128, 4], F32, stA) for i in range(NXS)]
        r_ss = [Res() for _ in range(NXS)]
        hT = [sb("hT%d" % i, [128, 8, 512], BF16, stA) for i in range(2)]
        r_hT = [Res() for _ in range(2)]
        st = {"x": 0, "xs": 0, "tb": 0, "ev": 0}
        TB = (0, 1)

        def evac(out, in_, r, w, scale=None):
            st["ev"] += 1
            if st["ev"] % 2 == 0:
                if scale is None:
                    T.op("act", lambda e: e.copy(out=out, in_=in_), r=r, w=w)
                else:
                    T.op("act", lambda e: e.mul(out=out, in_=in_, mul=scale), r=r, w=w)
            else:
                if scale is None:
                    T.op("dve", lambda e: e.tensor_copy(out=out, in_=in_), r=r, w=w)
                else:
                    T.op("dve", lambda e: e.tensor_scalar(out=out, in0=in_, scalar1=scale, scalar2=None,
                                                          op0=ALU.mult), r=r, w=w)

        def norm_T(src, row0, ntiles, hslot):
            for i in range(ntiles):
                s = st["x"] % NXS
                st["x"] += 1
                T.dma("sp", xt[s][:], src[row0 + i * 128: row0 + (i + 1) * 128, :], w=[r_xt[s]])
                T.op("act", lambda e: e.activation(out=junk[:], in_=xt[s][:], func=AF.Square,
                                                   accum_out=ssq[s][:, 0:1]), r=[r_xt[s]], w=[r_ss[s]])
                T.op("act", lambda e: e.activation(out=ssq[s][:, 1:2], in_=ssq[s][:, 0:1], func=AF.Ln,
                                                   bias=epst[:, 0:1], scale=1.0 / D),
                     r=[r_const], w=[r_ss[s]])
                T.op("act", lambda e: e.activation(out=ssq[s][:, 2:3], in_=ssq[s][:, 1:2], func=AF.Exp, scale=-0.5),
                     r=[], w=[r_ss[s]])
                b = st["xs"] % 2
                st["xs"] += 1
                T.op("dve", lambda e: e.tensor_scalar(out=xs[b][:], in0=xt[s][:], scalar1=ssq[s][:, 2:3],
                                                      scalar2=None, op0=ALU.mult),
                     r=[r_xt[s], r_ss[s]], w=[r_xs[b]])
                tb = TB[st["tb"] % 2]
                st["tb"] += 1
                pv = pbf(tb)
                for k in range(8):
                    T.op("pe", lambda e: e.transpose(pv[:, k * 128:(k + 1) * 128], xs[b][:, k * 128:(k + 1) * 128],
                                                     ident[:]),
                         r=[r_xs[b], r_const], w=[bank[tb]])
                evac(hT[hslot][:, :, i * 128:(i + 1) * 128],
                     pv.rearrange("p (k t) -> p k t", k=8), r=[], w=[bank[tb], r_hT[hslot]])

        def run_interleaved(main, side):
            n, m_ = len(main), len(side)
            j = 0
            for i_, f in enumerate(main):
                f()
                while j < m_ and (j + 1) * n <= (i_ + 1) * m_:
                    side[j]()
                    j += 1
            while j < m_:
                side[j]()
                j += 1

        def norm_items(src, row0, ntiles, hslot, tbs):
            slots = []
            for i in range(ntiles):
                slots.append((st["x"] % NXS, st["xs"] % 2, tbs[st["tb"] % len(tbs)]))
                st["x"] += 1
                st["xs"] += 1
                st["tb"] += 1

            def p0(i):
                s = slots[i][0]
                T.dma("sp", xt[s][:], src[row0 + i * 128: row0 + (i + 1) * 128, :], w=[r_xt[s]])

            def p1(i):
                s, b, tb = slots[i]
                T.op("act", lambda e: e.activation(out=junk[:], in_=xt[s][:], func=AF.Square,
                                                   accum_out=ssq[s][:, 0:1]), r=[r_xt[s]], w=[r_ss[s]])
                T.op("act", lambda e: e.activation(out=ssq[s][:, 1:2], in_=ssq[s][:, 0:1], func=AF.Ln,
                                                   bias=epst[:, 0:1], scale=1.0 / D), r=[r_const], w=[r_ss[s]])
                T.op("act", lambda e: e.activation(out=ssq[s][:, 2:3], in_=ssq[s][:, 1:2], func=AF.Exp, scale=-0.5),
                     r=[], w=[r_ss[s]])
                T.op("dve", lambda e: e.tensor_scalar(out=xs[b][:], in0=xt[s][:], scalar1=ssq[s][:, 2:3],
                                                      scalar2=None, op0=ALU.mult),
                     r=[r_xt[s], r_ss[s]], w=[r_xs[b]])

            def p2(i):
                s, b, tb = slots[i]
                pv = pbf(tb)
                for k in range(8):
                    T.op("pe", lambda e: e.transpose(pv[:, k * 128:(k + 1) * 128], xs[b][:, k * 128:(k + 1) * 128],
                                                     ident[:]), r=[r_xs[b], r_const], w=[bank[tb]])
                evac(hT[hslot][:, :, i * 128:(i + 1) * 128],
                     pv.rearrange("p (k t) -> p k t", k=8), r=[], w=[bank[tb], r_hT[hslot]])

            items = []
            for k_ in range(-1, ntiles + 1):
                def f(k_=k_):
                    if 0 <= k_ + 1 < ntiles:
                        p0(k_ + 1)
                    if 0 <= k_ - 1 < ntiles:
                        p2(k_ - 1)
                    if 0 <= k_ < ntiles:
                        p1(k_)
                items.append(f)
            return items

        wA = sb("wA", [128, 8, WA], BF16, stA)
        r_wA = Res()
        r_wM = Res()
        r_kmT = Res()
        kms = sb("kms", [128, 2], F32, stA)
        r_kms = Res()
        kst = [sb("kst%d" % i, [128, 512], BF16, stA) for i in range(3)]
        r_kst = [Res() for _ in range(3)]
        vst = [sb("vst%d" % i, [128, H, 4, 65], BF16, stA) for i in range(2)]
        r_vst = [Res() for _ in range(2)]
        bmask = sb("bmask", [128, NLT, NBLK], F32, stA)
        Et = [sb("Et%d" % i, [128, H, 2, 128], BF16, stA) for i in range(2)]
        r_Et = [Res(), Res()]
        gm = sb("gm", [128, 6, NBLK], F32, stA)
        r_gm = Res()
        m8 = sb("m8", [128, 6, 8], F32, stA)
        r_m8 = [Res() for _ in range(6)]
        thr = sb("thr", [128, 6], F32, stA)
        selb = sb("selb", [128, 6, NBLK], F32, stA)
        qmst = sb("qmst", [128, 2, 512], BF16, stA)
        r_qmst = Res()
        esb = [sb("esb%d" % i, [128, D], F32, stA) for i in range(2)]
        r_esb = [Res(), Res()]
        gts = [sb("gts%d" % i, [128, D], F32, stA) for i in range(2)]
        r_gts = [Res(), Res()]
        qa_sts = [sb("qa_st0", [128, H, 2, 512], BF16, stA), None]
        r_qasts = [Res(), Res()]
        stW0 = contextlib.ExitStack()
        stA.enter_context(stW0)
        wM = sb("wM", [128, 2, 8, 512], BF16, stW0)
        wst = [sb("wst%d" % i, [128, 1792], F32, stW0) for i in range(2)]
        r_wst = [Res(), Res()]
        stw = {"i": 0}

        def wload(dst, src, ncols, gcol, rdst):
            for c0 in range(0, ncols, 1792):
                n = min(1792, ncols - c0)
                s = stw["i"] % 2
                stw["i"] += 1
                T.dma("sp", wst[s][:, 0:n], src[:, c0:c0 + n], w=[r_wst[s]])
                if s == 0:
                    if gcol is None:
                        T.op("dve", lambda e: e.tensor_copy(out=dst[:, c0:c0 + n], in_=wst[s][:, 0:n]),
                             r=[r_wst[s]], w=[rdst])
                    else:
                        T.op("dve", lambda e: e.tensor_scalar(out=dst[:, c0:c0 + n], in0=wst[s][:, 0:n],
                                                              scalar1=gcol, scalar2=None, op0=ALU.mult),
                             r=[r_wst[s], r_const], w=[rdst])
                else:
                    if gcol is None:
                        T.op("act", lambda e: e.copy(out=dst[:, c0:c0 + n], in_=wst[s][:, 0:n]),
                             r=[r_wst[s]], w=[rdst])
                    else:
                        T.op("act", lambda e: e.activation(out=dst[:, c0:c0 + n], in_=wst[s][:, 0:n], func=AF.Copy,
                                                           scale=gcol),
                             r=[r_wst[s], r_const], w=[rdst])

        for L in range(2):
            for k in range(8):
                wload(wM[:, L, k, :], w_mem[L, k * 128:(k + 1) * 128, :], 512, gc[:, 16 + k:17 + k], r_wM)
        for k in range(8):
            wload(wA[:, k, :], w_in_a[k * 128:(k + 1) * 128, :], WA, gc[:, k:k + 1], r_wA)
        T.dma("sp", bmask[:].rearrange("p a b -> p (a b)"), bmask_d, w=[r_const])
        for i in range(2):
            T.op("dve", lambda e: e.memset(vst[i][:].rearrange("p a b c -> p (a b c)"), 1.0), w=[r_vst[i]])
            T.op("dve", lambda e: e.memset(Et[i][:].rearrange("p a b c -> p (a b c)"), 0.0), w=[r_Et[i]])

        r_kmem = Res()
        r_vmem = Res()
        T.op("dve", lambda e: e.memset(vmem[:].rearrange("p a b c d -> p (a b c d)"), 1.0), w=[r_vmem])
        norm_T(memb, 0, 2, 0)
        for L in range(2):
            for pr in range(2):
                bk = 2 + (L * 2 + pr) % 2
                for k in range(8):
                    T.op("pe", lambda e: e.matmul(pf(bk, 256), lhsT=wM[:, L, k, pr * 128:(pr + 1) * 128],
                                                  rhs=hT[0][:, k, 0:256], start=(k == 0), stop=(k == 7)),
                         r=[r_wM, r_hT[0]], w=[bank[bk]])
                evac(kmem[:, L, pr, :], pf(bk, 256), r=[], w=[bank[bk], r_kmem])
            for mt in range(2):
                bk = 4 + mt
                for k in range(8):
                    T.op("pe", lambda e: e.matmul(pf(bk, 256), lhsT=hT[0][:, k, mt * 128:(mt + 1) * 128],
                                                  rhs=wM[:, L, k, 256:512], start=(k == 0), stop=(k == 7)),
                         r=[r_wM, r_hT[0]], w=[bank[bk]])
                evac(vmem[:, L, mt, :, 0:64], pf(bk, 256).rearrange("p (h c) -> p h c", h=4),
                     r=[], w=[bank[bk], r_vmem])
        T.barrier()
        stW0.close()
        qa_sts[1] = sb("qa_st1", [128, H, 2, 512], BF16, stA)

        sk = {"k": 0, "pb": 0, "PB": (2, 3, 4, 5, 6, 7)}

        def nextbank():
            PB = sk["PB"]
            b = PB[sk["pb"] % len(PB)]
            sk["pb"] += 1
            return b

        def proj_items(hslot, ktdst, r_ktdst, vpdst, r_vpdst, col0, gi, do_kmean):
            items = []

            def kpair(p):
                bk = nextbank()
                for k in range(8):
                    T.op("pe", lambda e: e.matmul(pf(bk), lhsT=wA[:, k, 768 + p * 128: 768 + (p + 1) * 128],
                                                  rhs=hT[hslot][:, k, :], start=(k == 0), stop=(k == 7)),
                         r=[r_wA, r_hT[hslot]], w=[bank[bk]])
                s = sk["k"] % 3
                sk["k"] += 1
                if do_kmean:
                    T.op("dve", lambda e: e.tensor_reduce(out=kms[:], in_=pf(bk).rearrange("p (b t) -> p b t", b=2),
                                                          axis=AX.X, op=ALU.add),
                         r=[], w=[bank[bk], r_kms])
                    T.op("dve", lambda e: e.tensor_scalar(out=kmT[:, p, 2 * (gi // 4): 2 * (gi // 4) + 2], in0=kms[:],
                                                          scalar1=1.0 / 256.0, scalar2=None, op0=ALU.mult),
                         r=[r_kms], w=[r_kmT])
                evac(kst[s][:], pf(bk), r=[], w=[bank[bk], r_kst[s]])
                if not _DBG.get("nokst"):
                    T.dma("pool", ktdst[2 * p, 0:64, col0:col0 + 512], kst[s][0:64, :], r=[r_kst[s]], w=[r_ktdst[2 * p]])
                    T.dma("pool", ktdst[2 * p + 1, 64:128, col0:col0 + 512], kst[s][64:128, :], r=[r_kst[s]],
                          w=[r_ktdst[2 * p + 1]])

            vs = (gi // 4) % 2

            def vgrp(i, c0, n, h0, last):
                bk = nextbank()
                for k in range(8):
                    T.op("pe", lambda e: e.matmul(pf(bk, n), lhsT=hT[hslot][:, k, i * 128:(i + 1) * 128],
                                                  rhs=wA[:, k, 1536 + c0: 1536 + c0 + n],
                                                  start=(k == 0), stop=(k == 7)),
                         r=[r_wA, r_hT[hslot]], w=[bank[bk]])
                evac(vst[vs][:, h0:h0 + n // 64, i, 0:64], pf(bk, n).rearrange("p (h c) -> p h c", c=64),
                     r=[], w=[bank[bk], r_vst[vs]])
                if last and not _DBG.get("novst"):
                    T.dma("pool", vpdst[:, :, gi:gi + 4, :].rearrange("h p t c -> p h (t c)"),
                          vst[vs][:].rearrange("p h t c -> p h (t c)"), r=[r_vst[vs]], w=r_vpdst)

            for p in range(6):
                items.append(lambda p=p: kpair(p))
            for i in range(4):
                items.append(lambda i=i: vgrp(i, 0, 512, 0, False))
                items.append(lambda i=i: vgrp(i, 512, 256, 8, i == 3))
            return items

        r_VP = [Res("VP")]
        r_VPo = [Res("VPo")]
        NG1 = NTG // 4
        if stop_after == "A1s":
            NG1 = 2
        for f in norm_items(xg, 0, 4, 0, TB):
            f()
        for G in range(NG1):
            main = proj_items(G % 2, KT, r_KT, VP, r_VP, G * 512, G * 4, True)
            side = norm_items(xg, (G + 1) * 512, 4, (G + 1) % 2, TB) if G + 1 < NG1 else []
            run_interleaved(main, side)
        if KMD is not None:
            T.dma("sp", KMD, kmT[:].rearrange("p a b -> p (a b)"), r=[r_kmT])
        if stop_after in ("A1", "A1s"):
            T.barrier()
            return nc

        r_QM = Res()
        r_GT = Res()
        NG2 = NLT // 4
        if stop_after == "A2s":
            NG2 = 1
        cnt2 = {"e": 0, "z": 0}
        sk["PB"] = (5, 6, 7)
        TB2 = (0,)

        def a2_main_items(G):
            hs = G % 2
            qa_st = qa_sts[G % 2]
            r_qast = r_qasts[G % 2]
            items = proj_items(hs, KTo, r_KTo, VPo, r_VPo, G * 512, G * 4, False)

            def qpair(p):
                bk = nextbank()
                for k in range(8):
                    T.op("pe", lambda e: e.matmul(pf(bk), lhsT=wA[:, k, p * 128:(p + 1) * 128],
                                                  rhs=hT[hs][:, k, :], start=(k == 0), stop=(k == 7)),
                         r=[r_wA, r_hT[hs]], w=[bank[bk]])
                for e_ in range(2):
                    rows = slice(e_ * 64, (e_ + 1) * 64)
                    for v in range(2):
                        evac(qa_st[rows, 2 * p + e_, v, :], pbig[rows, bk * 512:(bk + 1) * 512], r=[],
                             w=[bank[bk], r_qast], scale=0.125)

            def qmpair(p):
                bk = nextbank()
                for k in range(8):
                    T.op("pe", lambda e: e.matmul(pf(bk), lhsT=wA[:, k, 2304 + p * 128: 2304 + (p + 1) * 128],
                                                  rhs=hT[hs][:, k, :], start=(k == 0), stop=(k == 7)),
                         r=[r_wA, r_hT[hs]], w=[bank[bk]])
                evac(qmst[:, p, :], pf(bk), r=[], w=[bank[bk], r_qmst], scale=0.125)
                if p == 1:
                    T.dma("pool", QM[:, :, G * 512:(G + 1) * 512].rearrange("a p c -> p a c"), qmst[:],
                          r=[r_qmst], w=[r_QM])

            def ztile(i):
                zb = 6
                for half in range(2):
                    for k in range(8):
                        T.op("pe", lambda e: e.matmul(pf(zb + half), lhsT=hT[hs][:, k, i * 128:(i + 1) * 128],
                                                      rhs=wA[:, k, 2560 + half * 512: 2560 + (half + 1) * 512],
                                                      start=(k == 0), stop=(k == 7)),
                             r=[r_wA, r_hT[hs]], w=[bank[zb + half]])
                zs = cnt2["z"] % 2
                cnt2["z"] += 1
                zps = pbig[:, zb * 512:(zb + 2) * 512]
                T.op("act", lambda e: e.activation(out=esb[zs][:], in_=zps, func=AF.Exp, scale=-1.0),
                     r=[], w=[bank[zb], bank[zb + 1], r_esb[zs]])
                T.op("act", lambda e: e.copy(out=gts[zs][:], in_=zps), r=[], w=[bank[zb], bank[zb + 1], r_gts[zs]])
                T.op("act", lambda e: e.activation(out=esb[zs][:], in_=esb[zs][:], func=AF.Ln, bias=1.0),
                     r=[], w=[r_esb[zs]])
                T.op("act", lambda e: e.activation(out=esb[zs][:], in_=esb[zs][:], func=AF.Exp, scale=-1.0),
                     r=[], w=[r_esb[zs]])
                T.op("dve", lambda e: e.tensor_tensor(out=gts[zs][:], in0=gts[zs][:], in1=esb[zs][:], op=ALU.mult),
                     r=[r_esb[zs]], w=[r_gts[zs]])
                lt = G * 4 + i
                T.dma("pool", GT[lt * 128:(lt + 1) * 128, :], gts[zs][:], r=[r_gts[zs]], w=[r_GT])

            for p in range(6):
                items.append(lambda p=p: qpair(p))
            for p in range(2):
                items.append(lambda p=p: qmpair(p))
            for i in range(4):
                items.append(lambda i=i: ztile(i))
            return items

        def a2_gate_items(G):
            qa_st = qa_sts[G % 2]
            r_qast = r_qasts[G % 2]
            items = []

            def gate(i, e_):
                lt = G * 4 + i
                E = Et[i % 2]
                r_E = r_Et[i % 2]
                gb = 1 + e_
                rows = slice(e_ * 64, (e_ + 1) * 64)
                for p in range(6):
                    T.op("pe", lambda e: e.matmul(pf(gb, 64, p * 64), lhsT=qa_st[rows, 2 * p + e_, 0, i * 128:(i + 1) * 128],
                                                  rhs=kmT[rows, p, :], start=True, stop=True),
                         r=[r_qast, r_kmT], w=[bank[gb]])
                T.op("dve", lambda e: e.tensor_tensor(out=gm[:], in0=pf(gb, 384).rearrange("p (a b) -> p a b", a=6),
                                                      in1=bmask[:, lt:lt + 1, :].to_broadcast([128, 6, NBLK]),
                                                      op=ALU.add),
                     r=[r_const], w=[bank[gb], r_gm])
                for p in range(6):
                    T.op("dve", lambda e: e.max(out=m8[:, p, :], in_=gm[:, p, :]), r=[r_gm], w=[r_m8[p]])
                T.op("dve", lambda e: e.tensor_scalar(out=thr[:], in0=m8[:, :, 2], scalar1=-1e29, scalar2=None,
                                                      op0=ALU.max), r=r_m8, w=[r_gm])
                T.op("dve", lambda e: e.tensor_tensor(out=selb[:], in0=gm[:],
                                                      in1=thr[:].unsqueeze(2).to_broadcast([128, 6, NBLK]),
                                                      op=ALU.is_ge), r=[], w=[r_gm])
                cb = (64 if e_ == 0 else 0) + 6
                for v in range(2):
                    T.op("dve", lambda e: e.tensor_scalar(out=E[:, e_::2, v, cb:cb + 32],
                                                          in0=selb[:, :, v * 32:(v + 1) * 32],
                                                          scalar1=1.0, scalar2=BIG, op0=ALU.subtract, op1=ALU.mult),
                         r=[], w=[r_gm, r_E])

            def etr(i, e_):
                E = Et[i % 2]
                r_E = r_Et[i % 2]
                ext = slice(64, 128) if e_ == 0 else slice(0, 64)
                for hb in range(2):
                    tbk = 3 + hb
                    pv = pbf(tbk)
                    for j in range(3):
                        h = e_ + 2 * (3 * hb + j)
                        for v in range(2):
                            sl_ = (j * 2 + v) * 128
                            T.op("pe", lambda e: e.transpose(pv[:, sl_:sl_ + 128], E[:, h, v, :], ident[:]),
                                 r=[r_E, r_const], w=[bank[tbk]])
                    h0 = e_ + 6 * hb
                    evac(qa_st[ext, h0:h0 + 5:2, :, i * 128:(i + 1) * 128],
                         pv[ext, 0:768].rearrange("p (a b t) -> p a b t", a=3, b=2),
                         r=[], w=[bank[tbk], r_qast])

            def store():
                cols = slice(G * 512, (G + 1) * 512)
                for e_ in range(2):
                    dh = slice(0, 64) if e_ == 0 else slice(64, 128)
                    ex = slice(70, 128) if e_ == 0 else slice(6, 64)
                    rq = [r_QA[h] for h in range(e_, H, 2)]
                    for v in range(2):
                        T.dma("sp", QA[e_::2, v, dh, cols].rearrange("h r c -> r h c"), qa_st[dh, e_::2, v, :],
                              r=[r_qast], w=rq)
                        T.dma("sp", QA[e_::2, v, ex, cols].rearrange("h r c -> r h c"), qa_st[ex, e_::2, v, :],
                              r=[r_qast], w=rq)

            g_ = [[(lambda i=i, e_=e_: gate(i, e_)) for e_ in range(2)] for i in range(4)]
            t_ = [[(lambda i=i, e_=e_: etr(i, e_)) for e_ in range(2)] for i in range(4)]
            items = g_[0] + g_[1] + t_[0] + g_[2] + t_[1] + g_[3] + t_[2] + t_[3] + [store]
            return items

        def merge(a, b):
            out = []
            n, m_ = len(a), len(b)
            j = 0
            for i_, f in enumerate(a):
                out.append(f)
                while j < m_ and (j + 1) * n <= (i_ + 1) * m_:
                    out.append(b[j])
                    j += 1
            out.extend(b[j:])
            return out

        for f in norm_items(xm, 0, 4, 0, TB2):
            f()
        for G in range(NG2 + 1):
            main = a2_main_items(G) if G < NG2 else []
            side = []
            if G >= 1:
                side = a2_gate_items(G - 1)
            if G + 1 < NG2:
                side = merge(side, norm_items(xm, (G + 1) * 512, 4, (G + 1) % 2, TB2)) if side else \
                    norm_items(xm, (G + 1) * 512, 4, (G + 1) % 2, TB2)
            if main:
                run_interleaved(main, side)
            else:
                for f in side:
                    f()
        if stop_after in ("A2", "A2s"):
            T.barrier()
            return nc

        T.barrier()
        stA.close()
        stY = contextlib.ExitStack()
        es.enter_context(stY)
        Y = sb("Y", [128, NQT, D], BF16, stY)
        r_Y = Res("Y")
        stB = contextlib.ExitStack()
        es.enter_context(stB)
        cmt = sb("cmt", [128, 2, 256], BF16, stB)
        T.dma("sp", cmt[:].rearrange("p a b -> p (a b)"), cm_d, w=[r_const])
        QAs = [sb("QAs%d" % i, [128, 2, NLOC], BF16, stB) for i in range(2)]
        r_QAs = [Res(), Res()]
        KTos = [sb("KTos%d" % i, [128, NLOC], BF16, stB) for i in range(2)]
        VPos = [sb("VPos%d" % i, [128, NLT, 65], BF16, stB) for i in range(2)]
        r_own = [Res(), Res()]
        NKS = 3
        kring = [sb("kring%d" % i, [128, 1024], BF16, stB) for i in range(NKS)]
        vring = [sb("vring%d" % i, [128, 8, 65], BF16, stB) for i in range(NKS)]
        r_ring = [Res() for _ in range(NKS)]
        NPT = 3
        pT = [sb("pT%d" % i, [128, 2, 384], BF16, stB) for i in range(NPT)]
        r_pT = [Res() for _ in range(NPT)]
        rec = sb("rec", [128, 9], F32, stB)
        r_rec = Res()
        qmT = sb("qmT", [128, 2, NLOC], BF16, stB)
        r_qmT = Res()
        T.dma("sp", qmT[:], QM.rearrange("a p c -> p a c"), r=[r_QM], w=[r_qmT])

        heads = list(range(H))
        units = list(range(NU))
        if stop_after == "B1s":
            heads = [0, 1, 7]
            units = [0, 1]

        def load_head(h, hb):
            for v in range(2):
                T.dma("sp", QAs[hb][:, v, :], QA[h, v], r=[r_QA[h]], w=[r_QAs[hb]])
            T.dma("sp", KTos[hb][:], KTo[h], r=[r_KTo[h]], w=[r_own[hb]])
            T.dma("sp", VPos[hb][:], VPo[h], r=r_VPo, w=[r_own[hb]])

        chunks = []
        for h in heads:
            for m in units:
                nb = 16 * m + 15
                for c in range((nb + 3) // 4):
                    chunks.append((h, m, c))
        cstate = {"next": 0}

        def ensure_loaded(ci):
            while cstate["next"] <= min(ci, len(chunks) - 1):
                n = cstate["next"]
                h, m, c = chunks[n]
                sl = n % NKS
                T.dma("sp", kring[sl][:], KT[h, :, c * 1024:(c + 1) * 1024], r=[r_KT[h]], w=[r_ring[sl]])
                T.dma("sp", vring[sl][:], VP[h, :, c * 8:(c + 1) * 8, :], r=r_VP, w=[r_ring[sl]])
                cstate["next"] += 1

        sst = {"s": 0}
        LAG = 2
        ci = 0
        for hi, h in enumerate(heads):
            hb = hi % 2
            if hi == 0:
                load_head(h, 0)
            if hi + 1 < len(heads):
                load_head(heads[hi + 1], (hi + 1) % 2)
            for m in units:
                nb = 16 * m + 15
                cnts = (16 * m + 12, 16 * m + 14, 16 * m + 15)
                steps = []
                for c in range((nb + 3) // 4):
                    for j in range(4 * c, min(4 * c + 4, nb)):
                        for g in range(3):
                            if j < cnts[g]:
                                steps.append(("past", j, g, ci + c))
                for qb in range(5):
                    steps.append(("own", qb, 0, -1))
                first = {6: True, 7: True}
                nst = len(steps)
                info = [None] * nst
                for idx in range(nst + LAG):
                    if idx < nst:
                        kind, a, g, cidx = steps[idx]
                        sbuf_i = sst["s"] % 3
                        sst["s"] += 1
                        b0 = 2 * sbuf_i
                        if kind == "past":
                            j = a
                            ensure_loaded(cidx + 1)
                            sl = cidx % NKS
                            v = j // 32
                            qc = (UT * m + 1 + 3 * g) * 128
                            N = 384
                            for kt2 in range(2):
                                kc = (j % 4) * 256 + kt2 * 128
                                T.op("pe", lambda e: e.matmul(pf(b0 + kt2, N), lhsT=kring[sl][:, kc:kc + 128],
                                                              rhs=QAs[hb][:, v, qc:qc + N], start=True, stop=True),
                                     r=[r_ring[sl], r_QAs[hb]], w=[bank[b0 + kt2]])
                            tqs = [3 * g + i_ for i_ in range(3)]
                            vsrc = [(vring[sl], (j % 4) * 2 + kt2) for kt2 in range(2)]
                            rres = [r_ring[sl]]
                        else:
                            qb = a
                            if qb == 0:
                                qc, N, c0 = (UT * m + 1) * 128, 128, 128
                                tqs = [0]
                            else:
                                qc, N, c0 = (UT * m + 2 * qb) * 128, 256, 0
                                tqs = [2 * qb - 1, 2 * qb]
                            for kt2 in range(2):
                                kc = (UT * m + 2 * qb + kt2) * 128
                                T.op("pe", lambda e: e.matmul(pf(b0 + kt2, N), lhsT=KTos[hb][:, kc:kc + 128],
                                                              rhs=QAs[hb][:, 0, qc:qc + N], start=True, stop=False),
                                     r=[r_own[hb], r_QAs[hb]], w=[bank[b0 + kt2]])
                                T.op("pe", lambda e: e.matmul(pf(b0 + kt2, N), lhsT=ident[:],
                                                              rhs=cmt[:, kt2, c0:c0 + N], start=False, stop=True),
                                     r=[r_const], w=[bank[b0 + kt2]])
                            vsrc = [(VPos[hb], UT * m + 2 * qb + kt2) for kt2 in range(2)]
                            rres = [r_own[hb]]
                        ps = sst["s"] % NPT
                        sview = pbig[:, b0 * 512:(b0 + 2) * 512].rearrange("p (a b) -> p a b", a=2)[:, :, 0:N]
                        T.op("act", lambda e: e.activation(out=pT[ps][:, :, 0:N], in_=sview, func=AF.Exp,
                                                           bias=abias[:, h:h + 1], scale=1.0),
                             r=[r_const], w=[bank[b0], bank[b0 + 1], r_pT[ps]])
                        info[idx] = (ps, tqs, vsrc, rres)
                    k_ = idx - LAG
                    if k_ >= 0:
                        ps, tqs, vsrc, rres = info[k_]
                        for qi_, tq in enumerate(tqs):
                            bk = 6 if tq < 5 else 7
                            col = (tq if tq < 5 else tq - 5) * 65
                            for kt2 in range(2):
                                vt, vi = vsrc[kt2]
                                fl = first[bk]
                                first[bk] = False
                                T.op("pe", lambda e: e.matmul(pf(bk, 65, col), lhsT=pT[ps][:, kt2, qi_ * 128:(qi_ + 1) * 128],
                                                              rhs=vt[:, vi, :], start=fl, stop=True,
                                                              skip_group_check=True),
                                     r=[r_pT[ps]] + rres, w=[bank[bk]])
                ci += (nb + 3) // 4
                for bk, nt, t0_ in ((6, 5, 0), (7, 4, 5)):
                    av = pf(bk, nt * 65).rearrange("p (a b) -> p a b", b=65)
                    T.op("dve", lambda e: e.reciprocal(out=rec[:, 0:nt], in_=av[:, :, 64]), r=[], w=[bank[bk], r_rec])
                    T.op("dve", lambda e: e.tensor_tensor(out=Y[:, 9 * m + t0_: 9 * m + t0_ + nt, h * 64:(h + 1) * 64],
                                                          in0=av[:, :, 0:64],
                                                          in1=rec[:, 0:nt].unsqueeze(2).to_broadcast([128, nt, 64]),
                                                          op=ALU.mult),
                         r=[r_rec], w=[bank[bk], r_Y])
        if YD is not None:
            T.dma("sp", YD, Y[:].rearrange("p a b -> p (a b)"), r=[r_Y])
        if stop_after in ("B1", "B1s"):
            T.barrier()
            return nc

        def mem_attn(L, qsrc, r_qsrc, qcols, dst_fn, r_dst, ntile, sbanks=(0, 2, 4), abanks=(6, 7), heads_=range(4)):
            N = ntile * 128
            for hm in heads_:
                pr, e_ = divmod(hm, 2)
                rows = slice(e_ * 64, (e_ + 1) * 64)
                sbuf_i = sst["s"] % len(sbanks)
                sst["s"] += 1
                b0 = sbanks[sbuf_i]
                for kt2 in range(2):
                    T.op("pe", lambda e: e.matmul(pf(b0 + kt2, N), lhsT=kmem[rows, L, pr, kt2 * 128:(kt2 + 1) * 128],
                                                  rhs=qsrc[rows, pr, qcols:qcols + N], start=True, stop=True),
                         r=[r_kmem, r_qsrc], w=[bank[b0 + kt2]])
                ps = sst["s"] % len(pT)
                sview = pbig[:, b0 * 512:(b0 + 2) * 512].rearrange("p (a b) -> p a b", a=2)[:, :, 0:N]
                T.op("act", lambda e: e.activation(out=pT[ps][:, :, 0:N], in_=sview, func=AF.Exp),
                     r=[], w=[bank[b0], bank[b0 + 1], r_pT[ps]])
                bk = abanks[hm % len(abanks)]
                fl = True
                for qt in range(ntile):
                    for kt2 in range(2):
                        T.op("pe", lambda e: e.matmul(pf(bk, 65, qt * 65), lhsT=pT[ps][:, kt2, qt * 128:(qt + 1) * 128],
                                                      rhs=vmem[:, L, kt2, hm, :], start=fl, stop=True,
                                                      skip_group_check=True),
                             r=[r_pT[ps], r_vmem], w=[bank[bk]])
                        fl = False
                av = pf(bk, ntile * 65).rearrange("p (a b) -> p a b", b=65)
                T.op("dve", lambda e: e.reciprocal(out=rec[:, 0:ntile], in_=av[:, :, 64]), r=[], w=[bank[bk], r_rec])
                T.op("dve", lambda e: e.tensor_tensor(out=dst_fn(hm), in0=av[:, :, 0:64],
                                                      in1=rec[:, 0:ntile].unsqueeze(2).to_broadcast([128, ntile, 64]),
                                                      op=ALU.mult),
                     r=[r_rec], w=[bank[bk], r_dst])

        for m in units:
            for g in range(3):
                q0 = 9 * m + 3 * g
                mem_attn(0, qmT, r_qmT, (UT * m + 1 + 3 * g) * 128,
                         lambda hm: Y[:, q0:q0 + 3, 768 + hm * 64: 768 + (hm + 1) * 64], r_Y, 3)
        if YD is not None:
            T.dma("sp", YD, Y[:].rearrange("p a b -> p (a b)"), r=[r_Y])
        r_YS = Res()
        T.dma("pool", YS, Y[:].rearrange("p a b -> p (a b)"), r=[r_Y], w=[r_YS])
        if stop_after in ("B2",):
            T.barrier()
            return nc

        T.barrier()
        stB.close()
        stY.close()
        stC = contextlib.ExitStack()
        es.enter_context(stC)
        wO0 = sb("wO0", [128, 8, D], BF16, stC)
        wO1 = sb("wO1", [128, 8, D], BF16, stC)
        wB = sb("wB", [128, 8, 2432], BF16, stC)
        r_wC = Res()
        stW = contextlib.ExitStack()
        stC.enter_context(stW)
        wst2 = [sb("wst2_%d" % i, [128, 1792], F32, stW) for i in range(2)]
        wst[0], wst[1] = wst2[0], wst2[1]
        r_wst[0], r_wst[1] = Res(), Res()
        for k in range(8):
            rows = slice(k * 128, (k + 1) * 128)
            wload(wO0[:, k, :], w_out[0, rows, :], D, None, r_wC)
            wload(wO1[:, k, :], w_out[1, rows, :], D, None, r_wC)
            g1c = gc[:, 8 + k: 9 + k]
            wload(wB[:, k, 0:768], w_in_b[rows, 0:768], 768, g1c, r_wC)
            for gk in range(2):
                for dup in range(2):
                    c0 = 768 + gk * 128 + dup * 64
                    wload(wB[:, k, c0:c0 + 64], w_in_b[rows, 768 + gk * 64: 768 + (gk + 1) * 64], 64, g1c, r_wC)
            wload(wB[:, k, 1024:2432], w_in_b[rows, 896:2304], 1408, g1c, r_wC)
        T.barrier()
        stW.close()
        swac = sb("swac", [128, 2, H, 128], F32, stC)
        fgt = sb("fgt", [128, D], F32, stC)
        esink = sb("esink", [128, H], F32, stC)
        halob = sb("halob", [128, NU], F32, stC)
        zero1 = sb("zero1", [128, 1], F32, stC)
        T.dma("sp", swac[:].rearrange("p a b c -> p (a b c)"), swac_d, w=[r_const])
        T.dma("sp", fgt[:], fgr, w=[r_const])
        T.dma("sp", halob[:], halob_d, w=[r_const])
        T.dma("sp", esink[:], sinkr, w=[r_const])
        T.op("act", lambda e: e.activation(out=esink[:], in_=esink[:], func=AF.Exp), r=[], w=[r_const])
        T.op("dve", lambda e: e.memset(zero1[:], 0.0), w=[r_const])

        yt = [sb("yt%d" % i, [128, D], BF16, stC) for i in range(2)]
        r_yt = [Res(), Res()]
        gtt = [sb("gtt%d" % i, [128, D], F32, stC) for i in range(2)]
        r_gtt = [Res(), Res()]
        x1g = [sb("x1g%d" % i, [128, 3, D], F32, stC) for i in range(2)]
        r_x1g = [[Res() for _ in range(3)] for _ in range(2)]
        yg = [sb("yg%d" % i, [128, D], BF16, stC) for i in range(2)]
        r_yg = [Res(), Res()]
        ygT = [sb("ygT%d" % i, [128, 8, 128], BF16, stC) for i in range(2)]
        r_ygT = [Res(), Res()]
        ssc = [sb("ssc%d" % i, [128, 4], F32, stC) for i in range(4)]
        r_ssc = [Res() for _ in range(4)]
        xs1 = [sb("xs1_%d" % i, [128, D], BF16, stC) for i in range(2)]
        r_xs1 = [Res(), Res()]
        h1T = [sb("h1T%d" % i, [128, 8, 384], BF16, stC) for i in range(2)]
        r_h1T = [Res(), Res()]
        q1T = [sb("q1T%d" % i, [128, 6, 384], BF16, stC) for i in range(2)]
        r_q1T = [Res(), Res()]
        qm1T = [sb("qm1T%d" % i, [128, 2, 384], BF16, stC) for i in range(2)]
        r_qm1T = [Res(), Res()]
        k1T = [sb("k1T0", [128, 2, 9 * 128], BF16, stC)] * 2
        v1 = [sb("v1_0", [128, 9, 2, 65], BF16, stC)] * 2
        r_kv1 = [Res()] * 2
        T.op("dve", lambda e: e.memset(v1[0][:].rearrange("p a b c -> p (a b c)"), 1.0), w=[r_kv1[0]])
        y1 = [sb("y1_0", [128, 3, D], BF16, stC)] * 2
        r_y1 = [Res()] * 2
        sbs = [sb("sbs%d" % i, [128, 6, 128], F32, stC) for i in range(2)]
        r_sbs = [Res(), Res()]
        p1T = [sb("p1T%d" % i, [128, 6, 128], BF16, stC) for i in range(2)]
        r_p1T = [Res(), Res()]
        den = sb("den", [128, 6], F32, stC)
        r_den = Res()
        es1 = [sb("es1_0", [128, D], F32, stC)] * 2
        r_es1 = [Res()] * 2
        ot = [sb("ot0", [128, D], F32, stC)] * 2
        r_ot = [Res()] * 2
        pT = [sb("pTc%d" % i, [128, 2, 384], BF16, stC) for i in range(2)]
        r_pT = [Res() for _ in range(2)]
        yg2 = sb("yg2", [128, D], BF16, stC)
        r_yg2 = Res()
        ygT2 = sb("ygT2", [128, 8, 128], BF16, stC)
        r_ygT2 = Res()
        rec = sb("recc", [128, 9], F32, stC)
        r_rec = Res()
        r_out = Res()
        cc = {"t": 0, "n": 0, "pj": 0, "sw": 0, "z": 0, "o": 0}
        TBK, ABK = 0, 1

        def rms_scale(src, r_src, dst_bf, r_dst):
            s_ = cc["n"] % 4
            cc["n"] += 1
            T.op("act", lambda e: e.activation(out=junk[:], in_=src, func=AF.Square, accum_out=ssc[s_][:, 0:1]),
                 r=[r_src], w=[r_ssc[s_]])
            T.op("act", lambda e: e.activation(out=ssc[s_][:, 1:2], in_=ssc[s_][:, 0:1], func=AF.Ln,
                                               bias=epst[:, 0:1], scale=1.0 / D), r=[r_const], w=[r_ssc[s_]])
            T.op("act", lambda e: e.activation(out=ssc[s_][:, 2:3], in_=ssc[s_][:, 1:2], func=AF.Exp, scale=-0.5),
                 r=[], w=[r_ssc[s_]])
            if dst_bf is not None:
                T.op("dve", lambda e: e.tensor_scalar(out=dst_bf, in0=src, scalar1=ssc[s_][:, 2:3], scalar2=None,
                                                      op0=ALU.mult), r=[r_src, r_ssc[s_]], w=[r_dst])
            return s_

        def transpose8(src_bf, r_src, dst, r_dstT):
            pv = pbf(TBK)
            for k in range(8):
                T.op("pe", lambda e: e.transpose(pv[:, k * 128:(k + 1) * 128], src_bf[:, k * 128:(k + 1) * 128], ident[:]),
                     r=[r_src, r_const], w=[bank[TBK]])
            evac(dst, pv.rearrange("p (k t) -> p k t", k=8), r=[], w=[bank[TBK], r_dstT])

        def outproj(srcT, r_srcT, w_, xres, r_xres):
            for half in range(2):
                for k in range(8):
                    T.op("pe", lambda e: e.matmul(pf(2 + half), lhsT=srcT[:, k, :], rhs=w_[:, k, half * 512:(half + 1) * 512],
                                                  start=(k == 0), stop=(k == 7)),
                         r=[r_srcT, r_wC], w=[bank[2 + half]])
            T.op("dve", lambda e: e.tensor_tensor(out=xres, in0=pbig[:, 1024:2048], in1=xres, op=ALU.add),
                 r=[], w=[bank[2], bank[3], r_xres])

        def featproj(hT_, r_hT_, c0, dst, r_dst, scale, N=384):
            bk = 4 + cc["pj"] % 2
            cc["pj"] += 1
            for k in range(8):
                T.op("pe", lambda e: e.matmul(pf(bk, N), lhsT=wB[:, k, c0:c0 + 128], rhs=hT_[:, k, 0:N],
                                              start=(k == 0), stop=(k == 7)),
                     r=[r_wC, r_hT_], w=[bank[bk]])
            evac(dst, pf(bk, N), r=[], w=[bank[bk], r_dst], scale=scale)

        groups = [(m, g) for m in units for g in range(3)]

        def stageP(m, g, gb):
            ub = m % 2
            u0 = 1 + 3 * g
            items = []

            def t_a(i):
                u = u0 + i
                lt = UT * m + u
                qi = 9 * m + u - 1
                ys = i % 2
                T.dma("sp", yt[ys][:], YS[:, qi * D:(qi + 1) * D], r=[r_YS], w=[r_yt[ys]])
                T.dma("sp", gtt[ys][:], GT[lt * 128:(lt + 1) * 128, :], r=[r_GT], w=[r_gtt[ys]])
                T.op("dve", lambda e: e.tensor_tensor(out=yg[ys][:], in0=yt[ys][:], in1=gtt[ys][:], op=ALU.mult),
                     r=[r_yt[ys], r_gtt[ys]], w=[r_yg[ys]])

            def t_x(i):
                lt = UT * m + u0 + i
                T.dma("sp", x1g[gb][:, i, :], xm[lt * 128:(lt + 1) * 128, :], w=[r_x1g[gb][i]])

            def t_b(i):
                ys = i % 2
                transpose8(yg[ys], r_yg[ys], ygT[ys][:], r_ygT[ys])
                outproj(ygT[ys], r_ygT[ys], wO0, x1g[gb][:, i, :], r_x1g[gb][i])

            def t_c(i):
                ys = i % 2
                rms_scale(x1g[gb][:, i, :], r_x1g[gb][i], xs1[ys][:], r_xs1[ys])
                transpose8(xs1[ys], r_xs1[ys], h1T[gb][:, :, i * 128:(i + 1) * 128], r_h1T[gb])

            def vproj(i):
                bk = 4 + cc["pj"] % 2
                cc["pj"] += 1
                for k in range(8):
                    T.op("pe", lambda e: e.matmul(pf(bk, 128), lhsT=h1T[gb][:, k, i * 128:(i + 1) * 128],
                                                  rhs=wB[:, k, 1024:1152], start=(k == 0), stop=(k == 7)),
                         r=[r_wC, r_h1T[gb]], w=[bank[bk]])
                evac(v1[ub][:, u0 - 1 + i, :, 0:64], pf(bk, 128).rearrange("p (a b) -> p a b", a=2),
                     r=[], w=[bank[bk], r_kv1[ub]])

            pre = [lambda: t_a(0), lambda: t_a(1)]
            items.append(lambda: (t_x(0), t_x(1), t_x(2)))
            items.append(lambda: t_b(0))
            items.append(lambda: t_b(1))
            items.append(lambda: t_a(2))
            items.append(lambda: t_c(0))
            items.append(lambda: t_b(2))
            items.append(lambda: t_c(1))
            items.append(lambda: t_c(2))
            for gk in range(2):
                items.append(lambda gk=gk: featproj(h1T[gb], r_h1T[gb], 768 + gk * 128,
                                                    k1T[ub][:, gk, (u0 - 1) * 128:(u0 + 2) * 128], r_kv1[ub], None))
            for i in range(3):
                items.append(lambda i=i: vproj(i))
            for p in range(6):
                items.append(lambda p=p: featproj(h1T[gb], r_h1T[gb], p * 128, q1T[gb][:, p, :], r_q1T[gb], 0.125))
            for p in range(2):
                items.append(lambda p=p: featproj(h1T[gb], r_h1T[gb], 1152 + p * 128, qm1T[gb][:, p, :],
                                                  r_qm1T[gb], 0.125))
            return pre, items

        def stageQ(m, g, gb):
            ub = m % 2
            u0 = 1 + 3 * g
            items = []

            tiles = [i for i in range(3) if u0 + i >= 2]
            steps = []

            def swa_s(i, e_, kt, sw):
                u = u0 + i
                rows = slice(e_ * 64, (e_ + 1) * 64)
                kcol = (u - 2 + kt) * 128
                for gk in range(2):
                    T.op("pe", lambda e: e.matmul(pf(6 + gk, 384).rearrange("p (a b) -> p a b", a=3),
                                                  lhsT=k1T[ub][rows, gk, kcol:kcol + 128],
                                                  rhs=q1T[gb][rows, 3 * gk:3 * gk + 3, i * 128:(i + 1) * 128],
                                                  start=True, stop=True),
                         r=[r_kv1[ub], r_q1T[gb]], w=[bank[6 + gk]])
                for gk in range(2):
                    T.op("dve", lambda e: e.tensor_tensor(
                        out=sbs[sw][:, 3 * gk:3 * gk + 3, :],
                        in0=pf(6 + gk, 384).rearrange("p (a b) -> p a b", a=3),
                        in1=swac[:, kt, e_ + 6 * gk: e_ + 6 * gk + 5:2, :], op=ALU.add),
                        r=[r_const], w=[bank[6 + gk], r_sbs[sw]])
                bias_ap = halob[:, m:m + 1] if (kt == 0 and u == 2) else zero1[:, 0:1]
                T.op("act", lambda e: e.activation(out=p1T[sw][:], in_=sbs[sw][:], func=AF.Exp,
                                                   bias=bias_ap, scale=1.0),
                     r=[r_sbs[sw], r_const], w=[r_p1T[sw]])

            def swa_pv(i, e_, kt, sw):
                u = u0 + i
                for hh in range(6):
                    gk = hh // 3
                    T.op("pe", lambda e: e.matmul(pf(ABK, 65, hh * 65), lhsT=p1T[sw][:, hh, :],
                                                  rhs=v1[ub][:, u - 2 + kt, gk, :], start=(kt == 0 and hh == 0),
                                                  stop=True, skip_group_check=True),
                         r=[r_p1T[sw], r_kv1[ub]], w=[bank[ABK]])
                if kt == 1:
                    av = pf(ABK, 390).rearrange("p (a b) -> p a b", b=65)
                    T.op("dve", lambda e: e.tensor_tensor(out=den[:], in0=av[:, :, 64], in1=esink[:, e_::2], op=ALU.add),
                         r=[r_const], w=[bank[ABK], r_den])
                    T.op("dve", lambda e: e.reciprocal(out=den[:], in_=den[:]), r=[], w=[r_den])
                    T.op("dve", lambda e: e.tensor_tensor(
                        out=y1[gb][:, i, 0:768].rearrange("p (h c) -> p h c", c=64)[:, e_::2, :],
                        in0=av[:, :, 0:64], in1=den[:].unsqueeze(2).to_broadcast([128, 6, 64]), op=ALU.mult),
                        r=[r_den], w=[bank[ABK], r_y1[gb]])

            def mem_s(hm, ps):
                pr, e_ = divmod(hm, 2)
                rows = slice(e_ * 64, (e_ + 1) * 64)
                for kt2 in range(2):
                    T.op("pe", lambda e: e.matmul(pf(6 + kt2, 384), lhsT=kmem[rows, 1, pr, kt2 * 128:(kt2 + 1) * 128],
                                                  rhs=qm1T[gb][rows, pr, 0:384], start=True, stop=True),
                         r=[r_kmem, r_qm1T[gb]], w=[bank[6 + kt2]])
                sview = pbig[:, 6 * 512:8 * 512].rearrange("p (a b) -> p a b", a=2)[:, :, 0:384]
                T.op("act", lambda e: e.activation(out=pT[ps][:], in_=sview, func=AF.Exp),
                     r=[], w=[bank[6], bank[7], r_pT[ps]])

            def mem_pv(hm, ps):
                fl = True
                for qt in range(3):
                    for kt2 in range(2):
                        T.op("pe", lambda e: e.matmul(pf(ABK, 65, qt * 65), lhsT=pT[ps][:, kt2, qt * 128:(qt + 1) * 128],
                                                      rhs=vmem[:, 1, kt2, hm, :], start=fl, stop=True,
                                                      skip_group_check=True),
                             r=[r_pT[ps], r_vmem], w=[bank[ABK]])
                        fl = False
                av = pf(ABK, 195).rearrange("p (a b) -> p a b", b=65)
                T.op("dve", lambda e: e.reciprocal(out=rec[:, 0:3], in_=av[:, :, 64]), r=[], w=[bank[ABK], r_rec])
                T.op("dve", lambda e: e.tensor_tensor(out=y1[gb][:, 0:3, 768 + hm * 64: 768 + (hm + 1) * 64],
                                                      in0=av[:, :, 0:64],
                                                      in1=rec[:, 0:3].unsqueeze(2).to_broadcast([128, 3, 64]),
                                                      op=ALU.mult),
                     r=[r_rec], w=[bank[ABK], r_y1[gb]])

            k_ = 0
            for i in tiles:
                for e_ in range(2):
                    for kt in range(2):
                        sw = k_ % 2
                        steps.append((lambda i=i, e_=e_, kt=kt, sw=sw: swa_s(i, e_, kt, sw),
                                      lambda i=i, e_=e_, kt=kt, sw=sw: swa_pv(i, e_, kt, sw)))
                        k_ += 1
            for hm in range(4):
                steps.append((lambda hm=hm: mem_s(hm, hm % 2), lambda hm=hm: mem_pv(hm, hm % 2)))
            for k in range(len(steps) + 1):
                def f(k=k):
                    if k < len(steps):
                        steps[k][0]()
                    if k >= 1:
                        steps[k - 1][1]()
                items.append(f)

            def t_z1(i):
                for half in range(2):
                    for k in range(8):
                        T.op("pe", lambda e: e.matmul(pf(2 + half), lhsT=h1T[gb][:, k, i * 128:(i + 1) * 128],
                                                      rhs=wB[:, k, 1408 + half * 512: 1408 + (half + 1) * 512],
                                                      start=(k == 0), stop=(k == 7)),
                             r=[r_wC, r_h1T[gb]], w=[bank[2 + half]])
                zs = 0
                zps = pbig[:, 1024:2048]
                T.op("act", lambda e: e.activation(out=es1[zs][:], in_=zps, func=AF.Exp, scale=-1.0),
                     r=[], w=[bank[2], bank[3], r_es1[zs]])
                T.op("act", lambda e: e.copy(out=ot[zs][:], in_=zps), r=[], w=[bank[2], bank[3], r_ot[zs]])
                T.op("act", lambda e: e.activation(out=es1[zs][:], in_=es1[zs][:], func=AF.Ln, bias=1.0),
                     r=[], w=[r_es1[zs]])
                T.op("act", lambda e: e.activation(out=es1[zs][:], in_=es1[zs][:], func=AF.Exp, scale=-1.0),
                     r=[], w=[r_es1[zs]])
                T.op("dve", lambda e: e.tensor_tensor(out=es1[zs][:], in0=ot[zs][:], in1=es1[zs][:], op=ALU.mult),
                     r=[r_ot[zs]], w=[r_es1[zs]])
                T.op("dve", lambda e: e.tensor_tensor(out=yg2[:], in0=y1[gb][:, i, :], in1=es1[zs][:], op=ALU.mult),
                     r=[r_y1[gb], r_es1[zs]], w=[r_yg2])

            def t_z2(i):
                transpose8(yg2, r_yg2, ygT2[:], r_ygT2)

            def t_o(i):
                u = u0 + i
                outproj(ygT2, r_ygT2, wO1, x1g[gb][:, i, :], r_x1g[gb][i])
                s_ = rms_scale(x1g[gb][:, i, :], r_x1g[gb][i], None, None)
                T.op("dve", lambda e: e.scalar_tensor_tensor(out=ot[0][:], in0=x1g[gb][:, i, :], scalar=ssc[s_][:, 2:3],
                                                             in1=fgt[:], op0=ALU.mult, op1=ALU.mult),
                     r=[r_x1g[gb][i], r_ssc[s_], r_const], w=[r_ot[0]])
                orow = (m * 8 + u - 2) * 128
                T.dma("pool", out_d[orow:orow + 128, :], ot[0][:], r=[r_ot[0]], w=[r_out])

            for i in tiles:
                items.append(lambda i=i: t_z1(i))
                items.append(lambda i=i: t_z2(i))
                items.append(lambda i=i: t_o(i))
            return items

        stP = [stageP(groups[k][0], groups[k][1], k % 2) for k in range(len(groups))]
        for f in stP[0][0]:
            f()
        for gi_ in range(len(groups) + 1):
            main = list(stP[gi_][1]) if gi_ < len(groups) else []
            if gi_ + 1 < len(groups):
                main = main + list(stP[gi_ + 1][0])
            side = stageQ(groups[gi_ - 1][0], groups[gi_ - 1][1], (gi_ - 1) % 2) if gi_ >= 1 else []
            if main:
                run_interleaved(main, side)
            else:
                for f in side:
                    f()
        T.barrier()
    return nc


def host_consts(q):
    sl = _slopes()
    c = {}
    kx = np.zeros((64, S), dtype=np.float32)
    kx[0:3, :] = 1.0
    kt = (np.arange(S) // 128).astype(np.float32)
    kx[3:6, :] = kt[None, :]
    blk = np.arange(S) // 256
    kx[6 + (blk % 32), np.arange(S)] = 1.0
    c["kx"] = kx.astype(NPBF)
    tpos = np.zeros(NLOC, dtype=np.int64)
    for m in range(NU):
        base = (16 * m + 4 * q) * 256 - 256
        tpos[m * UT * 128:(m + 1) * UT * 128] = base + np.arange(UT * 128)
    kxo = np.zeros((64, NLOC), dtype=np.float32)
    kxo[0:3, :] = 1.0
    kto = np.floor_divide(tpos, 128).astype(np.float32)
    kxo[3:6, :] = kto[None, :]
    c["kxo"] = kxo.astype(NPBF)
    qac = np.zeros((H, 6, NLOC), dtype=NPBF)
    for h in range(H):
        cc = (-sl[h] * tpos.astype(np.float32)).astype(np.float32)
        hi, mid, lo = _split3(cc)
        qac[h, 0], qac[h, 1], qac[h, 2] = hi, mid, lo
        s1, s2, s3 = _split3(np.float32(sl[h]))
        qac[h, 3] = (np.float32(128.0) * s1.astype(np.float32)).astype(NPBF)
        qac[h, 4] = (np.float32(128.0) * s2.astype(np.float32)).astype(NPBF)
        qac[h, 5] = (np.float32(128.0) * s3.astype(np.float32)).astype(NPBF)
    c["qac"] = qac
    c["abias"] = (np.arange(128, dtype=np.float32)[:, None] * sl[None, :]).astype(np.float32)
    bm = np.zeros((NLT, NBLK), dtype=np.float32)
    for lt in range(NLT):
        m, u = divmod(lt, UT)
        i_own = 16 * m + 4 * q - 1 + u // 2
        bm[lt, :] = np.where(np.arange(NBLK) < i_own, 0.0, -1e30)
    c["bmask"] = np.ascontiguousarray(np.broadcast_to(bm.reshape(1, -1), (128, NLT * NBLK))).astype(np.float32)
    p = np.arange(128)[:, None, None]
    k2 = np.arange(2)[None, :, None]
    t = np.arange(256)[None, None, :]
    cm = np.where(t >= 128 * k2 + p, 0.0, -BIG).astype(np.float32)
    c["cm"] = cm.reshape(128, 512).astype(NPBF)
    c["ident"] = np.eye(128, dtype=np.float32).astype(NPBF)
    pk = np.arange(128, dtype=np.float32)[:, None, None]
    tt = np.arange(128, dtype=np.float32)[None, None, :]
    slh = sl[None, :, None]
    d_cur = tt - pk
    cur = np.where(d_cur >= 0, -slh * d_cur, NEG)
    d_prev = 128.0 + tt - pk
    prev = np.where(d_prev < 128, -slh * d_prev, NEG)
    c["swac"] = np.stack([prev, cur], axis=1).astype(np.float32).reshape(128, 2 * H * 128)
    hb = np.zeros((128, NU), dtype=np.float32)
    if q == 0:
        hb[:, 0] = NEG
    c["halob"] = hb
    return c


def make_in_maps(x, mem, norm_g, w_in_a, w_in_b, sinks_b, w_mem_kv, w_out, mem_norm_g, final_norm_g, cores):
    x = np.asarray(x, dtype=np.float32)
    in_maps = []
    gcols = np.concatenate([np.asarray(norm_g[0], np.float32).reshape(8, 128).T,
                            np.asarray(norm_g[1], np.float32).reshape(8, 128).T,
                            np.asarray(mem_norm_g, np.float32).reshape(8, 128).T], axis=1)
    fgr = np.ascontiguousarray(np.broadcast_to(np.asarray(final_norm_g, np.float32)[None, :], (128, D)))
    sinkr = np.ascontiguousarray(np.broadcast_to(np.asarray(sinks_b, np.float32).reshape(1, H), (128, H)))
    cc = {q: host_consts(q) for q in range(4)}
    for c in cores:
        b, q = divmod(c, 4)
        xmine = np.zeros((NLOC, D), dtype=np.float32)
        for m in range(NU):
            t0 = (16 * m + 4 * q) * 256 - 256
            lo = max(t0, 0)
            xmine[m * 1280 + (lo - t0):(m + 1) * 1280] = x[b, lo:t0 + 1280]
        d = {"xg": np.ascontiguousarray(x[b]), "xm": xmine, "memb": np.ascontiguousarray(np.asarray(mem, np.float32)[b]),
             "w_in_a": np.ascontiguousarray(np.asarray(w_in_a, np.float32)[0]),
             "w_in_b": np.ascontiguousarray(np.asarray(w_in_b, np.float32)[0]),
             "w_mem": np.ascontiguousarray(np.asarray(w_mem_kv, np.float32)),
             "w_out": np.ascontiguousarray(np.asarray(w_out, np.float32)),
             "gcols": np.ascontiguousarray(gcols), "fgr": fgr, "sinkr": sinkr}
        d.update(cc[q])
        in_maps.append(d)
    return in_maps


def kernel(x, mem, norm_g, w_in_a, w_in_b, sinks_b, w_mem_kv, w_out, mem_norm_g, final_norm_g):
    cores = list(range(8))
    in_maps = make_in_maps(x, mem, norm_g, w_in_a, w_in_b, sinks_b, w_mem_kv, w_out, mem_norm_g, final_norm_g, cores)
    nc = build()
    res = run_bass_kernel_spmd(nc, in_maps, core_ids=cores)
    out = np.zeros((2, S, D), dtype=np.float32)
    for c in cores:
        b, q = divmod(c, 4)
        o = res.results[c]["out"]
        for m in range(NU):
            t0 = (16 * m + 4 * q) * 256
            out[b, t0:t0 + 1024] = o[m * 1024:(m + 1) * 1024]
    return out
```
